# Optimizing a Trainium2 kernel written in Bass

```python
import math
import jax
import jax.numpy as jnp
from jax import lax
import numpy as np

D_MODEL = 1024
BATCH = 2
SEQ = 16384
DEPTH = 4

CHUNK = 64
N_A_LAYERS = max(1, DEPTH // 2)
N_B_LAYERS = DEPTH - N_A_LAYERS

GLA_HEADS = 4
GLA_DK = D_MODEL // 2 // GLA_HEADS
GLA_DV = D_MODEL // GLA_HEADS
GLA_GATE_RANK = 16
GLA_GATE_TAU = 16.0
GLA_HK = GLA_HEADS * GLA_DK
GLA_HV = GLA_HEADS * GLA_DV
GLA_IN = 2 * GLA_HK + 2 * GLA_HV + GLA_GATE_RANK

DIFF_HEADS = 8
DIFF_DH = D_MODEL // (2 * DIFF_HEADS)
DIFF_DV = 2 * DIFF_DH
DIFF_QK = DIFF_HEADS * DIFF_DH
Q_BLOCK = 128

REL_BUCKETS = 32
REL_MAX_DIST = 128

MOE_GROUPS = 4
MOE_EXPERTS_PER_GROUP = 4
MOE_EXPERTS = MOE_GROUPS * MOE_EXPERTS_PER_GROUP
MOE_TOPK = 2
MOE_FF = D_MODEL // 2

DEEPNORM_ALPHA = (2.0 * DEPTH) ** 0.25
DEEPNORM_BETA = (8.0 * DEPTH) ** -0.25
LN_EPS = 1e-5
NEG_INF = -1e30

kernel_name = 'hybrid_gla_diffattn_hmoe_yoco'


def layer_norm(x, g, b):
    xf = x.astype(jnp.float32)
    mu = jnp.mean(xf, -1, keepdims=True)
    var = jnp.mean(jnp.square(xf - mu), -1, keepdims=True)
    return ((xf - mu) * lax.rsqrt(var + LN_EPS)).astype(x.dtype) * g + b


def rms_norm(x, g):
    xf = x.astype(jnp.float32)
    return (xf * lax.rsqrt(jnp.mean(xf * xf, -1, keepdims=True) + LN_EPS)).astype(x.dtype) * g


def gla_mixer(x, w_in, w_gate2, b_gate, g_norm, w_out):
    B, S, _ = x.shape
    nc = S // CHUNK
    h = x @ w_in
    q, k, v, r, lr = jnp.split(h, [GLA_HK, 2 * GLA_HK, 2 * GLA_HK + GLA_HV, 2 * GLA_HK + 2 * GLA_HV], axis=-1)
    log_a = jax.nn.log_sigmoid((lr @ w_gate2 + b_gate).astype(jnp.float32)) / GLA_GATE_TAU

    def to_chunks(t, d):
        return t.reshape(B, nc, CHUNK, GLA_HEADS, d).transpose(1, 0, 3, 2, 4)

    qc = to_chunks(q * (GLA_DK ** -0.5), GLA_DK)
    kc = to_chunks(k, GLA_DK)
    vc = to_chunks(v, GLA_DV)
    cum = jnp.cumsum(to_chunks(log_a, GLA_DK), axis=3)
    last = cum[:, :, :, -1:, :]
    k_dec = kc * jnp.exp(last - cum).astype(kc.dtype)
    chunk_decay = jnp.exp(last[:, :, :, 0, :])

    def step(state, inp):
        q_c, k_c, v_c, dec = inp
        state = dec[..., None] * state + jnp.einsum('bhck,bhcv->bhkv', k_c.astype(jnp.float32), v_c.astype(jnp.float32))
        o = jnp.einsum('bhck,bhkv->bhcv', q_c.astype(jnp.float32), state)
        return state, o

    s0 = jnp.zeros((B, GLA_HEADS, GLA_DK, GLA_DV), jnp.float32)
    _, o = lax.scan(step, s0, (qc, k_dec, vc, chunk_decay))
    o = o.astype(x.dtype).transpose(1, 0, 3, 2, 4).reshape(B, S, GLA_HEADS, GLA_DV)
    o = rms_norm(o, g_norm.reshape(GLA_HEADS, GLA_DV)).reshape(B, S, GLA_HV)
    return (o * jax.nn.silu(r)) @ w_out


def shared_kv(x, w_kv):
    B, S, _ = x.shape
    kv = x @ w_kv
    k1, k2, v = jnp.split(kv, [DIFF_QK, 2 * DIFF_QK], axis=-1)
    k1 = k1.reshape(B, S, DIFF_HEADS, DIFF_DH).transpose(0, 2, 1, 3)
    k2 = k2.reshape(B, S, DIFF_HEADS, DIFF_DH).transpose(0, 2, 1, 3)
    v = v.reshape(B, S, DIFF_HEADS, DIFF_DV).transpose(0, 2, 1, 3)
    return k1, k2, v


def rel_bucket(rel):
    nb = REL_BUCKETS // 2
    max_exact = nb // 2
    base = jnp.where(rel > 0, nb, 0)
    n = jnp.abs(rel)
    large = max_exact + (jnp.log(jnp.maximum(n, 1).astype(jnp.float32) / max_exact)
                         / math.log(REL_MAX_DIST / max_exact) * (nb - max_exact)).astype(jnp.int32)
    large = jnp.minimum(large, nb - 1)
    return base + jnp.where(n < max_exact, n, large)


def diff_attention(x, w_q, lam_q1, lam_k1, lam_q2, lam_k2, g_sub, w_out, k1, k2, v, rel_table, lambda_init):
    B, S, _ = x.shape
    nb = S // Q_BLOCK
    q1, q2 = jnp.split(x @ w_q, 2, axis=-1)

    def blocks(t):
        return t.reshape(B, nb, Q_BLOCK, DIFF_HEADS, DIFF_DH).transpose(1, 0, 3, 2, 4)

    lam = (jnp.exp(jnp.sum(lam_q1 * lam_k1)) - jnp.exp(jnp.sum(lam_q2 * lam_k2)) + lambda_init).astype(jnp.float32)
    kpos = jnp.arange(S)
    scale = DIFF_DH ** -0.5

    def attend(args):
        i, q1b, q2b = args
        qpos = i * Q_BLOCK + jnp.arange(Q_BLOCK)
        bias = rel_table[rel_bucket(kpos[None, :] - qpos[:, None])].transpose(2, 0, 1).astype(jnp.float32)
        visible = (kpos[None, :] // CHUNK) <= (qpos[:, None] // CHUNK)

        def probs(qb, kk):
            s = jnp.einsum('bhqd,bhkd->bhqk', qb, kk).astype(jnp.float32) * scale + bias
            return jax.nn.softmax(jnp.where(visible, s, NEG_INF), axis=-1)

        w = probs(q1b, k1) - lam * probs(q2b, k2)
        return jnp.einsum('bhqk,bhkv->bhqv', w.astype(v.dtype), v)

    o = lax.map(attend, (jnp.arange(nb), blocks(q1), blocks(q2)))
    o = rms_norm(o, g_sub) * (1.0 - lambda_init)
    o = o.transpose(1, 0, 3, 2, 4).reshape(B, S, DIFF_HEADS * DIFF_DV)
    return o @ w_out


def hier_moe(x, w_group, b_group, w_router, b_router, w_gate, w_up, w_down):
    B, S, D = x.shape
    t = x.reshape(-1, D)
    g_logits = (t @ w_group).astype(jnp.float32) + b_group
    g_prob = jax.nn.softmax(g_logits, axis=-1)
    g_idx = jnp.argmax(g_logits, axis=-1)
    g_w = jnp.take_along_axis(g_prob, g_idx[:, None], axis=-1)
    e_logits = (t @ w_router).astype(jnp.float32).reshape(-1, MOE_GROUPS, MOE_EXPERTS_PER_GROUP) + b_router
    e_logits = jnp.take_along_axis(e_logits, g_idx[:, None, None], axis=1)[:, 0]
    top_w, top_i = lax.top_k(jax.nn.softmax(e_logits, axis=-1), MOE_TOPK)
    top_w = top_w / jnp.sum(top_w, -1, keepdims=True)
    within = jnp.sum(jax.nn.one_hot(top_i, MOE_EXPERTS_PER_GROUP, dtype=jnp.float32) * top_w[..., None], axis=1)
    combine = (jax.nn.one_hot(g_idx, MOE_GROUPS, dtype=jnp.float32)[:, :, None]
               * within[:, None, :] * g_w[:, :, None]).reshape(-1, MOE_EXPERTS).astype(t.dtype)
    y = jnp.zeros_like(t)
    for e in range(MOE_EXPERTS):
        hdn = jax.nn.silu(t @ w_gate[e]) * (t @ w_up[e])
        y = y + combine[:, e:e + 1] * (hdn @ w_down[e])
    return y.reshape(B, S, D)


def setup_inputs(seed: int = 0) -> dict:
    key = jax.random.key(seed)
    ks = jax.random.split(key, 24)

    def nrm(k, shape, scale):
        return jax.random.normal(k, shape, jnp.float32) * scale

    D = D_MODEL
    return {
        'x': nrm(ks[0], (BATCH, SEQ, D), 1.0),
        'a_w_in': nrm(ks[1], (N_A_LAYERS, D, GLA_IN), D ** -0.5),
        'a_w_gate2': nrm(ks[2], (N_A_LAYERS, GLA_GATE_RANK, GLA_HK), GLA_GATE_RANK ** -0.5),
        'a_b_gate': nrm(ks[3], (N_A_LAYERS, GLA_HK), 0.1),
        'a_g_norm': 1.0 + nrm(ks[4], (N_A_LAYERS, GLA_HV), 0.02),
        'a_w_out': nrm(ks[5], (N_A_LAYERS, GLA_HV, D), GLA_HV ** -0.5 * DEEPNORM_BETA),
        'kv_w': nrm(ks[6], (D, 2 * DIFF_QK + DIFF_HEADS * DIFF_DV), D ** -0.5),
        'b_w_q': nrm(ks[7], (N_B_LAYERS, D, 2 * DIFF_QK), D ** -0.5),
        'b_lam_q1': nrm(ks[8], (N_B_LAYERS, DIFF_DH), 0.1),
        'b_lam_k1': nrm(ks[9], (N_B_LAYERS, DIFF_DH), 0.1),
        'b_lam_q2': nrm(ks[10], (N_B_LAYERS, DIFF_DH), 0.1),
        'b_lam_k2': nrm(ks[11], (N_B_LAYERS, DIFF_DH), 0.1),
        'b_g_sub': 1.0 + nrm(ks[12], (N_B_LAYERS, DIFF_DV), 0.02),
        'b_w_out': nrm(ks[13], (N_B_LAYERS, DIFF_HEADS * DIFF_DV, D), (DIFF_HEADS * DIFF_DV) ** -0.5 * DEEPNORM_BETA),
        'rel_table': nrm(ks[14], (REL_BUCKETS, DIFF_HEADS), 0.2),
        'moe_w_group': nrm(ks[15], (DEPTH, D, MOE_GROUPS), D ** -0.5),
        'moe_b_group': nrm(ks[16], (DEPTH, MOE_GROUPS), 0.01),
        'moe_w_router': nrm(ks[17], (DEPTH, D, MOE_EXPERTS), D ** -0.5),
        'moe_b_router': nrm(ks[18], (DEPTH, MOE_GROUPS, MOE_EXPERTS_PER_GROUP), 0.01),
        'moe_w_gate': nrm(ks[19], (DEPTH, MOE_EXPERTS, D, MOE_FF), D ** -0.5),
        'moe_w_up': nrm(ks[20], (DEPTH, MOE_EXPERTS, D, MOE_FF), D ** -0.5),
        'moe_w_down': nrm(ks[21], (DEPTH, MOE_EXPERTS, MOE_FF, D), MOE_FF ** -0.5 * DEEPNORM_BETA),
        'ln_g': 1.0 + nrm(ks[22], (DEPTH, 2, D), 0.02),
        'ln_b': nrm(ks[23], (DEPTH, 2, D), 0.02),
    }


def reference(x, a_w_in, a_w_gate2, a_b_gate, a_g_norm, a_w_out, kv_w, b_w_q, b_lam_q1, b_lam_k1,
              b_lam_q2, b_lam_k2, b_g_sub, b_w_out, rel_table, moe_w_group, moe_b_group, moe_w_router,
              moe_b_router, moe_w_gate, moe_w_up, moe_w_down, ln_g, ln_b):
    h = x
    k1 = k2 = v = None
    for layer in range(DEPTH):
        if layer < N_A_LAYERS:
            mix = gla_mixer(h, a_w_in[layer], a_w_gate2[layer], a_b_gate[layer], a_g_norm[layer], a_w_out[layer])
        else:
            j = layer - N_A_LAYERS
            lambda_init = 0.8 - 0.6 * math.exp(-0.3 * layer)
            mix = diff_attention(h, b_w_q[j], b_lam_q1[j], b_lam_k1[j], b_lam_q2[j], b_lam_k2[j], b_g_sub[j],
                                 b_w_out[j], k1, k2, v, rel_table, lambda_init)
        h = layer_norm(DEEPNORM_ALPHA * h + mix, ln_g[layer, 0], ln_b[layer, 0])
        ffn = hier_moe(h, moe_w_group[layer], moe_b_group[layer], moe_w_router[layer], moe_b_router[layer],
                       moe_w_gate[layer], moe_w_up[layer], moe_w_down[layer])
        h = layer_norm(DEEPNORM_ALPHA * h + ffn, ln_g[layer, 1], ln_b[layer, 1])
        if layer == N_A_LAYERS - 1:
            k1, k2, v = shared_kv(h, kv_w)
    return h
```

```python
from contextlib import ExitStack
import math
import numpy as np
import concourse.bass as bass
import concourse.mybir as mybir
from concourse.bass_utils import run_bass_kernel_spmd

F32 = mybir.dt.float32
BF16 = mybir.dt.bfloat16
AF = mybir.ActivationFunctionType
ALU = mybir.AluOpType
AX = mybir.AxisListType


class Sched:
    ENG = ("pe", "act", "dve", "pool", "sp")

    def __init__(self, nc, es):
        self.nc = nc
        self.es = es
        self.eng = {"pe": nc.tensor, "act": nc.scalar, "dve": nc.vector, "pool": nc.gpsimd, "sp": nc.sync}
        self.sem = {e: es.enter_context(nc.semaphore("s_" + e)) for e in self.ENG}
        self.cnt = {e: 0 for e in self.ENG}
        self.seen = {e: {} for e in self.ENG}
        self.snaps = {e: [None] for e in self.ENG}
        self.dsem = {}
        self.dcnt = {}
        self.lastw = {}
        self.readers = {}
        self.nwait = 0
        self.ninst = 0

    def _deps(self, r, w):
        deps = []
        for k in r:
            t = self.lastw.get(k)
            if t is not None:
                deps.append(t)
        for k in w:
            t = self.lastw.get(k)
            if t is not None:
                deps.append(t)
            deps.extend(self.readers.get(k, ()))
        return deps

    def _wait(self, e, deps, skip_dma_sem=None):
        seen = self.seen[e]
        need = {}
        for (src, val) in deps:
            if src == e and e == "pe":
                continue
            if skip_dma_sem is not None and src == skip_dma_sem:
                continue
            if seen.get(src, 0) >= val:
                continue
            if need.get(src, 0) < val:
                need[src] = val
        if not need:
            return
        seen = dict(seen)
        for src, val in need.items():
            if isinstance(src, tuple):
                self.eng[e].wait_ge(self.dsem[src[1]], val)
            else:
                self.eng[e].wait_ge(self.sem[src], val)
                snap = self.snaps[src][val]
                if snap:
                    for s2, v2 in snap.items():
                        if seen.get(s2, 0) < v2:
                            seen[s2] = v2
            if seen.get(src, 0) < val:
                seen[src] = val
            self.nwait += 1
        self.seen[e] = seen

    def _commit(self, tok, r, w):
        for k in w:
            self.lastw[k] = tok
            self.readers[k] = []
        for k in r:
            self.readers.setdefault(k, []).append(tok)

    def op(self, e, fn, r=(), w=()):
        px = [k for k in r if isinstance(k, tuple) and k[0] == "ps"]
        if px:
            r = [k for k in r if k not in px]
            w = list(w) + px
        self._wait(e, self._deps(r, w))
        ins = fn()
        self.cnt[e] += 1
        ins.then_inc(self.sem[e], 1)
        self.snaps[e].append(self.seen[e])
        self._commit((e, self.cnt[e]), r, w)
        self.ninst += 1
        return ins

    def dma(self, q, out, in_, r=(), w=(), sem=None):
        assert sem is not None
        if sem not in self.dsem:
            self.dsem[sem] = self.es.enter_context(self.nc.semaphore("d_%d" % len(self.dsem)))
            self.dcnt[sem] = 0
        self._wait(q, self._deps(r, w), skip_dma_sem=("dma", sem))
        ins = self.eng[q].dma_start(out=out, in_=in_)
        self.dcnt[sem] += 16
        ins.then_inc(self.dsem[sem], 16)
        self._commit((("dma", sem), self.dcnt[sem]), r, w)
        self.ninst += 1
        return ins

    def finish(self, keys):
        deps = []
        for k in keys:
            t = self.lastw.get(k)
            if t is not None:
                deps.append(t)
            deps.extend(self.readers.get(k, ()))
        self._wait("sp", deps)


def _sched_barrier(self):
    for e in self.ENG:
        deps = [(f, self.cnt[f]) for f in self.ENG if self.cnt[f] > 0]
        deps += [(("dma", k), v) for k, v in self.dcnt.items() if v > 0]
        self._wait(e, deps)
    self.lastw = {}
    self.readers = {}


def _sched_coll(self, kind, groups, in_ap, out_ap):
    self.barrier()
    if "cc" not in self.dsem:
        self.dsem["cc"] = self.es.enter_context(self.nc.semaphore("d_cc"))
        self.dcnt["cc"] = 0
    ins = self.nc.gpsimd.collective_compute(kind, ALU.bypass, replica_groups=groups, ins=[in_ap], outs=[out_ap])
    self.dcnt["cc"] += 1
    ins.then_inc(self.dsem["cc"])
    self.ninst += 1
    self.barrier()


Sched.barrier = _sched_barrier
Sched.coll = _sched_coll


def _sched_dma_fn(self, q, fn, r=(), w=(), sem=None):
    if sem not in self.dsem:
        self.dsem[sem] = self.es.enter_context(self.nc.semaphore("d_%d" % len(self.dsem)))
        self.dcnt[sem] = 0
    self._wait(q, self._deps(r, w), skip_dma_sem=("dma", sem))
    ins = fn()
    self.dcnt[sem] += 16
    ins.then_inc(self.dsem[sem], 16)
    self._commit((("dma", sem), self.dcnt[sem]), r, w)
    self.ninst += 1
    return ins


Sched.dma_fn = _sched_dma_fn


def _sched_coll_multi(self, kind, groups, pairs):
    self.barrier()
    if "cc" not in self.dsem:
        self.dsem["cc"] = self.es.enter_context(self.nc.semaphore("d_cc"))
        self.dcnt["cc"] = 0
    for (in_ap, out_ap) in pairs:
        ins = self.nc.gpsimd.collective_compute(kind, ALU.bypass, replica_groups=groups, ins=[in_ap], outs=[out_ap])
        self.dcnt["cc"] += 1
        ins.then_inc(self.dsem["cc"])
        self.ninst += 1
        self.nc.gpsimd.wait_ge(self.dsem["cc"], self.dcnt["cc"])
    self.barrier()


Sched.coll_multi = _sched_coll_multi


def _sched_coll_async(self, kind, groups, in_ap, out_ap, wait_sems):
    if "cc" not in self.dsem:
        self.dsem["cc"] = self.es.enter_context(self.nc.semaphore("d_cc"))
        self.dcnt["cc"] = 0
    deps = [(("dma", k), self.dcnt[k]) for k in wait_sems if self.dcnt.get(k, 0) > 0]
    if self.dcnt["cc"] > 0:
        deps.append((("dma", "cc"), self.dcnt["cc"]))
    self._wait("pool", deps)
    ins = self.nc.gpsimd.collective_compute(kind, ALU.bypass, replica_groups=groups, ins=[in_ap], outs=[out_ap])
    self.dcnt["cc"] += 1
    ins.then_inc(self.dsem["cc"])
    self.ninst += 1


Sched.coll_async = _sched_coll_async

ALPHA = (2.0 * 4) ** 0.25
LN_EPS = 1e-5
TAU = 16.0
NEGB = -30000.0

LN_EPS = 1e-5
TAU = 16.0


def gla_consts():
    s = np.arange(128)
    same = (s[:, None] // 64) == (s[None, :] // 64)
    trirev = ((s[:, None] > s[None, :]) & same).astype(np.float32) * (-1.0 / TAU)
    cind = np.zeros((128, 2), np.float32)
    cind[:64, 0] = -1.0 / TAU
    cind[64:, 1] = -1.0 / TAU
    return {"ident": np.eye(128, dtype=np.float32), "trirev": trirev, "cind": cind,
            "ones1": np.ones((1, 128), np.float32)}


LN_EPS = 1e-5
NEGB = -30000.0


def rel_bucket_np(rel):
    nb = 16
    max_exact = 8
    base = np.where(rel > 0, nb, 0)
    n = np.abs(rel)
    large = max_exact + (np.log(np.maximum(n, 1).astype(np.float32) / np.float32(max_exact))
                         / np.float32(math.log(128 / max_exact)) * np.float32(nb - max_exact)).astype(np.int32)
    large = np.minimum(large, nb - 1)
    return base + np.where(n < max_exact, n, large)


def att_bias_tiles(rel_table, heads):
    kl = np.arange(128)[:, None]
    ql = np.arange(128)[None, :]
    out = np.zeros((len(heads), 2, 128, 128), np.float32)
    bd = rel_bucket_np(kl - ql)
    bp = rel_bucket_np(kl - ql - 128)
    vis = (kl // 64) <= (ql // 64)
    for i, h in enumerate(heads):
        out[i, 0] = np.where(vis, rel_table[bd, h], np.float32(NEGB))
        out[i, 1] = rel_table[bp, h]
    return out


def emit_post(nc, S, ps, D, tag, NTOK=4096, SG=1024, NEXP=16, do_A=True, do_B=True, do_R=True, stage=9):
    D_ = 1024
    D, DD = D_, D
    FF = 512
    y_d, hp_d, wout_d, lnp_d, wr_d, rb_d = DD["y"], DD["hp"], DD["wout"], DD["lnp"], DD["wr"], DD["rb"]
    wg_d, wu_d, wd_d, id_d, out_d = DD["wg"], DD["wu"], DD["wd"], DD["ident"], DD["out"]
    chunk_done = DD.get("chunk_done")
    NSG = NTOK // SG
    TPS = SG // 128
    GPS = SG // 512
    with ExitStack() as es:
        def sb(name, shape, dt=F32):
            return es.enter_context(nc.sbuf_tensor("sb_" + tag + "_" + name, shape, dt))
        ident = sb("ident", [128, 128])
        idx = sb("idx", [128, 128], mybir.dt.uint32)
        wout = sb("wout", [128, 8, D], BF16)
        wr = sb("wr", [128, 8, 20])
        rb = sb("rb", [128, 20])
        lnp = sb("lnp", [128, 4, D])
        hT = sb("hT", [128, 8, SG], BF16)
        yacc = sb("yacc", [128, TPS, D])
        comb = sb("comb", [128, TPS, 16])
        wg = [sb("wg%d" % i, [128, 8, FF], BF16) for i in range(2)]
        wu = [sb("wu%d" % i, [128, 8, FF], BF16) for i in range(2)]
        wd = [sb("wd%d" % i, [128, 4, D], BF16) for i in range(2)]
        yt = [sb("yt%d" % i, [128, D]) for i in range(2)]
        hpt = [sb("hpt%d" % i, [128, D]) for i in range(2)]
        yT = [sb("yT%d" % i, [128, 8, 128], BF16) for i in range(2)]
        zt = [sb("z%d" % i, [128, D]) for i in range(2)]
        hT32 = [sb("hT32_%d" % i, [128, 8, 128]) for i in range(2)]
        sg = [sb("sg%d" % i, [128, 512]) for i in range(2)]
        hdn = [sb("hdn%d" % i, [128, 4, 512], BF16) for i in range(2)]
        ot = [sb("ot%d" % i, [128, D]) for i in range(2)]
        st = sb("stats", [128, 2, 6])
        mv = sb("mv", [128, 2])
        rstd = sb("rstd", [128, 1])
        lg = sb("lg", [128, 20])
        r_gmax = sb("r_gmax", [128, 1])
        r_gmask = sb("r_gmask", [128, 4])
        r_gt = sb("r_gt", [128, 4])
        r_gsum = sb("r_gsum", [128, 1])
        r_m1 = sb("r_m1", [128, 4])
        r_m2 = sb("r_m2", [128, 4])
        r_is1 = sb("r_is1", [128, 4, 4])
        r_is2 = sb("r_is2", [128, 4, 4])
        r_e2 = sb("r_e2", [128, 4, 4])
        r_w1 = sb("r_w1", [128, 4])
        r_w2 = sb("r_w2", [128, 4])
        r_gs = sb("r_gs", [128, 4])

        V, A, P, G = nc.vector, nc.scalar, nc.tensor, nc.gpsimd

        S.dma("sp", ident[:], id_d[:, :], w=["ident"], sem="ident")
        S.dma("sp", idx[:], DD["idx"], w=["idx"], sem="idx")
        S.dma("pool", wout[:], wout_d.rearrange("(kc p) f -> p kc f", p=128), w=["wout"], sem="wout")
        S.dma("sp", wr[:], wr_d.rearrange("(kc p) f -> p kc f", p=128), w=["wr"], sem="wr")
        S.dma("sp", rb[:], rb_d[0:1, :].partition_broadcast(128), w=["rb"], sem="rb")
        S.dma("sp", lnp[:], lnp_d.partition_broadcast(128), w=["lnp"], sem="lnp")

        def load_expert(e, slot):
            S.dma("pool", wg[slot][:], wg_d[e].rearrange("(kc p) f -> p kc f", p=128), w=[("wg", slot)], sem=("wg", slot))
            S.dma("pool", wu[slot][:], wu_d[e].rearrange("(kc p) f -> p kc f", p=128), w=[("wu", slot)], sem=("wu", slot))
            S.dma("pool", wd[slot][:], wd_d[e].rearrange("(kc p) f -> p kc f", p=128), w=[("wd", slot)], sem=("wd", slot))

        def layernorm(src, dst, gi, sl):
            for c in range(2):
                S.op("dve", lambda c=c: V.bn_stats(out=st[:, c, :], in_=src[:, c * 512:(c + 1) * 512]), r=[sl], w=["st"])
            S.op("dve", lambda: V.bn_aggr(out=mv[:], in_=st[:].rearrange("p a b -> p (a b)")), r=["st"], w=["mv"])
            S.op("dve", lambda: V.tensor_scalar_add(out=rstd[:], in0=mv[:, 1:2], scalar1=LN_EPS), r=["mv"], w=["rstd"])
            S.op("act", lambda: A.activation(out=rstd[:], in_=rstd[:], func=AF.Ln), r=["rstd"], w=["rstd"])
            S.op("act", lambda: A.activation(out=rstd[:], in_=rstd[:], func=AF.Exp, scale=-0.5), r=["rstd"], w=["rstd"])
            S.op("dve", lambda: V.tensor_scalar(out=dst, in0=src, scalar1=mv[:, 0:1], scalar2=rstd[:],
                                                op0=ALU.subtract, op1=ALU.mult), r=["mv", "rstd", sl], w=[sl])
            S.op("pool", lambda: G.tensor_tensor(out=dst, in0=dst, in1=lnp[:, gi, :], op=ALU.mult), r=["lnp", sl], w=[sl])
            S.op("pool", lambda: G.tensor_tensor(out=dst, in0=dst, in1=lnp[:, gi + 1, :], op=ALU.add), r=["lnp", sl], w=[sl])

        pending = []
        cur_e = [0]
        nload = 0
        for s in range(NSG):
            if do_B:
                load_expert(0, nload % 2)
            for t in range(TPS if do_A else 0):
                tok0 = s * SG + t * 128
                sl = t % 2
                tg = s * TPS + t
                for r_ in range(4):
                    S.dma_fn("pool", lambda: G.indirect_dma_start(out=yt[sl][:, r_ * 256:(r_ + 1) * 256], out_offset=None, in_=y_d,
                                                                  in_offset=bass.IndirectOffsetOnAxis(ap=idx[:, r_ * 32 + tg:r_ * 32 + tg + 1], axis=0)),
                             r=["idx"], w=[("yt", sl)], sem=("yt", sl))
                S.dma("sp", hpt[sl][:], hp_d[tok0:tok0 + 128, :], w=[("hp", sl)], sem=("hp", sl))
                for hb in range(2):
                    for j in range(4):
                        kc = hb * 4 + j
                        S.op("pe", lambda kc=kc, j=j, hb=hb: P.transpose(out=ps[hb][:, j * 128:(j + 1) * 128],
                                                                        in_=yt[sl][:, kc * 128:(kc + 1) * 128], identity=ident[:]),
                             r=[("yt", sl), "ident"], w=[("ps", hb)])
                S.op("act", lambda: A.copy(out=yT[sl][:, 0:4, :], in_=ps[0][:].rearrange("p (a b) -> p a b", a=4)),
                     r=[("ps", 0)], w=[("yT", sl, 0)])
                S.op("dve", lambda: V.tensor_copy(out=yT[sl][:, 4:8, :], in_=ps[1][:].rearrange("p (a b) -> p a b", a=4)),
                     r=[("ps", 1)], w=[("yT", sl, 1)])
                if stage < 2:
                    continue
                for half in range(2):
                    for kc in range(8):
                        S.op("pe", lambda kc=kc, half=half: P.matmul(ps[2 + half][:], lhsT=yT[sl][:, kc, :],
                                                                     rhs=wout[:, kc, half * 512:(half + 1) * 512],
                                                                     start=(kc == 0), stop=(kc == 7)),
                             r=[("yT", sl, kc // 4), "wout"], w=[("ps", 2 + half)])
                if stage < 3:
                    continue
                for half in range(2):
                    S.op("dve", lambda half=half: V.scalar_tensor_tensor(out=zt[sl][:, half * 512:(half + 1) * 512],
                                                                         in0=hpt[sl][:, half * 512:(half + 1) * 512], scalar=ALPHA,
                                                                         in1=ps[2 + half][:], op0=ALU.mult, op1=ALU.add),
                         r=[("hp", sl), ("ps", 2 + half)], w=[("z", sl)])
                layernorm(zt[sl][:], zt[sl][:], 0, ("z", sl))
                if stage < 4:
                    continue
                S.op("act", lambda: A.mul(out=yacc[:, t, :], in_=zt[sl][:], mul=ALPHA), r=[("z", sl)], w=[("yacc", t)])
                for hb in range(2):
                    for j in range(4):
                        kc = hb * 4 + j
                        S.op("pe", lambda kc=kc, j=j, hb=hb: P.transpose(out=ps[4 + hb][:, j * 128:(j + 1) * 128],
                                                                        in_=zt[sl][:, kc * 128:(kc + 1) * 128], identity=ident[:]),
                             r=[("z", sl), "ident"], w=[("ps", 4 + hb)])
                for hb in range(2):
                    S.op("act", lambda hb=hb: A.copy(out=hT32[sl][:, hb * 4:(hb + 1) * 4, :],
                                                     in_=ps[4 + hb][:].rearrange("p (a b) -> p a b", a=4)),
                         r=[("ps", 4 + hb)], w=[("hT32", sl, hb)])
                    S.op("dve", lambda hb=hb: V.tensor_copy(out=hT[:, hb * 4:(hb + 1) * 4, t * 128:(t + 1) * 128],
                                                            in_=ps[4 + hb][:].rearrange("p (a b) -> p a b", a=4)),
                         r=[("ps", 4 + hb)], w=[("hT", t)])
                if stage < 5:
                    continue
                for kc in range(8):
                    S.op("pe", lambda kc=kc: P.matmul(ps[6][:, 0:20], lhsT=hT32[sl][:, kc, :], rhs=wr[:, kc, :],
                                                      start=(kc == 0), stop=(kc == 7)),
                         r=[("hT32", sl, kc // 4), "wr"], w=[("ps", 6)])
                if not do_R:
                    continue
                RT = ["rt"]
                S.op("dve", lambda: V.tensor_tensor(out=lg[:], in0=ps[6][:, 0:20], in1=rb[:], op=ALU.add),
                     r=[("ps", 6), "rb"], w=RT)
                S.op("dve", lambda: V.tensor_reduce(out=r_gmax[:], in_=lg[:, 0:4], axis=AX.X, op=ALU.max), r=RT, w=RT)
                S.op("dve", lambda: V.tensor_scalar(out=r_gmask[:], in0=lg[:, 0:4], scalar1=r_gmax[:], scalar2=None,
                                                    op0=ALU.is_ge), r=RT, w=RT)
                S.op("dve", lambda: V.tensor_scalar(out=r_gt[:], in0=lg[:, 0:4], scalar1=r_gmax[:], scalar2=None,
                                                    op0=ALU.subtract), r=RT, w=RT)
                S.op("act", lambda: A.activation(out=r_gt[:], in_=r_gt[:], func=AF.Exp, accum_out=r_gsum[:]), r=RT, w=RT)
                S.op("dve", lambda: V.reciprocal(out=r_gsum[:], in_=r_gsum[:]), r=RT, w=RT)
                S.op("dve", lambda: V.tensor_scalar(out=r_gs[:], in0=r_gmask[:], scalar1=r_gsum[:], scalar2=None,
                                                    op0=ALU.mult), r=RT, w=RT)
                ev = lg[:, 4:20].rearrange("p (g j) -> p g j", g=4)
                S.op("dve", lambda: V.tensor_reduce(out=r_m1[:], in_=ev, axis=AX.X, op=ALU.max), r=RT, w=RT)
                S.op("dve", lambda: V.tensor_tensor(out=r_is1[:], in0=ev, in1=r_m1[:].unsqueeze(2).to_broadcast([128, 4, 4]),
                                                    op=ALU.is_equal), r=RT, w=RT)
                S.op("dve", lambda: V.scalar_tensor_tensor(out=r_e2[:], in0=r_is1[:], scalar=-1e30, in1=ev,
                                                           op0=ALU.mult, op1=ALU.add), r=RT, w=RT)
                S.op("dve", lambda: V.tensor_reduce(out=r_m2[:], in_=r_e2[:], axis=AX.X, op=ALU.max), r=RT, w=RT)
                S.op("dve", lambda: V.tensor_tensor(out=r_is2[:], in0=r_e2[:], in1=r_m2[:].unsqueeze(2).to_broadcast([128, 4, 4]),
                                                    op=ALU.is_equal), r=RT, w=RT)
                S.op("dve", lambda: V.tensor_tensor(out=r_w1[:], in0=r_m2[:], in1=r_m1[:], op=ALU.subtract), r=RT, w=RT)
                S.op("act", lambda: A.activation(out=r_w1[:], in_=r_w1[:], func=AF.Exp), r=RT, w=RT)
                S.op("dve", lambda: V.tensor_scalar_add(out=r_w1[:], in0=r_w1[:], scalar1=1.0), r=RT, w=RT)
                S.op("dve", lambda: V.reciprocal(out=r_w1[:], in_=r_w1[:]), r=RT, w=RT)
                S.op("dve", lambda: V.tensor_scalar(out=r_w2[:], in0=r_w1[:], scalar1=-1.0, scalar2=1.0,
                                                    op0=ALU.mult, op1=ALU.add), r=RT, w=RT)
                S.op("dve", lambda: V.tensor_tensor(out=r_w1[:], in0=r_w1[:], in1=r_gs[:], op=ALU.mult), r=RT, w=RT)
                S.op("dve", lambda: V.tensor_tensor(out=r_w2[:], in0=r_w2[:], in1=r_gs[:], op=ALU.mult), r=RT, w=RT)
                S.op("dve", lambda: V.tensor_tensor(out=r_is1[:], in0=r_is1[:], in1=r_w1[:].unsqueeze(2).to_broadcast([128, 4, 4]),
                                                    op=ALU.mult), r=RT, w=RT)
                S.op("dve", lambda: V.tensor_tensor(out=r_is2[:], in0=r_is2[:], in1=r_w2[:].unsqueeze(2).to_broadcast([128, 4, 4]),
                                                    op=ALU.mult), r=RT, w=RT)
                S.op("dve", lambda: V.tensor_tensor(out=comb[:, t, :].rearrange("p (g j) -> p g j", g=4), in0=r_is1[:], in1=r_is2[:],
                                                    op=ALU.add), r=RT, w=[("comb", t)])
            for e in range(NEXP if do_B else 0):
                slot = nload % 2
                if pending and e in (3, 6, 9, 12):
                    chunk_done(pending.pop(0))
                nload += 1
                if e + 1 < NEXP:
                    load_expert(e + 1, nload % 2)
                for g in range(GPS):
                    hs = g % 2
                    for fc in range(4):
                        pg = fc % 2
                        for kc in range(8):
                            S.op("pe", lambda kc=kc, fc=fc, pg=pg: P.matmul(ps[pg][:], lhsT=wg[slot][:, kc, fc * 128:(fc + 1) * 128],
                                                                          rhs=hT[:, kc, g * 512:(g + 1) * 512],
                                                                          start=(kc == 0), stop=(kc == 7)),
                                 r=[("wg", slot)] + [("hT", g * 4 + i) for i in range(4)], w=[("ps", pg)])
                        for kc in range(8):
                            S.op("pe", lambda kc=kc, fc=fc, pg=pg: P.matmul(ps[2 + pg][:], lhsT=wu[slot][:, kc, fc * 128:(fc + 1) * 128],
                                                                          rhs=hT[:, kc, g * 512:(g + 1) * 512],
                                                                          start=(kc == 0), stop=(kc == 7)),
                                 r=[("wu", slot)] + [("hT", g * 4 + i) for i in range(4)], w=[("ps", 2 + pg)])
                        S.op("act", lambda pg=pg: A.activation(out=sg[pg][:], in_=ps[pg][:], func=AF.Silu),
                             r=[("ps", pg)], w=[("sg", pg)])
                        S.op("dve", lambda pg=pg, fc=fc: V.tensor_tensor(out=hdn[hs][:, fc, :], in0=ps[2 + pg][:], in1=sg[pg][:], op=ALU.mult),
                             r=[("ps", 2 + pg), ("sg", pg)], w=[("hdn", hs, fc)])
                    for tt in range(4):
                        t = g * 4 + tt
                        for half in range(2):
                            pb = 4 + (tt * 2 + half) % 4
                            for fc in range(4):
                                S.op("pe", lambda fc=fc, half=half, pb=pb, tt=tt: P.matmul(ps[pb][:], lhsT=hdn[hs][:, fc, tt * 128:(tt + 1) * 128],
                                                                                       rhs=wd[slot][:, fc, half * 512:(half + 1) * 512],
                                                                                       start=(fc == 0), stop=(fc == 3)),
                                     r=[("hdn", hs, fc), ("wd", slot)], w=[("ps", pb)])
                            S.op("dve", lambda half=half, pb=pb, t=t: V.scalar_tensor_tensor(
                                out=yacc[:, t, half * 512:(half + 1) * 512], in0=ps[pb][:], scalar=comb[:, t, e:e + 1],
                                in1=yacc[:, t, half * 512:(half + 1) * 512], op0=ALU.mult, op1=ALU.add),
                                r=[("ps", pb), ("comb", t), ("yacc", t)], w=[("yacc", t)])
            for t in range(TPS):
                tok0 = s * SG + t * 128
                sl = t % 2
                src = yacc[:, t, :]
                for c in range(2):
                    S.op("dve", lambda c=c: V.bn_stats(out=st[:, c, :], in_=src[:, c * 512:(c + 1) * 512]), r=[("yacc", t)], w=["st"])
                S.op("dve", lambda: V.bn_aggr(out=mv[:], in_=st[:].rearrange("p a b -> p (a b)")), r=["st"], w=["mv"])
                S.op("dve", lambda: V.tensor_scalar_add(out=rstd[:], in0=mv[:, 1:2], scalar1=LN_EPS), r=["mv"], w=["rstd"])
                S.op("act", lambda: A.activation(out=rstd[:], in_=rstd[:], func=AF.Ln), r=["rstd"], w=["rstd"])
                S.op("act", lambda: A.activation(out=rstd[:], in_=rstd[:], func=AF.Exp, scale=-0.5), r=["rstd"], w=["rstd"])
                S.op("dve", lambda: V.tensor_scalar(out=ot[sl][:], in0=src, scalar1=mv[:, 0:1], scalar2=rstd[:],
                                                    op0=ALU.subtract, op1=ALU.mult), r=["mv", "rstd", ("yacc", t)], w=[("ot", sl)])
                S.op("pool", lambda: G.tensor_tensor(out=ot[sl][:], in0=ot[sl][:], in1=lnp[:, 2, :], op=ALU.mult), r=["lnp", ("ot", sl)], w=[("ot", sl)])
                S.op("pool", lambda: G.tensor_tensor(out=ot[sl][:], in0=ot[sl][:], in1=lnp[:, 3, :], op=ALU.add), r=["lnp", ("ot", sl)], w=[("ot", sl)])
                S.dma("sp", out_d[tok0:tok0 + 128, :], ot[sl][:], r=[("ot", sl)], sem=("ot", sl))
                if t % 2 == 1 and chunk_done:
                    pending.append(tok0 // 256)
        while pending:
            chunk_done(pending.pop(0))


def emit_gla(nc, S, ps, DD, tag, S_LEN=16384):
    D = 1024
    NT = S_LEN // 128
    h_d, wcat_d, wg2_d, bg_d, gn_d = DD["h"], DD["wcat"], DD["wg2"], DD["bg"], DD["gn"]
    id_d, tr_d, ci_d, on_d, y_d = DD["ident"], DD["trirev"], DD["cind"], DD["ones1"], DD["yout"]
    hmap = DD.get("hmap", lambda n: n)
    chunk_done = DD.get("chunk_done")
    with ExitStack() as es:
        def sb(name, shape, dt=F32):
            return es.enter_context(nc.sbuf_tensor("sb_" + tag + "_" + name, shape, dt))
        ident = sb("ident", [128, 128])
        trirev = sb("trirev", [128, 128])
        cind = sb("cind", [128, 2])
        ones1 = sb("ones1", [1, 128])
        wcat = sb("wcat", [128, 8, 784], BF16)
        wg2 = sb("wg2", [16, 128])
        bg = sb("bg", [1, 128])
        gn = sb("gn", [128, 256])
        state = sb("state", [128, 256])
        qlo = sb("qlo", [128, 128])
        qhi = sb("qhi", [128, 128])
        ht = [sb("ht%d" % i, [128, D]) for i in range(2)]
        hT = [sb("hT%d" % i, [128, 8, 128], BF16) for i in range(2)]
        lrT = sb("lrT", [16, 128])
        la = sb("la", [128, 128])
        kd = sb("kd", [128, 128])
        dec = sb("dec", [128, 2])
        kdec = sb("kdec", [128, 128], BF16)
        vbf = sb("vbf", [128, 256], BF16)
        er = sb("er", [128, 256])
        junk = sb("junk", [128, 256])
        ss = sb("ss", [128, 1])
        yo = [sb("yo%d" % i, [128, 256]) for i in range(2)]
        V, A, P, G = nc.vector, nc.scalar, nc.tensor, nc.gpsimd

        S.dma("sp", ident[:], id_d[:, :], w=["ident"], sem="ident")
        S.dma("sp", trirev[:], tr_d[:, :], w=["trirev"], sem="trirev")
        S.dma("sp", cind[:], ci_d[:, :], w=["cind"], sem="cind")
        S.dma("sp", ones1[:], on_d[:, :], w=["ones1"], sem="ones1")
        S.dma("pool", wcat[:], wcat_d.rearrange("(kc p) f -> p kc f", p=128), w=["wcat"], sem="wcat")
        S.dma("sp", wg2[:], wg2_d[:, :], w=["wg2"], sem="wg2")
        S.dma("sp", bg[:], bg_d[:, :], w=["bg"], sem="bg")
        S.dma("sp", gn[:], gn_d[0:1, :].partition_broadcast(128), w=["gn"], sem="gn")
        S.op("dve", lambda: V.memset(state[:], 0.0), w=["state"])
        S.op("dve", lambda: V.memset(qlo[:], 0.0), w=["qlo"])
        S.op("dve", lambda: V.memset(qhi[:], 0.0), w=["qhi"])

        for t in range(NT):
            tok0 = t * 128
            sl = t % 2
            S.dma("sp", ht[sl][:], h_d[hmap(tok0):hmap(tok0) + 128, :], w=[("ht", sl)], sem=("ht", sl))
            for hb in range(2):
                for j in range(4):
                    kc = hb * 4 + j
                    S.op("pe", lambda: P.transpose(out=ps[hb][:, j * 128:(j + 1) * 128],
                                                   in_=ht[sl][:, kc * 128:(kc + 1) * 128], identity=ident[:]),
                         r=[("ht", sl), "ident"], w=[("ps", hb)])
            S.op("act", lambda: A.copy(out=hT[sl][:, 0:4, :], in_=ps[0][:].rearrange("p (a b) -> p a b", a=4)),
                 r=[("ps", 0)], w=[("hT", sl, 0)])
            S.op("dve", lambda: V.tensor_copy(out=hT[sl][:, 4:8, :], in_=ps[1][:].rearrange("p (a b) -> p a b", a=4)),
                 r=[("ps", 1)], w=[("hT", sl, 1)])
            for kc in range(8):
                S.op("pe", lambda: P.matmul(ps[2][:, 0:128], lhsT=wcat[:, kc, 0:128], rhs=hT[sl][:, kc, :],
                                            start=(kc == 0), stop=(kc == 7)),
                     r=["wcat", ("hT", sl, kc // 4)], w=[("ps", 2)])
            for kc in range(8):
                S.op("pe", lambda: P.matmul(ps[2][0:16, 128:256], lhsT=wcat[:, kc, 768:784], rhs=hT[sl][:, kc, :],
                                            start=(kc == 0), stop=(kc == 7)),
                     r=["wcat", ("hT", sl, kc // 4)], w=[("ps", 2)])
            for kc in range(8):
                S.op("pe", lambda: P.matmul(ps[3][:, 0:384], lhsT=hT[sl][:, kc, :], rhs=wcat[:, kc, 128:512],
                                            start=(kc == 0), stop=(kc == 7)),
                     r=["wcat", ("hT", sl, kc // 4)], w=[("ps", 3)])
            for kc in range(8):
                S.op("pe", lambda: P.matmul(ps[4][:, 0:256], lhsT=hT[sl][:, kc, :], rhs=wcat[:, kc, 512:768],
                                            start=(kc == 0), stop=(kc == 7)),
                     r=["wcat", ("hT", sl, kc // 4)], w=[("ps", 4)])
            S.op("act", lambda: A.mul(out=qlo[:, 0:64], in_=ps[2][:, 0:64], mul=128 ** -0.5), r=[("ps", 2)], w=["qlo"])
            S.op("act", lambda: A.mul(out=qhi[:, 64:128], in_=ps[2][:, 64:128], mul=128 ** -0.5), r=[("ps", 2)], w=["qhi"])
            S.op("dve", lambda: V.tensor_copy(out=lrT[:], in_=ps[2][0:16, 128:256]), r=[("ps", 2)], w=["lrT"])
            S.op("pe", lambda: P.matmul(ps[2][:, 0:128], lhsT=lrT[:], rhs=wg2[:], start=True, stop=False),
                 r=["lrT", "wg2"], w=[("ps", 2)])
            S.op("pe", lambda: P.matmul(ps[2][:, 0:128], lhsT=ones1[:], rhs=bg[:], start=False, stop=True),
                 r=["ones1", "bg"], w=[("ps", 2)])
            S.op("act", lambda: A.activation(out=la[:], in_=ps[2][:, 0:128], func=AF.Exp, scale=-1.0), r=[("ps", 2)], w=["la"])
            S.op("dve", lambda: V.tensor_scalar_add(out=la[:], in0=la[:], scalar1=1.0), r=["la"], w=["la"])
            S.op("act", lambda: A.activation(out=la[:], in_=la[:], func=AF.Ln), r=["la"], w=["la"])
            S.op("pe", lambda: P.matmul(ps[2][:, 128:256], lhsT=trirev[:], rhs=la[:], start=True, stop=True),
                 r=["trirev", "la"], w=[("ps", 2)])
            S.op("pe", lambda: P.matmul(ps[2][:, 256:258], lhsT=la[:], rhs=cind[:], start=True, stop=True),
                 r=["cind", "la"], w=[("ps", 2)])
            S.op("act", lambda: A.activation(out=kd[:], in_=ps[2][:, 128:256], func=AF.Exp), r=[("ps", 2)], w=["kd"])
            S.op("act", lambda: A.activation(out=dec[:], in_=ps[2][:, 256:258], func=AF.Exp), r=[("ps", 2)], w=["dec"])
            S.op("dve", lambda: V.tensor_tensor(out=kdec[:], in0=ps[3][:, 0:128], in1=kd[:], op=ALU.mult),
                 r=[("ps", 3), "kd"], w=["kdec"])
            S.op("act", lambda: A.copy(out=vbf[:], in_=ps[3][:, 128:384]), r=[("ps", 3)], w=["vbf"])
            for c in range(2):
                pb = 5 + c
                S.op("pe", lambda: P.matmul(ps[pb][:, 0:256], lhsT=kdec[c * 64:(c + 1) * 64, :], rhs=vbf[c * 64:(c + 1) * 64, :],
                                            start=True, stop=True),
                     r=["kdec", "vbf"], w=[("ps", pb)])
            for c in range(2):
                pb = 5 + c
                S.op("dve", lambda: V.scalar_tensor_tensor(out=state[:], in0=state[:], scalar=dec[:, c:c + 1], in1=ps[pb][:, 0:256],
                                                           op0=ALU.mult, op1=ALU.add),
                     r=["dec", ("ps", pb), "state"], w=["state"])
                S.op("pe", lambda: P.matmul(ps[7][:, 0:256], lhsT=(qlo if c == 0 else qhi)[:], rhs=state[:],
                                            start=(c == 0), stop=(c == 1)),
                     r=["state", "qlo" if c == 0 else "qhi"], w=[("ps", 7)])
            S.op("act", lambda: A.activation(out=er[:], in_=ps[4][:, 0:256], func=AF.Exp, scale=-1.0), r=[("ps", 4)], w=["er"])
            S.op("dve", lambda: V.tensor_scalar_add(out=er[:], in0=er[:], scalar1=1.0), r=["er"], w=["er"])
            S.op("dve", lambda: V.reciprocal(out=er[:], in_=er[:]), r=["er"], w=["er"])
            S.op("dve", lambda: V.tensor_tensor(out=er[:], in0=ps[4][:, 0:256], in1=er[:], op=ALU.mult), r=[("ps", 4), "er"], w=["er"])
            S.op("dve", lambda: V.tensor_tensor(out=er[:], in0=er[:], in1=gn[:], op=ALU.mult), r=["er", "gn"], w=["er"])
            S.op("act", lambda: A.activation(out=junk[:], in_=ps[7][:, 0:256], func=AF.Square, accum_out=ss[:]),
                 r=[("ps", 7)], w=["junk", "ss"])
            S.op("dve", lambda: V.tensor_scalar(out=ss[:], in0=ss[:], scalar1=1.0 / 256, scalar2=LN_EPS, op0=ALU.mult, op1=ALU.add),
                 r=["ss"], w=["ss"])
            S.op("act", lambda: A.activation(out=ss[:], in_=ss[:], func=AF.Ln), r=["ss"], w=["ss"])
            S.op("act", lambda: A.activation(out=ss[:], in_=ss[:], func=AF.Exp, scale=-0.5), r=["ss"], w=["ss"])
            S.op("dve", lambda: V.scalar_tensor_tensor(out=yo[sl][:], in0=ps[7][:, 0:256], scalar=ss[:], in1=er[:],
                                                       op0=ALU.mult, op1=ALU.mult),
                 r=[("ps", 7), "ss", "er"], w=[("yo", sl)])
            S.dma("sp", y_d[tok0:tok0 + 128, :], yo[sl][:], r=[("yo", sl)], sem=("yo", sl))
            if (t + 1) % 8 == 0 and chunk_done:
                chunk_done(t // 8)


def emit_att(nc, S, ps, DD, tag, S_LEN=16384):
    D = 1024
    NT = S_LEN // 128
    NG = S_LEN // 512
    h_d, hkv_d, wq_d, wk_d, wv_d = DD["h"], DD["hkv"], DD["wq"], DD["wk"], DD["wv"]
    bt_d, cfar_d, lam_d, gsub_d, cst_d = DD["biasT"], DD["cfar"], DD["lamv"], DD["gsub"], DD["cst"]
    id_d, on_d, y_d = DD["ident"], DD["ones128"], DD["yout"]
    hmap = DD.get("hmap", lambda n: n)
    chunk_done = DD.get("chunk_done")
    with ExitStack() as es:
        def sb(name, shape, dt=F32):
            return es.enter_context(nc.sbuf_tensor("sb_" + tag + "_" + name, shape, dt))
        ident = sb("ident", [128, 128])
        wq = sb("wq", [128, 8, 256], BF16)
        wk = sb("wk", [128, 8, 256], BF16)
        wv = sb("wv", [128, 8, 256], BF16)
        bt = sb("bt", [128, 4, 128])
        cfar = sb("cfar", [128, 2])
        lamv = sb("lamv", [128, 4, 64])
        lam = sb("lam", [128, 4])
        cst = sb("cst", [128, 2])
        KT = [sb("KT%d" % i, [128, S_LEN], BF16) for i in range(2)]
        VV = [sb("V%d" % i, [128, NT, 128], BF16) for i in range(2)]
        QT = [[sb("QT%d_%d" % (i, s), [128, 512], BF16) for s in range(2)] for i in range(2)]
        ht = [sb("ht%d" % i, [128, D]) for i in range(2)]
        hT = sb("hT", [128, 8, 512], BF16)
        NPT = 6
        PT = [sb("PT%d" % i, [128, 512], BF16) for i in range(NPT)]
        tmp = [sb("tmp%d" % i, [128, 128]) for i in range(2)]
        ones = sb("ones", [128, 128])
        gcol = sb("gcol", [128, 1])
        Pacc = [[sb("Pacc%d_%d" % (m, i), [128, 512]) for i in range(2)] for m in range(2)]
        rinv = [sb("rinv%d" % m, [128, 512]) for m in range(2)]
        oT = sb("oT", [128, 512])
        o2 = sb("o2", [128, 512])
        yT = [sb("yT%d" % i, [128, 512]) for i in range(2)]
        yo = [sb("yo%d" % i, [128, 4, 128]) for i in range(2)]
        V, A, P, G = nc.vector, nc.scalar, nc.tensor, nc.gpsimd

        S.dma("sp", ident[:], id_d[:, :], w=["ident"], sem="ident")
        S.dma("pool", wq[:], wq_d.rearrange("(kc p) f -> p kc f", p=128), w=["wq"], sem="wq")
        S.dma("pool", wk[:], wk_d.rearrange("(kc p) f -> p kc f", p=128), w=["wk"], sem="wk")
        S.dma("pool", wv[:], wv_d.rearrange("(kc p) f -> p kc f", p=128), w=["wv"], sem="wv")
        S.dma("sp", bt[:], bt_d.rearrange("h t k q -> k (h t) q"), w=["bt"], sem="bt")
        S.dma("sp", cfar[:], cfar_d[:, :], w=["cfar"], sem="cfar")
        S.dma("sp", cst[:], cst_d[:, :], w=["cst"], sem="cst")
        S.dma("sp", lamv[:], lam_d.partition_broadcast(128), w=["lamv"], sem="lamv")
        S.dma("sp", ones[:], on_d[:, :], w=["ones"], sem="ones")
        S.dma("sp", gcol[:], gsub_d.rearrange("o d -> d o"), w=["gcol"], sem="gcol")
        L = ["lam"]
        S.op("dve", lambda: V.tensor_tensor(out=lamv[:, 0, :], in0=lamv[:, 0, :], in1=lamv[:, 1, :], op=ALU.mult), r=["lamv"], w=["lamv"])
        S.op("dve", lambda: V.tensor_tensor(out=lamv[:, 2, :], in0=lamv[:, 2, :], in1=lamv[:, 3, :], op=ALU.mult), r=["lamv"], w=["lamv"])
        S.op("dve", lambda: V.tensor_reduce(out=lam[:, 0:1], in_=lamv[:, 0, :], axis=AX.X, op=ALU.add), r=["lamv"], w=L)
        S.op("dve", lambda: V.tensor_reduce(out=lam[:, 1:2], in_=lamv[:, 2, :], axis=AX.X, op=ALU.add), r=["lamv"], w=L)
        S.op("act", lambda: A.activation(out=lam[:, 0:2], in_=lam[:, 0:2], func=AF.Exp), r=L, w=L)
        S.op("dve", lambda: V.tensor_tensor(out=lam[:, 2:3], in0=lam[:, 1:2], in1=lam[:, 0:1], op=ALU.subtract), r=L, w=L)
        S.op("dve", lambda: V.tensor_tensor(out=lam[:, 3:4], in0=lam[:, 2:3], in1=cst[:, 0:1], op=ALU.subtract), r=L + ["cst"], w=L)
        S.op("dve", lambda: V.tensor_tensor(out=gcol[:], in0=gcol[:], in1=cst[:, 1:2], op=ALU.mult), r=["gcol", "cst"], w=["gcol"])

        def load_hT(src_d, g):
            for tt in range(4):
                tok0 = g * 512 + tt * 128
                sl = tt % 2
                S.dma("sp", ht[sl][:], src_d[hmap(tok0):hmap(tok0) + 128, :], w=[("ht", sl)], sem=("ht", sl))
                for hb in range(2):
                    for j in range(4):
                        kc = hb * 4 + j
                        S.op("pe", lambda: P.transpose(out=ps[6 + hb][:, j * 128:(j + 1) * 128],
                                                       in_=ht[sl][:, kc * 128:(kc + 1) * 128], identity=ident[:]),
                             r=[("ht", sl), "ident"], w=[("ps", 6 + hb)])
                S.op("act", lambda: A.copy(out=hT[:, 0:4, tt * 128:(tt + 1) * 128], in_=ps[6][:].rearrange("p (a b) -> p a b", a=4)),
                     r=[("ps", 6)], w=[("hT", tt)])
                S.op("dve", lambda: V.tensor_copy(out=hT[:, 4:8, tt * 128:(tt + 1) * 128], in_=ps[7][:].rearrange("p (a b) -> p a b", a=4)),
                     r=[("ps", 7)], w=[("hT", tt)])
        HTK = [("hT", i) for i in range(4)]

        for g in range(NG):
            load_hT(hkv_d, g)
            for i in range(2):
                for kc in range(8):
                    S.op("pe", lambda: P.matmul(ps[6][:], lhsT=wk[:, kc, i * 128:(i + 1) * 128], rhs=hT[:, kc, :],
                                                start=(kc == 0), stop=(kc == 7)), r=["wk"] + HTK, w=[("ps", 6)])
                S.op("act" if i == 0 else "dve",
                     (lambda: A.copy(out=KT[i][:, g * 512:(g + 1) * 512], in_=ps[6][:])) if i == 0 else
                     (lambda: V.tensor_copy(out=KT[i][:, g * 512:(g + 1) * 512], in_=ps[6][:])),
                     r=[("ps", 6)], w=[("KT", i)])
            for tt in range(4):
                for kc in range(8):
                    S.op("pe", lambda: P.matmul(ps[7][:, 0:256], lhsT=hT[:, kc, tt * 128:(tt + 1) * 128], rhs=wv[:, kc, :],
                                                start=(kc == 0), stop=(kc == 7)), r=["wv"] + HTK, w=[("ps", 7)])
                S.op("act", lambda: A.copy(out=VV[0][:, g * 4 + tt, 0:128], in_=ps[7][:, 0:128]), r=[("ps", 7)], w=[("V", 0)])
                S.op("dve", lambda: V.tensor_copy(out=VV[1][:, g * 4 + tt, 0:128], in_=ps[7][:, 128:256]), r=[("ps", 7)], w=[("V", 1)])

        state = {"pt": 0, "sb": 0}
        SBANK = [(0, 1), (4, 5)]
        RS = 7
        pinit = {}

        def emit_S(it):
            (g, hd, j, qs) = it
            c0 = max(0, j - 4 * g)
            banks = SBANK[state["sb"] % 2]
            state["sb"] += 1
            ptis = (state["pt"] % NPT, (state["pt"] + 1) % NPT)
            state["pt"] += 2
            for mp in range(2):
                lo = mp * 64
                S.op("pe", lambda: P.matmul(ps[banks[mp]][:, c0 * 128:512], lhsT=KT[hd][lo:lo + 64, j * 128:(j + 1) * 128],
                                            rhs=QT[hd][qs][lo:lo + 64, c0 * 128:512], start=True, stop=True),
                     r=[("KT", hd), ("QT", hd, qs)], w=[("ps", banks[mp])])
            for mp in range(2):
                sbk = banks[mp]
                pti = ptis[mp]
                if j < 4 * g - 1:
                    S.op("act", lambda: A.activation(out=PT[pti][:], in_=ps[sbk][:], func=AF.Exp, scale=0.125, bias=cfar[:, hd:hd + 1]),
                         r=[("ps", sbk), "cfar"], w=[("PT", pti)])
                else:
                    for c in range(c0, 4):
                        i = 4 * g + c
                        cs = slice(c * 128, (c + 1) * 128)
                        if j < i - 1:
                            S.op("act", lambda: A.activation(out=PT[pti][:, cs], in_=ps[sbk][:, cs], func=AF.Exp, scale=0.125,
                                                             bias=cfar[:, hd:hd + 1]),
                                 r=[("ps", sbk), "cfar"], w=[("PT", pti)])
                        else:
                            ty = 0 if j == i else 1
                            tb = (c + mp) % 2
                            S.op("dve", lambda: V.scalar_tensor_tensor(out=tmp[tb][:], in0=ps[sbk][:, cs], scalar=0.125,
                                                                       in1=bt[:, hd * 2 + ty, :], op0=ALU.mult, op1=ALU.add),
                                 r=[("ps", sbk), "bt"], w=[("tmp", tb)])
                            S.op("act", lambda: A.activation(out=PT[pti][:, cs], in_=tmp[tb][:], func=AF.Exp),
                                 r=[("tmp", tb)], w=[("PT", pti)])
            return (c0, ptis)

        def emit_PV(it, info):
            (g, hd, j, qs) = it
            (c0, ptis) = info
            cs = slice(c0 * 128, 512)
            for mp in range(2):
                pti = ptis[mp]
                S.op("pe", lambda: P.matmul(ps[2 + mp][:, cs], lhsT=VV[hd][:, j, :], rhs=PT[pti][:, cs],
                                            start=(j == 0), stop=(j == 4 * g + 3), skip_group_check=True),
                     r=[("PT", pti), ("V", hd)], w=[("ps", 2 + mp)])
            for mp in range(2):
                pti = ptis[mp]
                a = 1 if (2 * j + mp) % 3 == 0 else 0
                eng, E = ("dve", V) if a == 0 else ("pool", G)
                key = (g, hd, mp, a)
                if key not in pinit:
                    pinit[key] = True
                    if c0 > 0:
                        S.op(eng, lambda: E.memset(Pacc[mp][a][:, 0:c0 * 128], 0.0), w=[("Pacc", mp, a)])
                    S.op(eng, lambda: E.tensor_copy(out=Pacc[mp][a][:, cs], in_=PT[pti][:, cs]), r=[("PT", pti)], w=[("Pacc", mp, a)])
                else:
                    S.op(eng, lambda: E.tensor_tensor(out=Pacc[mp][a][:, cs], in0=Pacc[mp][a][:, cs], in1=PT[pti][:, cs], op=ALU.add),
                         r=[("PT", pti), ("Pacc", mp, a)], w=[("Pacc", mp, a)])
            if j == 4 * g + 3:
                for mp in range(2):
                    accs = [a2 for a2 in range(2) if (g, hd, mp, a2) in pinit]
                    for n2, a2 in enumerate(accs):
                        S.op("pe", lambda: P.matmul(ps[RS][:], lhsT=ones[:], rhs=Pacc[mp][a2][:], start=(n2 == 0), stop=(n2 == len(accs) - 1)),
                             r=["ones", ("Pacc", mp, a2)], w=[("ps", RS)])
                    S.op("dve", lambda: V.reciprocal(out=rinv[mp][:], in_=ps[RS][:]), r=[("ps", RS)], w=[("rinv", mp)])

        def epilogue(g, hd, ys):
            S.op("dve", lambda: V.tensor_tensor(out=oT[:], in0=ps[2][:], in1=rinv[0][:], op=ALU.mult), r=[("ps", 2), ("rinv", 0)], w=["oT"])
            S.op("dve", lambda: V.tensor_tensor(out=o2[:], in0=ps[3][:], in1=rinv[1][:], op=ALU.mult), r=[("ps", 3), ("rinv", 1)], w=["o2"])
            S.op("dve", lambda: V.scalar_tensor_tensor(out=oT[:], in0=o2[:], scalar=lam[:, 3:4], in1=oT[:], op0=ALU.mult, op1=ALU.add),
                 r=["o2", "oT", "lam"], w=["oT"])
            S.op("act", lambda: A.activation(out=o2[:], in_=oT[:], func=AF.Square), r=["oT"], w=["o2"])
            S.op("pe", lambda: P.matmul(ps[RS][:], lhsT=ones[:], rhs=o2[:], start=True, stop=True), r=["ones", "o2"], w=[("ps", RS)])
            S.op("dve", lambda: V.tensor_scalar(out=o2[:], in0=ps[RS][:], scalar1=1.0 / 128, scalar2=LN_EPS, op0=ALU.mult, op1=ALU.add),
                 r=[("ps", RS)], w=["o2"])
            S.op("act", lambda: A.activation(out=o2[:], in_=o2[:], func=AF.Ln), r=["o2"], w=["o2"])
            S.op("act", lambda: A.activation(out=o2[:], in_=o2[:], func=AF.Exp, scale=-0.5), r=["o2"], w=["o2"])
            S.op("dve", lambda: V.scalar_tensor_tensor(out=yT[hd][:], in0=oT[:], scalar=gcol[:, 0:1], in1=o2[:], op0=ALU.mult, op1=ALU.mult),
                 r=["oT", "o2", "gcol"], w=[("yT", hd)])
            for c in range(4):
                S.op("pe", lambda: P.transpose(out=ps[6][:, c * 128:(c + 1) * 128], in_=yT[hd][:, c * 128:(c + 1) * 128], identity=ident[:]),
                     r=[("yT", hd), "ident"], w=[("ps", 6)])
            S.op("act", lambda: A.copy(out=yo[hd][:], in_=ps[6][:].rearrange("p (c f) -> p c f", c=4)), r=[("ps", 6)], w=[("yo", hd)])
            S.dma("sp", y_d[g * 512:(g + 1) * 512, hd * 128:(hd + 1) * 128].rearrange("(c p) f -> p c f", p=128), yo[hd][:],
                  r=[("yo", hd)], sem=("yo", hd))

        for g in range(NG):
            qs = g % 2
            ys = g % 2
            load_hT(h_d, g)
            for hd in range(2):
                for kc in range(8):
                    S.op("pe", lambda: P.matmul(ps[6][:], lhsT=wq[:, kc, hd * 128:(hd + 1) * 128], rhs=hT[:, kc, :],
                                                start=(kc == 0), stop=(kc == 7)), r=["wq"] + HTK, w=[("ps", 6)])
                S.op("dve", lambda: V.tensor_copy(out=QT[hd][qs][:], in_=ps[6][:]), r=[("ps", 6)], w=[("QT", hd, qs)])
            if g >= 2 and g % 2 == 0 and chunk_done:
                chunk_done(g // 2 - 1)
            for hd in range(2):
                items = [(g, hd, j, qs) for j in range(4 * g + 4)]
                info = emit_S(items[0])
                for n in range(len(items)):
                    nxt = emit_S(items[n + 1]) if n + 1 < len(items) else None
                    emit_PV(items[n], info)
                    info = nxt
                epilogue(g, hd, ys)
        if chunk_done:
            chunk_done(NG // 2 - 1)


def build_fused():
    nc = bass.Bass("TRN2", target_bir_lowering=False)
    SL, D = 16384, 1024
    ext = lambda name, shape: nc.dram_tensor(name, shape, F32, kind="ExternalInput").ap()
    loc = lambda name, shape: nc.dram_tensor(name, shape, F32).ap()
    xb = ext("xb", [SL, D])
    xs = ext("xs", [4096, D])
    idx_d = nc.dram_tensor("idx", [128, 128], mybir.dt.uint32, kind="ExternalInput").ap()
    g_wcat, g_wg2, g_bg, g_gn = ext("g_wcat", [2, D, 784]), ext("g_wg2", [2, 16, 128]), ext("g_bg", [2, 1, 128]), ext("g_gn", [2, 1, 256])
    a_wq, a_wk, a_wv = ext("a_wq", [2, D, 256]), ext("a_wk", [D, 256]), ext("a_wv", [D, 256])
    a_bt, a_cfar, a_lamv = ext("a_biasT", [2, 2, 128, 128]), ext("a_cfar", [128, 2]), ext("a_lamv", [2, 4, 64])
    a_gsub, a_cst = ext("a_gsub", [2, 1, 128]), ext("a_cst", [2, 128, 2])
    p_wout, p_lnp, p_wr, p_rb = ext("p_wout", [4, D, D]), ext("p_lnp", [4, 4, D]), ext("p_wr", [4, D, 20]), ext("p_rb", [4, 1, 20])
    p_wg, p_wu, p_wd = ext("p_wg", [4, 16, D, 512]), ext("p_wu", [4, 16, D, 512]), ext("p_wd", [4, 16, 512, D])
    ident, trirev, cind = ext("ident", [128, 128]), ext("trirev", [128, 128]), ext("cind", [128, 2])
    ones1, ones128 = ext("ones1", [1, 128]), ext("ones128", [128, 128])
    out = nc.dram_tensor("out", [4096, D], F32, kind="ExternalOutput").ap()
    yloc, yg = loc("yloc", [SL, 256]), loc("yg", [4 * SL, 256])
    hloc = [loc("hloc%d" % i, [4096, D]) for i in range(3)]
    hg0, hkvg, hg2 = loc("hg0", [SL, D]), loc("hkvg", [SL, D]), loc("hg2", [SL, D])
    GROUPS = [[0, 1, 2, 3], [4, 5, 6, 7]]

    def hperm(n):
        r_, w_ = n // 4096, n % 4096
        return (w_ // 256) * 1024 + r_ * 256 + (w_ % 256)

    def gather_y(S):
        S.coll_multi("AllGather", GROUPS, [(yloc[i * 1024:(i + 1) * 1024, :], yg[i * 4096:(i + 1) * 4096, :]) for i in range(16)])

    def gather_h(S, src, dst):
        S.coll_multi("AllGather", GROUPS, [(src[i * 256:(i + 1) * 256, :], dst[i * 1024:(i + 1) * 1024, :]) for i in range(16)])
    with ExitStack() as es:
        S = Sched(nc, es)
        ps = [es.enter_context(nc.psum_tensor("ps%d" % i, [128, 512], F32)) for i in range(8)]

        def ydone(i):
            S.coll_async("AllGather", GROUPS, yloc[i * 1024:(i + 1) * 1024, :], yg[i * 4096:(i + 1) * 4096, :], [("yo", 0), ("yo", 1)])

        def post(layer, ysrc, hp, dst, gdst=None):
            hdone = None
            if gdst is not None:
                hdone = lambda i: S.coll_async("AllGather", GROUPS, dst[i * 256:(i + 1) * 256, :], gdst[i * 1024:(i + 1) * 1024, :],
                                               [("ot", 0), ("ot", 1)])
            emit_post(nc, S, ps, {"y": ysrc[:, :], "idx": idx_d[:, :], "hp": hp, "chunk_done": hdone, "wout": p_wout[layer], "lnp": p_lnp[layer], "wr": p_wr[layer], "rb": p_rb[layer],
                                  "wg": p_wg[layer], "wu": p_wu[layer], "wd": p_wd[layer], "ident": ident, "out": dst},
                      "p%d" % layer)

        def gla(layer, h):
            emit_gla(nc, S, ps, {"h": h, "chunk_done": ydone, "hmap": (hperm if layer > 0 else (lambda n: n)), "wcat": g_wcat[layer], "wg2": g_wg2[layer], "bg": g_bg[layer], "gn": g_gn[layer],
                                 "ident": ident, "trirev": trirev, "cind": cind, "ones1": ones1, "yout": yloc}, "g%d" % layer)

        def att(j, h):
            emit_att(nc, S, ps, {"h": h, "hkv": hkvg, "chunk_done": ydone, "hmap": hperm, "wq": a_wq[j], "wk": a_wk, "wv": a_wv, "biasT": a_bt, "cfar": a_cfar,
                                 "lamv": a_lamv[j], "gsub": a_gsub[j], "cst": a_cst[j], "ident": ident, "ones128": ones128,
                                 "yout": yloc}, "a%d" % j)

        import os
        LEVEL = int(os.environ.get("FUSED_LEVEL", "99"))
        steps = [
            lambda: gla(0, xb), lambda: S.barrier(), lambda: post(0, yg, xs, hloc[0], hg0), lambda: S.barrier(),
            lambda: gla(1, hg0), lambda: S.barrier(), lambda: post(1, yg, hloc[0], hloc[1], hkvg), lambda: S.barrier(),
            lambda: att(0, hkvg), lambda: S.barrier(), lambda: post(2, yg, hloc[1], hloc[2], hg2), lambda: S.barrier(),
            lambda: att(1, hg2), lambda: S.barrier(), lambda: post(3, yg, hloc[2], out),
        ]
        for i_, st_ in enumerate(steps):
            if i_ >= LEVEL:
                break
            st_()
        S.barrier()
    return nc


_NC = {}


def kernel(x, a_w_in, a_w_gate2, a_b_gate, a_g_norm, a_w_out, kv_w, b_w_q, b_lam_q1, b_lam_k1,
           b_lam_q2, b_lam_k2, b_g_sub, b_w_out, rel_table, moe_w_group, moe_b_group, moe_w_router,
           moe_b_router, moe_w_gate, moe_w_up, moe_w_down, ln_g, ln_b):
    f32 = np.float32
    A = lambda a: np.ascontiguousarray(np.asarray(a, dtype=f32))
    x = A(x)
    B, S_, D = x.shape
    if "nc" not in _NC:
        _NC["nc"] = build_fused()
    nc = _NC["nc"]
    w_in = A(a_w_in)
    kvw = A(kv_w)
    wqf = A(b_w_q)
    rt = A(rel_table)
    gconst = gla_consts()
    p_wout = A(np.stack([a_w_out[0], a_w_out[1], b_w_out[0], b_w_out[1]]))
    p_lnp = A(np.stack([np.stack([ln_g[l, 0], ln_b[l, 0], ln_g[l, 1], ln_b[l, 1]]) for l in range(4)]))
    p_wr = A(np.concatenate([moe_w_group, moe_w_router], axis=2))
    p_rb = A(np.concatenate([np.asarray(moe_b_group).reshape(4, -1), np.asarray(moe_b_router).reshape(4, -1)], axis=1)[:, None, :])
    p_wg, p_wu, p_wd = A(moe_w_gate), A(moe_w_up), A(moe_w_down)
    linits = [0.8 - 0.6 * math.exp(-0.3 * layer) for layer in (2, 3)]
    a_cst = A(np.stack([np.broadcast_to(np.array([[li, 1.0 - li]], f32), (128, 2)) for li in linits]))
    a_lamv = A(np.stack([np.stack([b_lam_q1[j], b_lam_k1[j], b_lam_q2[j], b_lam_k2[j]]) for j in range(2)]))
    a_gsub = A(np.asarray(b_g_sub)[:, None, :])
    in_maps = []
    for c in range(8):
        b, r = c // 4, c % 4
        hd = r
        g_wcat = np.stack([np.concatenate([w_in[l][:, hd * 128:(hd + 1) * 128], w_in[l][:, 512 + hd * 128:512 + (hd + 1) * 128],
                                           w_in[l][:, 1024 + hd * 256:1024 + (hd + 1) * 256], w_in[l][:, 2048 + hd * 256:2048 + (hd + 1) * 256],
                                           w_in[l][:, 3072:3088]], axis=1) for l in range(2)])
        heads = [2 * r, 2 * r + 1]
        a_wq = np.stack([np.concatenate([wqf[j][:, hh * 64:(hh + 1) * 64] if m_ == 0 else wqf[j][:, 512 + hh * 64:512 + (hh + 1) * 64]
                                         for hh in heads for m_ in range(2)], axis=1) for j in range(2)])
        a_wk = np.concatenate([kvw[:, hh * 64:(hh + 1) * 64] if m_ == 0 else kvw[:, 512 + hh * 64:512 + (hh + 1) * 64]
                               for hh in heads for m_ in range(2)], axis=1)
        a_wv = np.concatenate([kvw[:, 1024 + hh * 128:1024 + (hh + 1) * 128] for hh in heads], axis=1)
        idxv = np.zeros((128, 128), np.uint32)
        for r_ in range(4):
            for t_ in range(32):
                n_ = r * 4096 + t_ * 128
                idxv[:, r_ * 32 + t_] = (n_ // 1024) * 4096 + r_ * 1024 + (n_ % 1024) + np.arange(128)
        m = {"xb": x[b], "xs": np.ascontiguousarray(x[b, r * 4096:(r + 1) * 4096]), "idx": idxv, "g_wcat": A(g_wcat), "g_wg2": A(np.asarray(a_w_gate2)[:, :, hd * 128:(hd + 1) * 128]),
             "g_bg": A(np.asarray(a_b_gate)[:, None, hd * 128:(hd + 1) * 128]), "g_gn": A(np.asarray(a_g_norm)[:, None, hd * 256:(hd + 1) * 256]),
             "a_wq": A(a_wq), "a_wk": A(a_wk), "a_wv": A(a_wv), "a_biasT": att_bias_tiles(rt, heads),
             "a_cfar": A(np.broadcast_to(rt[15, heads][None, :], (128, 2))), "a_lamv": a_lamv, "a_gsub": a_gsub, "a_cst": a_cst,
             "p_wout": p_wout, "p_lnp": p_lnp, "p_wr": p_wr, "p_rb": p_rb, "p_wg": p_wg, "p_wu": p_wu, "p_wd": p_wd,
             "ident": gconst["ident"], "trirev": gconst["trirev"], "cind": gconst["cind"], "ones1": gconst["ones1"],
             "ones128": np.ones((128, 128), f32)}
        in_maps.append(m)
    res = run_bass_kernel_spmd(nc, in_maps, core_ids=list(range(8)))
    return np.concatenate([res.results[c]["out"] for c in range(8)], axis=0).reshape(B, S_, D)
```

```python
from contextlib import ExitStack
import math
import numpy as np
import concourse.bass as bass
import concourse.mybir as mybir
from concourse.bass_utils import run_bass_kernel_spmd

F32 = mybir.dt.float32
BF16 = mybir.dt.bfloat16
AF = mybir.ActivationFunctionType
ALU = mybir.AluOpType
AX = mybir.AxisListType


class Sched:
    ENG = ("pe", "act", "dve", "pool", "sp")

    def __init__(self, nc, es):
        self.nc = nc
        self.es = es
        self.eng = {"pe": nc.tensor, "act": nc.scalar, "dve": nc.vector, "pool": nc.gpsimd, "sp": nc.sync}
        self.sem = {e: es.enter_context(nc.semaphore("s_" + e)) for e in self.ENG}
        self.cnt = {e: 0 for e in self.ENG}
        self.seen = {e: {} for e in self.ENG}
        self.snaps = {e: [None] for e in self.ENG}
        self.dsem = {}
        self.dcnt = {}
        self.lastw = {}
        self.readers = {}
        self.nwait = 0
        self.ninst = 0

    def _deps(self, r, w):
        deps = []
        for k in r:
            t = self.lastw.get(k)
            if t is not None:
                deps.append(t)
        for k in w:
            t = self.lastw.get(k)
            if t is not None:
                deps.append(t)
            deps.extend(self.readers.get(k, ()))
        return deps

    def _wait(self, e, deps, skip_dma_sem=None):
        seen = self.seen[e]
        need = {}
        for (src, val) in deps:
            if src == e and e == "pe":
                continue
            if skip_dma_sem is not None and src == skip_dma_sem:
                continue
            if seen.get(src, 0) >= val:
                continue
            if need.get(src, 0) < val:
                need[src] = val
        if not need:
            return
        seen = dict(seen)
        for src, val in need.items():
            if isinstance(src, tuple):
                self.eng[e].wait_ge(self.dsem[src[1]], val)
            else:
                self.eng[e].wait_ge(self.sem[src], val)
                snap = self.snaps[src][val]
                if snap:
                    for s2, v2 in snap.items():
                        if seen.get(s2, 0) < v2:
                            seen[s2] = v2
            if seen.get(src, 0) < val:
                seen[src] = val
            self.nwait += 1
        self.seen[e] = seen

    def _commit(self, tok, r, w):
        for k in w:
            self.lastw[k] = tok
            self.readers[k] = []
        for k in r:
            self.readers.setdefault(k, []).append(tok)

    def op(self, e, fn, r=(), w=()):
        px = [k for k in r if isinstance(k, tuple) and k[0] == "ps"]
        if px:
            r = [k for k in r if k not in px]
            w = list(w) + px
        self._wait(e, self._deps(r, w))
        ins = fn()
        self.cnt[e] += 1
        ins.then_inc(self.sem[e], 1)
        self.snaps[e].append(self.seen[e])
        self._commit((e, self.cnt[e]), r, w)
        self.ninst += 1
        return ins

    def dma(self, q, out, in_, r=(), w=(), sem=None):
        assert sem is not None
        if sem not in self.dsem:
            self.dsem[sem] = self.es.enter_context(self.nc.semaphore("d_%d" % len(self.dsem)))
            self.dcnt[sem] = 0
        self._wait(q, self._deps(r, w), skip_dma_sem=("dma", sem))
        ins = self.eng[q].dma_start(out=out, in_=in_)
        self.dcnt[sem] += 16
        ins.then_inc(self.dsem[sem], 16)
        self._commit((("dma", sem), self.dcnt[sem]), r, w)
        self.ninst += 1
        return ins

    def finish(self, keys):
        deps = []
        for k in keys:
            t = self.lastw.get(k)
            if t is not None:
                deps.append(t)
            deps.extend(self.readers.get(k, ()))
        self._wait("sp", deps)


def _sched_barrier(self):
    for e in self.ENG:
        deps = [(f, self.cnt[f]) for f in self.ENG if self.cnt[f] > 0]
        deps += [(("dma", k), v) for k, v in self.dcnt.items() if v > 0]
        self._wait(e, deps)
    self.lastw = {}
    self.readers = {}


def _sched_coll(self, kind, groups, in_ap, out_ap):
    self.barrier()
    if "cc" not in self.dsem:
        self.dsem["cc"] = self.es.enter_context(self.nc.semaphore("d_cc"))
        self.dcnt["cc"] = 0
    ins = self.nc.gpsimd.collective_compute(kind, ALU.bypass, replica_groups=groups, ins=[in_ap], outs=[out_ap])
    self.dcnt["cc"] += 1
    ins.then_inc(self.dsem["cc"])
    self.ninst += 1
    self.barrier()


Sched.barrier = _sched_barrier
Sched.coll = _sched_coll


def _sched_dma_fn(self, q, fn, r=(), w=(), sem=None):
    if sem not in self.dsem:
        self.dsem[sem] = self.es.enter_context(self.nc.semaphore("d_%d" % len(self.dsem)))
        self.dcnt[sem] = 0
    self._wait(q, self._deps(r, w), skip_dma_sem=("dma", sem))
    ins = fn()
    self.dcnt[sem] += 16
    ins.then_inc(self.dsem[sem], 16)
    self._commit((("dma", sem), self.dcnt[sem]), r, w)
    self.ninst += 1
    return ins


Sched.dma_fn = _sched_dma_fn


def _sched_coll_multi(self, kind, groups, pairs):
    self.barrier()
    if "cc" not in self.dsem:
        self.dsem["cc"] = self.es.enter_context(self.nc.semaphore("d_cc"))
        self.dcnt["cc"] = 0
    for (in_ap, out_ap) in pairs:
        ins = self.nc.gpsimd.collective_compute(kind, ALU.bypass, replica_groups=groups, ins=[in_ap], outs=[out_ap])
        self.dcnt["cc"] += 1
        ins.then_inc(self.dsem["cc"])
        self.ninst += 1
        self.nc.gpsimd.wait_ge(self.dsem["cc"], self.dcnt["cc"])
    self.barrier()


Sched.coll_multi = _sched_coll_multi


def _sched_coll_async(self, kind, groups, in_ap, out_ap, wait_sems):
    if "cc" not in self.dsem:
        self.dsem["cc"] = self.es.enter_context(self.nc.semaphore("d_cc"))
        self.dcnt["cc"] = 0
    deps = [(("dma", k), self.dcnt[k]) for k in wait_sems if self.dcnt.get(k, 0) > 0]
    if self.dcnt["cc"] > 0:
        deps.append((("dma", "cc"), self.dcnt["cc"]))
    self._wait("pool", deps)
    ins = self.nc.gpsimd.collective_compute(kind, ALU.bypass, replica_groups=groups, ins=[in_ap], outs=[out_ap], dma_qos="P3")
    self.dcnt["cc"] += 1
    ins.then_inc(self.dsem["cc"])
    self.ninst += 1


Sched.coll_async = _sched_coll_async

ALPHA = (2.0 * 4) ** 0.25
LN_EPS = 1e-5
TAU = 16.0
NEGB = -30000.0

LN_EPS = 1e-5
TAU = 16.0


def gla_consts():
    s = np.arange(128)
    same = (s[:, None] // 64) == (s[None, :] // 64)
    trirev = ((s[:, None] > s[None, :]) & same).astype(np.float32) * (-1.0 / TAU)
    cind = np.zeros((128, 2), np.float32)
    cind[:64, 0] = -1.0 / TAU
    cind[64:, 1] = -1.0 / TAU
    return {"ident": np.eye(128, dtype=np.float32), "trirev": trirev, "cind": cind,
            "ones1": np.ones((1, 128), np.float32)}


LN_EPS = 1e-5
NEGB = -30000.0


def rel_bucket_np(rel):
    nb = 16
    max_exact = 8
    base = np.where(rel > 0, nb, 0)
    n = np.abs(rel)
    large = max_exact + (np.log(np.maximum(n, 1).astype(np.float32) / np.float32(max_exact))
                         / np.float32(math.log(128 / max_exact)) * np.float32(nb - max_exact)).astype(np.int32)
    large = np.minimum(large, nb - 1)
    return base + np.where(n < max_exact, n, large)


def att_bias_tiles(rel_table, heads):
    kl = np.arange(128)[:, None]
    ql = np.arange(128)[None, :]
    out = np.zeros((len(heads), 2, 128, 128), np.float32)
    bd = rel_bucket_np(kl - ql)
    bp = rel_bucket_np(kl - ql - 128)
    vis = (kl // 64) <= (ql // 64)
    for i, h in enumerate(heads):
        out[i, 0] = np.where(vis, rel_table[bd, h], np.float32(NEGB))
        out[i, 1] = rel_table[bp, h]
    return out


def emit_post(nc, S, ps, D, tag, NTOK=4096, SG=1024, NEXP=16, do_A=True, do_B=True, do_R=True, stage=9):
    D_ = 1024
    D, DD = D_, D
    FF = 512
    y_d, hp_d, wout_d, lnp_d, wr_d, rb_d = DD["y"], DD["hp"], DD["wout"], DD["lnp"], DD["wr"], DD["rb"]
    wg_d, wu_d, wd_d, id_d, out_d = DD["wg"], DD["wu"], DD["wd"], DD["ident"], DD["out"]
    chunk_done = DD.get("chunk_done")
    NSG = NTOK // SG
    TPS = SG // 128
    GPS = SG // 512
    with ExitStack() as es:
        def sb(name, shape, dt=F32):
            return es.enter_context(nc.sbuf_tensor("sb_" + tag + "_" + name, shape, dt))
        ident = sb("ident", [128, 128])
        idx = sb("idx", [128, 128], mybir.dt.uint32)
        wout = sb("wout", [128, 8, D], BF16)
        wr = sb("wr", [128, 8, 20])
        rb = sb("rb", [128, 20])
        lnp = sb("lnp", [128, 4, D])
        hT = sb("hT", [128, 8, SG], BF16)
        yacc = sb("yacc", [128, TPS, D])
        comb = sb("comb", [128, TPS, 16])
        wg = [sb("wg%d" % i, [128, 8, FF], BF16) for i in range(2)]
        wu = [sb("wu%d" % i, [128, 8, FF], BF16) for i in range(2)]
        wd = [sb("wd%d" % i, [128, 4, D], BF16) for i in range(2)]
        yt = [sb("yt%d" % i, [128, D]) for i in range(2)]
        hpt = [sb("hpt%d" % i, [128, D]) for i in range(2)]
        yT = [sb("yT%d" % i, [128, 8, 128], BF16) for i in range(2)]
        zt = [sb("z%d" % i, [128, D]) for i in range(2)]
        hT32 = [sb("hT32_%d" % i, [128, 8, 128]) for i in range(2)]
        sg = [sb("sg%d" % i, [128, 512]) for i in range(2)]
        hdn = [sb("hdn%d" % i, [128, 4, 512], BF16) for i in range(2)]
        ot = [sb("ot%d" % i, [128, D]) for i in range(2)]
        st = sb("stats", [128, 2, 6])
        mv = sb("mv", [128, 2])
        rstd = sb("rstd", [128, 1])
        lg = sb("lg", [128, 20])
        r_gmax = sb("r_gmax", [128, 1])
        r_gmask = sb("r_gmask", [128, 4])
        r_gt = sb("r_gt", [128, 4])
        r_gsum = sb("r_gsum", [128, 1])
        r_m1 = sb("r_m1", [128, 4])
        r_m2 = sb("r_m2", [128, 4])
        r_is1 = sb("r_is1", [128, 4, 4])
        r_is2 = sb("r_is2", [128, 4, 4])
        r_e2 = sb("r_e2", [128, 4, 4])
        r_w1 = sb("r_w1", [128, 4])
        r_w2 = sb("r_w2", [128, 4])
        r_gs = sb("r_gs", [128, 4])

        V, A, P, G = nc.vector, nc.scalar, nc.tensor, nc.gpsimd

        S.dma("sp", ident[:], id_d[:, :], w=["ident"], sem="ident")
        S.dma("sp", idx[:], DD["idx"], w=["idx"], sem="idx")
        S.dma("pool", wout[:], wout_d.rearrange("(kc p) f -> p kc f", p=128), w=["wout"], sem="wout")
        S.dma("sp", wr[:], wr_d.rearrange("(kc p) f -> p kc f", p=128), w=["wr"], sem="wr")
        S.dma("sp", rb[:], rb_d[0:1, :].partition_broadcast(128), w=["rb"], sem="rb")
        S.dma("sp", lnp[:], lnp_d.partition_broadcast(128), w=["lnp"], sem="lnp")

        def load_expert(e, slot):
            S.dma("pool", wg[slot][:], wg_d[e].rearrange("(kc p) f -> p kc f", p=128), w=[("wg", slot)], sem=("wg", slot))
            S.dma("pool", wu[slot][:], wu_d[e].rearrange("(kc p) f -> p kc f", p=128), w=[("wu", slot)], sem=("wu", slot))
            S.dma("pool", wd[slot][:], wd_d[e].rearrange("(kc p) f -> p kc f", p=128), w=[("wd", slot)], sem=("wd", slot))

        def layernorm(src, dst, gi, sl):
            for c in range(2):
                S.op("dve", lambda c=c: V.bn_stats(out=st[:, c, :], in_=src[:, c * 512:(c + 1) * 512]), r=[sl], w=["st"])
            S.op("dve", lambda: V.bn_aggr(out=mv[:], in_=st[:].rearrange("p a b -> p (a b)")), r=["st"], w=["mv"])
            S.op("dve", lambda: V.tensor_scalar_add(out=rstd[:], in0=mv[:, 1:2], scalar1=LN_EPS), r=["mv"], w=["rstd"])
            S.op("act", lambda: A.activation(out=rstd[:], in_=rstd[:], func=AF.Ln), r=["rstd"], w=["rstd"])
            S.op("act", lambda: A.activation(out=rstd[:], in_=rstd[:], func=AF.Exp, scale=-0.5), r=["rstd"], w=["rstd"])
            S.op("dve", lambda: V.tensor_scalar(out=dst, in0=src, scalar1=mv[:, 0:1], scalar2=rstd[:],
                                                op0=ALU.subtract, op1=ALU.mult), r=["mv", "rstd", sl], w=[sl])
            S.op("pool", lambda: G.tensor_tensor(out=dst, in0=dst, in1=lnp[:, gi, :], op=ALU.mult), r=["lnp", sl], w=[sl])
            S.op("pool", lambda: G.tensor_tensor(out=dst, in0=dst, in1=lnp[:, gi + 1, :], op=ALU.add), r=["lnp", sl], w=[sl])

        pending = []
        cur_e = [0]
        nload = 0
        for s in range(NSG):
            if do_B:
                load_expert(0, nload % 2)
            for t in range(TPS if do_A else 0):
                tok0 = s * SG + t * 128
                sl = t % 2
                tg = s * TPS + t
                for r_ in range(4):
                    S.dma_fn("pool", lambda: G.indirect_dma_start(out=yt[sl][:, r_ * 256:(r_ + 1) * 256], out_offset=None, in_=y_d,
                                                                  in_offset=bass.IndirectOffsetOnAxis(ap=idx[:, r_ * 32 + tg:r_ * 32 + tg + 1], axis=0)),
                             r=["idx"], w=[("yt", sl)], sem=("yt", sl))
                S.dma("sp", hpt[sl][:], hp_d[tok0:tok0 + 128, :], w=[("hp", sl)], sem=("hp", sl))
                for hb in range(2):
                    for j in range(4):
                        kc = hb * 4 + j
                        S.op("pe", lambda kc=kc, j=j, hb=hb: P.transpose(out=ps[hb][:, j * 128:(j + 1) * 128],
                                                                        in_=yt[sl][:, kc * 128:(kc + 1) * 128], identity=ident[:]),
                             r=[("yt", sl), "ident"], w=[("ps", hb)])
                S.op("act", lambda: A.copy(out=yT[sl][:, 0:4, :], in_=ps[0][:].rearrange("p (a b) -> p a b", a=4)),
                     r=[("ps", 0)], w=[("yT", sl, 0)])
                S.op("dve", lambda: V.tensor_copy(out=yT[sl][:, 4:8, :], in_=ps[1][:].rearrange("p (a b) -> p a b", a=4)),
                     r=[("ps", 1)], w=[("yT", sl, 1)])
                if stage < 2:
                    continue
                for half in range(2):
                    for kc in range(8):
                        S.op("pe", lambda kc=kc, half=half: P.matmul(ps[2 + half][:], lhsT=yT[sl][:, kc, :],
                                                                     rhs=wout[:, kc, half * 512:(half + 1) * 512],
                                                                     start=(kc == 0), stop=(kc == 7)),
                             r=[("yT", sl, kc // 4), "wout"], w=[("ps", 2 + half)])
                if stage < 3:
                    continue
                for half in range(2):
                    S.op("dve", lambda half=half: V.scalar_tensor_tensor(out=zt[sl][:, half * 512:(half + 1) * 512],
                                                                         in0=hpt[sl][:, half * 512:(half + 1) * 512], scalar=ALPHA,
                                                                         in1=ps[2 + half][:], op0=ALU.mult, op1=ALU.add),
                         r=[("hp", sl), ("ps", 2 + half)], w=[("z", sl)])
                layernorm(zt[sl][:], zt[sl][:], 0, ("z", sl))
                if stage < 4:
                    continue
                S.op("act", lambda: A.mul(out=yacc[:, t, :], in_=zt[sl][:], mul=ALPHA), r=[("z", sl)], w=[("yacc", t)])
                for hb in range(2):
                    for j in range(4):
                        kc = hb * 4 + j
                        S.op("pe", lambda kc=kc, j=j, hb=hb: P.transpose(out=ps[4 + hb][:, j * 128:(j + 1) * 128],
                                                                        in_=zt[sl][:, kc * 128:(kc + 1) * 128], identity=ident[:]),
                             r=[("z", sl), "ident"], w=[("ps", 4 + hb)])
                for hb in range(2):
                    S.op("act", lambda hb=hb: A.copy(out=hT32[sl][:, hb * 4:(hb + 1) * 4, :],
                                                     in_=ps[4 + hb][:].rearrange("p (a b) -> p a b", a=4)),
                         r=[("ps", 4 + hb)], w=[("hT32", sl, hb)])
                    S.op("dve", lambda hb=hb: V.tensor_copy(out=hT[:, hb * 4:(hb + 1) * 4, t * 128:(t + 1) * 128],
                                                            in_=ps[4 + hb][:].rearrange("p (a b) -> p a b", a=4)),
                         r=[("ps", 4 + hb)], w=[("hT", t)])
                if stage < 5:
                    continue
                for kc in range(8):
                    S.op("pe", lambda kc=kc: P.matmul(ps[6][:, 0:20], lhsT=hT32[sl][:, kc, :], rhs=wr[:, kc, :],
                                                      start=(kc == 0), stop=(kc == 7)),
                         r=[("hT32", sl, kc // 4), "wr"], w=[("ps", 6)])
                if not do_R:
                    continue
                RT = ["rt"]
                S.op("dve", lambda: V.tensor_tensor(out=lg[:], in0=ps[6][:, 0:20], in1=rb[:], op=ALU.add),
                     r=[("ps", 6), "rb"], w=RT)
                S.op("dve", lambda: V.tensor_reduce(out=r_gmax[:], in_=lg[:, 0:4], axis=AX.X, op=ALU.max), r=RT, w=RT)
                S.op("dve", lambda: V.tensor_scalar(out=r_gmask[:], in0=lg[:, 0:4], scalar1=r_gmax[:], scalar2=None,
                                                    op0=ALU.is_ge), r=RT, w=RT)
                S.op("dve", lambda: V.tensor_scalar(out=r_gt[:], in0=lg[:, 0:4], scalar1=r_gmax[:], scalar2=None,
                                                    op0=ALU.subtract), r=RT, w=RT)
                S.op("act", lambda: A.activation(out=r_gt[:], in_=r_gt[:], func=AF.Exp, accum_out=r_gsum[:]), r=RT, w=RT)
                S.op("dve", lambda: V.reciprocal(out=r_gsum[:], in_=r_gsum[:]), r=RT, w=RT)
                S.op("dve", lambda: V.tensor_scalar(out=r_gs[:], in0=r_gmask[:], scalar1=r_gsum[:], scalar2=None,
                                                    op0=ALU.mult), r=RT, w=RT)
                ev = lg[:, 4:20].rearrange("p (g j) -> p g j", g=4)
                S.op("dve", lambda: V.tensor_reduce(out=r_m1[:], in_=ev, axis=AX.X, op=ALU.max), r=RT, w=RT)
                S.op("dve", lambda: V.tensor_tensor(out=r_is1[:], in0=ev, in1=r_m1[:].unsqueeze(2).to_broadcast([128, 4, 4]),
                                                    op=ALU.is_equal), r=RT, w=RT)
                S.op("dve", lambda: V.scalar_tensor_tensor(out=r_e2[:], in0=r_is1[:], scalar=-1e30, in1=ev,
                                                           op0=ALU.mult, op1=ALU.add), r=RT, w=RT)
                S.op("dve", lambda: V.tensor_reduce(out=r_m2[:], in_=r_e2[:], axis=AX.X, op=ALU.max), r=RT, w=RT)
                S.op("dve", lambda: V.tensor_tensor(out=r_is2[:], in0=r_e2[:], in1=r_m2[:].unsqueeze(2).to_broadcast([128, 4, 4]),
                                                    op=ALU.is_equal), r=RT, w=RT)
                S.op("dve", lambda: V.tensor_tensor(out=r_w1[:], in0=r_m2[:], in1=r_m1[:], op=ALU.subtract), r=RT, w=RT)
                S.op("act", lambda: A.activation(out=r_w1[:], in_=r_w1[:], func=AF.Exp), r=RT, w=RT)
                S.op("dve", lambda: V.tensor_scalar_add(out=r_w1[:], in0=r_w1[:], scalar1=1.0), r=RT, w=RT)
                S.op("dve", lambda: V.reciprocal(out=r_w1[:], in_=r_w1[:]), r=RT, w=RT)
                S.op("dve", lambda: V.tensor_scalar(out=r_w2[:], in0=r_w1[:], scalar1=-1.0, scalar2=1.0,
                                                    op0=ALU.mult, op1=ALU.add), r=RT, w=RT)
                S.op("dve", lambda: V.tensor_tensor(out=r_w1[:], in0=r_w1[:], in1=r_gs[:], op=ALU.mult), r=RT, w=RT)
                S.op("dve", lambda: V.tensor_tensor(out=r_w2[:], in0=r_w2[:], in1=r_gs[:], op=ALU.mult), r=RT, w=RT)
                S.op("dve", lambda: V.tensor_tensor(out=r_is1[:], in0=r_is1[:], in1=r_w1[:].unsqueeze(2).to_broadcast([128, 4, 4]),
                                                    op=ALU.mult), r=RT, w=RT)
                S.op("dve", lambda: V.tensor_tensor(out=r_is2[:], in0=r_is2[:], in1=r_w2[:].unsqueeze(2).to_broadcast([128, 4, 4]),
                                                    op=ALU.mult), r=RT, w=RT)
                S.op("dve", lambda: V.tensor_tensor(out=comb[:, t, :].rearrange("p (g j) -> p g j", g=4), in0=r_is1[:], in1=r_is2[:],
                                                    op=ALU.add), r=RT, w=[("comb", t)])
            for e in range(NEXP if do_B else 0):
                slot = nload % 2
                if pending and e in (3, 6, 9, 12):
                    chunk_done(pending.pop(0))
                nload += 1
                if e + 1 < NEXP:
                    load_expert(e + 1, nload % 2)
                for g in range(GPS):
                    hs = g % 2
                    for fc in range(4):
                        pg = fc % 2
                        for kc in range(8):
                            S.op("pe", lambda kc=kc, fc=fc, pg=pg: P.matmul(ps[pg][:], lhsT=wg[slot][:, kc, fc * 128:(fc + 1) * 128],
                                                                          rhs=hT[:, kc, g * 512:(g + 1) * 512],
                                                                          start=(kc == 0), stop=(kc == 7)),
                                 r=[("wg", slot)] + [("hT", g * 4 + i) for i in range(4)], w=[("ps", pg)])
                        for kc in range(8):
                            S.op("pe", lambda kc=kc, fc=fc, pg=pg: P.matmul(ps[2 + pg][:], lhsT=wu[slot][:, kc, fc * 128:(fc + 1) * 128],
                                                                          rhs=hT[:, kc, g * 512:(g + 1) * 512],
                                                                          start=(kc == 0), stop=(kc == 7)),
                                 r=[("wu", slot)] + [("hT", g * 4 + i) for i in range(4)], w=[("ps", 2 + pg)])
                        S.op("act", lambda pg=pg: A.activation(out=sg[pg][:], in_=ps[pg][:], func=AF.Silu),
                             r=[("ps", pg)], w=[("sg", pg)])
                        S.op("dve", lambda pg=pg, fc=fc: V.tensor_tensor(out=hdn[hs][:, fc, :], in0=ps[2 + pg][:], in1=sg[pg][:], op=ALU.mult),
                             r=[("ps", 2 + pg), ("sg", pg)], w=[("hdn", hs, fc)])
                    for tt in range(4):
                        t = g * 4 + tt
                        for half in range(2):
                            pb = 4 + (tt * 2 + half) % 4
                            for fc in range(4):
                                S.op("pe", lambda fc=fc, half=half, pb=pb, tt=tt: P.matmul(ps[pb][:], lhsT=hdn[hs][:, fc, tt * 128:(tt + 1) * 128],
                                                                                       rhs=wd[slot][:, fc, half * 512:(half + 1) * 512],
                                                                                       start=(fc == 0), stop=(fc == 3)),
                                     r=[("hdn", hs, fc), ("wd", slot)], w=[("ps", pb)])
                            S.op("dve", lambda half=half, pb=pb, t=t: V.scalar_tensor_tensor(
                                out=yacc[:, t, half * 512:(half + 1) * 512], in0=ps[pb][:], scalar=comb[:, t, e:e + 1],
                                in1=yacc[:, t, half * 512:(half + 1) * 512], op0=ALU.mult, op1=ALU.add),
                                r=[("ps", pb), ("comb", t), ("yacc", t)], w=[("yacc", t)])
            for t in range(TPS):
                tok0 = s * SG + t * 128
                sl = t % 2
                src = yacc[:, t, :]
                for c in range(2):
                    S.op("dve", lambda c=c: V.bn_stats(out=st[:, c, :], in_=src[:, c * 512:(c + 1) * 512]), r=[("yacc", t)], w=["st"])
                S.op("dve", lambda: V.bn_aggr(out=mv[:], in_=st[:].rearrange("p a b -> p (a b)")), r=["st"], w=["mv"])
                S.op("dve", lambda: V.tensor_scalar_add(out=rstd[:], in0=mv[:, 1:2], scalar1=LN_EPS), r=["mv"], w=["rstd"])
                S.op("act", lambda: A.activation(out=rstd[:], in_=rstd[:], func=AF.Ln), r=["rstd"], w=["rstd"])
                S.op("act", lambda: A.activation(out=rstd[:], in_=rstd[:], func=AF.Exp, scale=-0.5), r=["rstd"], w=["rstd"])
                S.op("dve", lambda: V.tensor_scalar(out=ot[sl][:], in0=src, scalar1=mv[:, 0:1], scalar2=rstd[:],
                                                    op0=ALU.subtract, op1=ALU.mult), r=["mv", "rstd", ("yacc", t)], w=[("ot", sl)])
                S.op("pool", lambda: G.tensor_tensor(out=ot[sl][:], in0=ot[sl][:], in1=lnp[:, 2, :], op=ALU.mult), r=["lnp", ("ot", sl)], w=[("ot", sl)])
                S.op("pool", lambda: G.tensor_tensor(out=ot[sl][:], in0=ot[sl][:], in1=lnp[:, 3, :], op=ALU.add), r=["lnp", ("ot", sl)], w=[("ot", sl)])
                S.dma("sp", out_d[tok0:tok0 + 128, :], ot[sl][:], r=[("ot", sl)], sem=("ot", sl))
                if t % 2 == 1 and chunk_done:
                    pending.append(tok0 // 256)
        while pending:
            chunk_done(pending.pop(0))


def emit_gla(nc, S, ps, DD, tag, S_LEN=16384):
    D = 1024
    NT = S_LEN // 128
    h_d, wcat_d, wg2_d, bg_d, gn_d = DD["h"], DD["wcat"], DD["wg2"], DD["bg"], DD["gn"]
    id_d, tr_d, ci_d, on_d, y_d = DD["ident"], DD["trirev"], DD["cind"], DD["ones1"], DD["yout"]
    hmap = DD.get("hmap", lambda n: n)
    chunk_done = DD.get("chunk_done")
    with ExitStack() as es:
        def sb(name, shape, dt=F32):
            return es.enter_context(nc.sbuf_tensor("sb_" + tag + "_" + name, shape, dt))
        ident = sb("ident", [128, 128])
        trirev = sb("trirev", [128, 128])
        cind = sb("cind", [128, 2])
        ones1 = sb("ones1", [1, 128])
        wcat = sb("wcat", [128, 8, 784], BF16)
        wg2 = sb("wg2", [16, 128])
        bg = sb("bg", [1, 128])
        gn = sb("gn", [128, 256])
        state = sb("state", [128, 256])
        qlo = sb("qlo", [128, 128])
        qhi = sb("qhi", [128, 128])
        ht = [sb("ht%d" % i, [128, D]) for i in range(2)]
        hT = [sb("hT%d" % i, [128, 8, 128], BF16) for i in range(2)]
        lrT = sb("lrT", [16, 128])
        la = sb("la", [128, 128])
        kd = sb("kd", [128, 128])
        dec = sb("dec", [128, 2])
        kdec = sb("kdec", [128, 128], BF16)
        vbf = sb("vbf", [128, 256], BF16)
        er = sb("er", [128, 256])
        junk = sb("junk", [128, 256])
        ss = sb("ss", [128, 1])
        yo = [sb("yo%d" % i, [128, 256]) for i in range(2)]
        V, A, P, G = nc.vector, nc.scalar, nc.tensor, nc.gpsimd

        S.dma("sp", ident[:], id_d[:, :], w=["ident"], sem="ident")
        S.dma("sp", trirev[:], tr_d[:, :], w=["trirev"], sem="trirev")
        S.dma("sp", cind[:], ci_d[:, :], w=["cind"], sem="cind")
        S.dma("sp", ones1[:], on_d[:, :], w=["ones1"], sem="ones1")
        S.dma("pool", wcat[:], wcat_d.rearrange("(kc p) f -> p kc f", p=128), w=["wcat"], sem="wcat")
        S.dma("sp", wg2[:], wg2_d[:, :], w=["wg2"], sem="wg2")
        S.dma("sp", bg[:], bg_d[:, :], w=["bg"], sem="bg")
        S.dma("sp", gn[:], gn_d[0:1, :].partition_broadcast(128), w=["gn"], sem="gn")
        S.op("dve", lambda: V.memset(state[:], 0.0), w=["state"])
        S.op("dve", lambda: V.memset(qlo[:], 0.0), w=["qlo"])
        S.op("dve", lambda: V.memset(qhi[:], 0.0), w=["qhi"])

        for t in range(NT):
            tok0 = t * 128
            sl = t % 2
            S.dma("sp", ht[sl][:], h_d[hmap(tok0):hmap(tok0) + 128, :], w=[("ht", sl)], sem=("ht", sl))
            for hb in range(2):
                for j in range(4):
                    kc = hb * 4 + j
                    S.op("pe", lambda: P.transpose(out=ps[hb][:, j * 128:(j + 1) * 128],
                                                   in_=ht[sl][:, kc * 128:(kc + 1) * 128], identity=ident[:]),
                         r=[("ht", sl), "ident"], w=[("ps", hb)])
            S.op("act", lambda: A.copy(out=hT[sl][:, 0:4, :], in_=ps[0][:].rearrange("p (a b) -> p a b", a=4)),
                 r=[("ps", 0)], w=[("hT", sl, 0)])
            S.op("dve", lambda: V.tensor_copy(out=hT[sl][:, 4:8, :], in_=ps[1][:].rearrange("p (a b) -> p a b", a=4)),
                 r=[("ps", 1)], w=[("hT", sl, 1)])
            for kc in range(8):
                S.op("pe", lambda: P.matmul(ps[2][:, 0:128], lhsT=wcat[:, kc, 0:128], rhs=hT[sl][:, kc, :],
                                            start=(kc == 0), stop=(kc == 7)),
                     r=["wcat", ("hT", sl, kc // 4)], w=[("ps", 2)])
            for kc in range(8):
                S.op("pe", lambda: P.matmul(ps[2][0:16, 128:256], lhsT=wcat[:, kc, 768:784], rhs=hT[sl][:, kc, :],
                                            start=(kc == 0), stop=(kc == 7)),
                     r=["wcat", ("hT", sl, kc // 4)], w=[("ps", 2)])
            for kc in range(8):
                S.op("pe", lambda: P.matmul(ps[3][:, 0:384], lhsT=hT[sl][:, kc, :], rhs=wcat[:, kc, 128:512],
                                            start=(kc == 0), stop=(kc == 7)),
                     r=["wcat", ("hT", sl, kc // 4)], w=[("ps", 3)])
            for kc in range(8):
                S.op("pe", lambda: P.matmul(ps[4][:, 0:256], lhsT=hT[sl][:, kc, :], rhs=wcat[:, kc, 512:768],
                                            start=(kc == 0), stop=(kc == 7)),
                     r=["wcat", ("hT", sl, kc // 4)], w=[("ps", 4)])
            S.op("act", lambda: A.mul(out=qlo[:, 0:64], in_=ps[2][:, 0:64], mul=128 ** -0.5), r=[("ps", 2)], w=["qlo"])
            S.op("act", lambda: A.mul(out=qhi[:, 64:128], in_=ps[2][:, 64:128], mul=128 ** -0.5), r=[("ps", 2)], w=["qhi"])
            S.op("dve", lambda: V.tensor_copy(out=lrT[:], in_=ps[2][0:16, 128:256]), r=[("ps", 2)], w=["lrT"])
            S.op("pe", lambda: P.matmul(ps[2][:, 0:128], lhsT=lrT[:], rhs=wg2[:], start=True, stop=False),
                 r=["lrT", "wg2"], w=[("ps", 2)])
            S.op("pe", lambda: P.matmul(ps[2][:, 0:128], lhsT=ones1[:], rhs=bg[:], start=False, stop=True),
                 r=["ones1", "bg"], w=[("ps", 2)])
            S.op("act", lambda: A.activation(out=la[:], in_=ps[2][:, 0:128], func=AF.Exp, scale=-1.0), r=[("ps", 2)], w=["la"])
            S.op("dve", lambda: V.tensor_scalar_add(out=la[:], in0=la[:], scalar1=1.0), r=["la"], w=["la"])
            S.op("act", lambda: A.activation(out=la[:], in_=la[:], func=AF.Ln), r=["la"], w=["la"])
            S.op("pe", lambda: P.matmul(ps[2][:, 128:256], lhsT=trirev[:], rhs=la[:], start=True, stop=True),
                 r=["trirev", "la"], w=[("ps", 2)])
            S.op("pe", lambda: P.matmul(ps[2][:, 256:258], lhsT=la[:], rhs=cind[:], start=True, stop=True),
                 r=["cind", "la"], w=[("ps", 2)])
            S.op("act", lambda: A.activation(out=kd[:], in_=ps[2][:, 128:256], func=AF.Exp), r=[("ps", 2)], w=["kd"])
            S.op("act", lambda: A.activation(out=dec[:], in_=ps[2][:, 256:258], func=AF.Exp), r=[("ps", 2)], w=["dec"])
            S.op("dve", lambda: V.tensor_tensor(out=kdec[:], in0=ps[3][:, 0:128], in1=kd[:], op=ALU.mult),
                 r=[("ps", 3), "kd"], w=["kdec"])
            S.op("act", lambda: A.copy(out=vbf[:], in_=ps[3][:, 128:384]), r=[("ps", 3)], w=["vbf"])
            for c in range(2):
                pb = 5 + c
                S.op("pe", lambda: P.matmul(ps[pb][:, 0:256], lhsT=kdec[c * 64:(c + 1) * 64, :], rhs=vbf[c * 64:(c + 1) * 64, :],
                                            start=True, stop=True),
                     r=["kdec", "vbf"], w=[("ps", pb)])
            for c in range(2):
                pb = 5 + c
                S.op("dve", lambda: V.scalar_tensor_tensor(out=state[:], in0=state[:], scalar=dec[:, c:c + 1], in1=ps[pb][:, 0:256],
                                                           op0=ALU.mult, op1=ALU.add),
                     r=["dec", ("ps", pb), "state"], w=["state"])
                S.op("pe", lambda: P.matmul(ps[7][:, 0:256], lhsT=(qlo if c == 0 else qhi)[:], rhs=state[:],
                                            start=(c == 0), stop=(c == 1)),
                     r=["state", "qlo" if c == 0 else "qhi"], w=[("ps", 7)])
            S.op("act", lambda: A.activation(out=er[:], in_=ps[4][:, 0:256], func=AF.Exp, scale=-1.0), r=[("ps", 4)], w=["er"])
            S.op("dve", lambda: V.tensor_scalar_add(out=er[:], in0=er[:], scalar1=1.0), r=["er"], w=["er"])
            S.op("dve", lambda: V.reciprocal(out=er[:], in_=er[:]), r=["er"], w=["er"])
            S.op("dve", lambda: V.tensor_tensor(out=er[:], in0=ps[4][:, 0:256], in1=er[:], op=ALU.mult), r=[("ps", 4), "er"], w=["er"])
            S.op("dve", lambda: V.tensor_tensor(out=er[:], in0=er[:], in1=gn[:], op=ALU.mult), r=["er", "gn"], w=["er"])
            S.op("act", lambda: A.activation(out=junk[:], in_=ps[7][:, 0:256], func=AF.Square, accum_out=ss[:]),
                 r=[("ps", 7)], w=["junk", "ss"])
            S.op("dve", lambda: V.tensor_scalar(out=ss[:], in0=ss[:], scalar1=1.0 / 256, scalar2=LN_EPS, op0=ALU.mult, op1=ALU.add),
                 r=["ss"], w=["ss"])
            S.op("act", lambda: A.activation(out=ss[:], in_=ss[:], func=AF.Ln), r=["ss"], w=["ss"])
            S.op("act", lambda: A.activation(out=ss[:], in_=ss[:], func=AF.Exp, scale=-0.5), r=["ss"], w=["ss"])
            S.op("dve", lambda: V.scalar_tensor_tensor(out=yo[sl][:], in0=ps[7][:, 0:256], scalar=ss[:], in1=er[:],
                                                       op0=ALU.mult, op1=ALU.mult),
                 r=[("ps", 7), "ss", "er"], w=[("yo", sl)])
            S.dma("sp", y_d[tok0:tok0 + 128, :], yo[sl][:], r=[("yo", sl)], sem=("yo", sl))
            if (t + 1) % 8 == 0 and chunk_done:
                chunk_done(t // 8)


def emit_att(nc, S, ps, DD, tag, S_LEN=16384):
    D = 1024
    NT = S_LEN // 128
    NG = S_LEN // 512
    h_d, hkv_d, wq_d, wk_d, wv_d = DD["h"], DD["hkv"], DD["wq"], DD["wk"], DD["wv"]
    bt_d, cfar_d, lam_d, gsub_d, cst_d = DD["biasT"], DD["cfar"], DD["lamv"], DD["gsub"], DD["cst"]
    id_d, on_d, y_d = DD["ident"], DD["ones128"], DD["yout"]
    hmap = DD.get("hmap", lambda n: n)
    chunk_done = DD.get("chunk_done")
    with ExitStack() as es:
        def sb(name, shape, dt=F32):
            return es.enter_context(nc.sbuf_tensor("sb_" + tag + "_" + name, shape, dt))
        ident = sb("ident", [128, 128])
        wq = sb("wq", [128, 8, 256], BF16)
        wk = sb("wk", [128, 8, 256], BF16)
        wv = sb("wv", [128, 8, 256], BF16)
        bt = sb("bt", [128, 4, 128])
        cfar = sb("cfar", [128, 2])
        lamv = sb("lamv", [128, 4, 64])
        lam = sb("lam", [128, 4])
        cst = sb("cst", [128, 2])
        KT = [sb("KT%d" % i, [128, S_LEN], BF16) for i in range(2)]
        VV = [sb("V%d" % i, [128, NT, 128], BF16) for i in range(2)]
        QT = [[sb("QT%d_%d" % (i, s), [128, 512], BF16) for s in range(2)] for i in range(2)]
        ht = [sb("ht%d" % i, [128, D]) for i in range(2)]
        hT = sb("hT", [128, 8, 512], BF16)
        NPT = 6
        PT = [sb("PT%d" % i, [128, 512], BF16) for i in range(NPT)]
        tmp = [sb("tmp%d" % i, [128, 128]) for i in range(2)]
        ones = sb("ones", [128, 128])
        gcol = sb("gcol", [128, 1])
        Pacc = [[sb("Pacc%d_%d" % (m, i), [128, 512]) for i in range(2)] for m in range(2)]
        rinv = [sb("rinv%d" % m, [128, 512]) for m in range(2)]
        oT = sb("oT", [128, 512])
        o2 = sb("o2", [128, 512])
        yT = [sb("yT%d" % i, [128, 512]) for i in range(2)]
        yo = [sb("yo%d" % i, [128, 4, 128]) for i in range(2)]
        V, A, P, G = nc.vector, nc.scalar, nc.tensor, nc.gpsimd

        S.dma("sp", ident[:], id_d[:, :], w=["ident"], sem="ident")
        S.dma("pool", wq[:], wq_d.rearrange("(kc p) f -> p kc f", p=128), w=["wq"], sem="wq")
        S.dma("pool", wk[:], wk_d.rearrange("(kc p) f -> p kc f", p=128), w=["wk"], sem="wk")
        S.dma("pool", wv[:], wv_d.rearrange("(kc p) f -> p kc f", p=128), w=["wv"], sem="wv")
        S.dma("sp", bt[:], bt_d.rearrange("h t k q -> k (h t) q"), w=["bt"], sem="bt")
        S.dma("sp", cfar[:], cfar_d[:, :], w=["cfar"], sem="cfar")
        S.dma("sp", cst[:], cst_d[:, :], w=["cst"], sem="cst")
        S.dma("sp", lamv[:], lam_d.partition_broadcast(128), w=["lamv"], sem="lamv")
        S.dma("sp", ones[:], on_d[:, :], w=["ones"], sem="ones")
        S.dma("sp", gcol[:], gsub_d.rearrange("o d -> d o"), w=["gcol"], sem="gcol")
        L = ["lam"]
        S.op("dve", lambda: V.tensor_tensor(out=lamv[:, 0, :], in0=lamv[:, 0, :], in1=lamv[:, 1, :], op=ALU.mult), r=["lamv"], w=["lamv"])
        S.op("dve", lambda: V.tensor_tensor(out=lamv[:, 2, :], in0=lamv[:, 2, :], in1=lamv[:, 3, :], op=ALU.mult), r=["lamv"], w=["lamv"])
        S.op("dve", lambda: V.tensor_reduce(out=lam[:, 0:1], in_=lamv[:, 0, :], axis=AX.X, op=ALU.add), r=["lamv"], w=L)
        S.op("dve", lambda: V.tensor_reduce(out=lam[:, 1:2], in_=lamv[:, 2, :], axis=AX.X, op=ALU.add), r=["lamv"], w=L)
        S.op("act", lambda: A.activation(out=lam[:, 0:2], in_=lam[:, 0:2], func=AF.Exp), r=L, w=L)
        S.op("dve", lambda: V.tensor_tensor(out=lam[:, 2:3], in0=lam[:, 1:2], in1=lam[:, 0:1], op=ALU.subtract), r=L, w=L)
        S.op("dve", lambda: V.tensor_tensor(out=lam[:, 3:4], in0=lam[:, 2:3], in1=cst[:, 0:1], op=ALU.subtract), r=L + ["cst"], w=L)
        S.op("dve", lambda: V.tensor_tensor(out=gcol[:], in0=gcol[:], in1=cst[:, 1:2], op=ALU.mult), r=["gcol", "cst"], w=["gcol"])

        def load_hT(src_d, g):
            for tt in range(4):
                tok0 = g * 512 + tt * 128
                sl = tt % 2
                S.dma("sp", ht[sl][:], src_d[hmap(tok0):hmap(tok0) + 128, :], w=[("ht", sl)], sem=("ht", sl))
                for hb in range(2):
                    for j in range(4):
                        kc = hb * 4 + j
                        S.op("pe", lambda: P.transpose(out=ps[6 + hb][:, j * 128:(j + 1) * 128],
                                                       in_=ht[sl][:, kc * 128:(kc + 1) * 128], identity=ident[:]),
                             r=[("ht", sl), "ident"], w=[("ps", 6 + hb)])
                S.op("act", lambda: A.copy(out=hT[:, 0:4, tt * 128:(tt + 1) * 128], in_=ps[6][:].rearrange("p (a b) -> p a b", a=4)),
                     r=[("ps", 6)], w=[("hT", tt)])
                S.op("dve", lambda: V.tensor_copy(out=hT[:, 4:8, tt * 128:(tt + 1) * 128], in_=ps[7][:].rearrange("p (a b) -> p a b", a=4)),
                     r=[("ps", 7)], w=[("hT", tt)])
        HTK = [("hT", i) for i in range(4)]

        for g in range(NG):
            load_hT(hkv_d, g)
            for i in range(2):
                for kc in range(8):
                    S.op("pe", lambda: P.matmul(ps[6][:], lhsT=wk[:, kc, i * 128:(i + 1) * 128], rhs=hT[:, kc, :],
                                                start=(kc == 0), stop=(kc == 7)), r=["wk"] + HTK, w=[("ps", 6)])
                S.op("act" if i == 0 else "dve",
                     (lambda: A.copy(out=KT[i][:, g * 512:(g + 1) * 512], in_=ps[6][:])) if i == 0 else
                     (lambda: V.tensor_copy(out=KT[i][:, g * 512:(g + 1) * 512], in_=ps[6][:])),
                     r=[("ps", 6)], w=[("KT", i)])
            for tt in range(4):
                for kc in range(8):
                    S.op("pe", lambda: P.matmul(ps[7][:, 0:256], lhsT=hT[:, kc, tt * 128:(tt + 1) * 128], rhs=wv[:, kc, :],
                                                start=(kc == 0), stop=(kc == 7)), r=["wv"] + HTK, w=[("ps", 7)])
                S.op("act", lambda: A.copy(out=VV[0][:, g * 4 + tt, 0:128], in_=ps[7][:, 0:128]), r=[("ps", 7)], w=[("V", 0)])
                S.op("dve", lambda: V.tensor_copy(out=VV[1][:, g * 4 + tt, 0:128], in_=ps[7][:, 128:256]), r=[("ps", 7)], w=[("V", 1)])

        state = {"pt": 0, "sb": 0}
        SBANK = [(0, 1), (4, 5)]
        RS = 7
        pinit = {}

        def emit_S(it):
            (g, hd, j, qs) = it
            c0 = max(0, j - 4 * g)
            banks = SBANK[state["sb"] % 2]
            state["sb"] += 1
            ptis = (state["pt"] % NPT, (state["pt"] + 1) % NPT)
            state["pt"] += 2
            for mp in range(2):
                lo = mp * 64
                S.op("pe", lambda: P.matmul(ps[banks[mp]][:, c0 * 128:512], lhsT=KT[hd][lo:lo + 64, j * 128:(j + 1) * 128],
                                            rhs=QT[hd][qs][lo:lo + 64, c0 * 128:512], start=True, stop=True),
                     r=[("KT", hd), ("QT", hd, qs)], w=[("ps", banks[mp])])
            for mp in range(2):
                sbk = banks[mp]
                pti = ptis[mp]
                if j < 4 * g - 1:
                    S.op("act", lambda: A.activation(out=PT[pti][:], in_=ps[sbk][:], func=AF.Exp, scale=0.125, bias=cfar[:, hd:hd + 1]),
                         r=[("ps", sbk), "cfar"], w=[("PT", pti)])
                else:
                    for c in range(c0, 4):
                        i = 4 * g + c
                        cs = slice(c * 128, (c + 1) * 128)
                        if j < i - 1:
                            S.op("act", lambda: A.activation(out=PT[pti][:, cs], in_=ps[sbk][:, cs], func=AF.Exp, scale=0.125,
                                                             bias=cfar[:, hd:hd + 1]),
                                 r=[("ps", sbk), "cfar"], w=[("PT", pti)])
                        else:
                            ty = 0 if j == i else 1
                            tb = (c + mp) % 2
                            S.op("dve", lambda: V.scalar_tensor_tensor(out=tmp[tb][:], in0=ps[sbk][:, cs], scalar=0.125,
                                                                       in1=bt[:, hd * 2 + ty, :], op0=ALU.mult, op1=ALU.add),
                                 r=[("ps", sbk), "bt"], w=[("tmp", tb)])
                            S.op("act", lambda: A.activation(out=PT[pti][:, cs], in_=tmp[tb][:], func=AF.Exp),
                                 r=[("tmp", tb)], w=[("PT", pti)])
            return (c0, ptis)

        def emit_PV(it, info):
            (g, hd, j, qs) = it
            (c0, ptis) = info
            cs = slice(c0 * 128, 512)
            for mp in range(2):
                pti = ptis[mp]
                S.op("pe", lambda: P.matmul(ps[2 + mp][:, cs], lhsT=VV[hd][:, j, :], rhs=PT[pti][:, cs],
                                            start=(j == 0), stop=(j == 4 * g + 3), skip_group_check=True),
                     r=[("PT", pti), ("V", hd)], w=[("ps", 2 + mp)])
            for mp in range(2):
                pti = ptis[mp]
                a = 1 if (2 * j + mp) % 3 == 0 else 0
                eng, E = ("dve", V) if a == 0 else ("pool", G)
                key = (g, hd, mp, a)
                if key not in pinit:
                    pinit[key] = True
                    if c0 > 0:
                        S.op(eng, lambda: E.memset(Pacc[mp][a][:, 0:c0 * 128], 0.0), w=[("Pacc", mp, a)])
                    S.op(eng, lambda: E.tensor_copy(out=Pacc[mp][a][:, cs], in_=PT[pti][:, cs]), r=[("PT", pti)], w=[("Pacc", mp, a)])
                else:
                    S.op(eng, lambda: E.tensor_tensor(out=Pacc[mp][a][:, cs], in0=Pacc[mp][a][:, cs], in1=PT[pti][:, cs], op=ALU.add),
                         r=[("PT", pti), ("Pacc", mp, a)], w=[("Pacc", mp, a)])
            if j == 4 * g + 3:
                for mp in range(2):
                    accs = [a2 for a2 in range(2) if (g, hd, mp, a2) in pinit]
                    for n2, a2 in enumerate(accs):
                        S.op("pe", lambda: P.matmul(ps[RS][:], lhsT=ones[:], rhs=Pacc[mp][a2][:], start=(n2 == 0), stop=(n2 == len(accs) - 1)),
                             r=["ones", ("Pacc", mp, a2)], w=[("ps", RS)])
                    S.op("dve", lambda: V.reciprocal(out=rinv[mp][:], in_=ps[RS][:]), r=[("ps", RS)], w=[("rinv", mp)])

        def epilogue(g, hd, ys):
            S.op("dve", lambda: V.tensor_tensor(out=oT[:], in0=ps[2][:], in1=rinv[0][:], op=ALU.mult), r=[("ps", 2), ("rinv", 0)], w=["oT"])
            S.op("dve", lambda: V.tensor_tensor(out=o2[:], in0=ps[3][:], in1=rinv[1][:], op=ALU.mult), r=[("ps", 3), ("rinv", 1)], w=["o2"])
            S.op("dve", lambda: V.scalar_tensor_tensor(out=oT[:], in0=o2[:], scalar=lam[:, 3:4], in1=oT[:], op0=ALU.mult, op1=ALU.add),
                 r=["o2", "oT", "lam"], w=["oT"])
            S.op("act", lambda: A.activation(out=o2[:], in_=oT[:], func=AF.Square), r=["oT"], w=["o2"])
            S.op("pe", lambda: P.matmul(ps[RS][:], lhsT=ones[:], rhs=o2[:], start=True, stop=True), r=["ones", "o2"], w=[("ps", RS)])
            S.op("dve", lambda: V.tensor_scalar(out=o2[:], in0=ps[RS][:], scalar1=1.0 / 128, scalar2=LN_EPS, op0=ALU.mult, op1=ALU.add),
                 r=[("ps", RS)], w=["o2"])
            S.op("act", lambda: A.activation(out=o2[:], in_=o2[:], func=AF.Ln), r=["o2"], w=["o2"])
            S.op("act", lambda: A.activation(out=o2[:], in_=o2[:], func=AF.Exp, scale=-0.5), r=["o2"], w=["o2"])
            S.op("dve", lambda: V.scalar_tensor_tensor(out=yT[hd][:], in0=oT[:], scalar=gcol[:, 0:1], in1=o2[:], op0=ALU.mult, op1=ALU.mult),
                 r=["oT", "o2", "gcol"], w=[("yT", hd)])
            for c in range(4):
                S.op("pe", lambda: P.transpose(out=ps[6][:, c * 128:(c + 1) * 128], in_=yT[hd][:, c * 128:(c + 1) * 128], identity=ident[:]),
                     r=[("yT", hd), "ident"], w=[("ps", 6)])
            S.op("act", lambda: A.copy(out=yo[hd][:], in_=ps[6][:].rearrange("p (c f) -> p c f", c=4)), r=[("ps", 6)], w=[("yo", hd)])
            S.dma("sp", y_d[g * 512:(g + 1) * 512, hd * 128:(hd + 1) * 128].rearrange("(c p) f -> p c f", p=128), yo[hd][:],
                  r=[("yo", hd)], sem=("yo", hd))

        for g in range(NG):
            qs = g % 2
            ys = g % 2
            load_hT(h_d, g)
            for hd in range(2):
                for kc in range(8):
                    S.op("pe", lambda: P.matmul(ps[6][:], lhsT=wq[:, kc, hd * 128:(hd + 1) * 128], rhs=hT[:, kc, :],
                                                start=(kc == 0), stop=(kc == 7)), r=["wq"] + HTK, w=[("ps", 6)])
                S.op("dve", lambda: V.tensor_copy(out=QT[hd][qs][:], in_=ps[6][:]), r=[("ps", 6)], w=[("QT", hd, qs)])
            if g >= 2 and g % 2 == 0 and chunk_done:
                chunk_done(g // 2 - 1)
            for hd in range(2):
                items = [(g, hd, j, qs) for j in range(4 * g + 4)]
                info = emit_S(items[0])
                for n in range(len(items)):
                    nxt = emit_S(items[n + 1]) if n + 1 < len(items) else None
                    emit_PV(items[n], info)
                    info = nxt
                epilogue(g, hd, ys)
        if chunk_done:
            chunk_done(NG // 2 - 1)


def build_fused():
    nc = bass.Bass("TRN2", target_bir_lowering=False)
    SL, D = 16384, 1024
    ext = lambda name, shape: nc.dram_tensor(name, shape, F32, kind="ExternalInput").ap()
    loc = lambda name, shape: nc.dram_tensor(name, shape, F32).ap()
    xb = ext("xb", [SL, D])
    xs = ext("xs", [4096, D])
    idx_d = nc.dram_tensor("idx", [128, 128], mybir.dt.uint32, kind="ExternalInput").ap()
    g_wcat, g_wg2, g_bg, g_gn = ext("g_wcat", [2, D, 784]), ext("g_wg2", [2, 16, 128]), ext("g_bg", [2, 1, 128]), ext("g_gn", [2, 1, 256])
    a_wq, a_wk, a_wv = ext("a_wq", [2, D, 256]), ext("a_wk", [D, 256]), ext("a_wv", [D, 256])
    a_bt, a_cfar, a_lamv = ext("a_biasT", [2, 2, 128, 128]), ext("a_cfar", [128, 2]), ext("a_lamv", [2, 4, 64])
    a_gsub, a_cst = ext("a_gsub", [2, 1, 128]), ext("a_cst", [2, 128, 2])
    p_wout, p_lnp, p_wr, p_rb = ext("p_wout", [4, D, D]), ext("p_lnp", [4, 4, D]), ext("p_wr", [4, D, 20]), ext("p_rb", [4, 1, 20])
    p_wg, p_wu, p_wd = ext("p_wg", [4, 16, D, 512]), ext("p_wu", [4, 16, D, 512]), ext("p_wd", [4, 16, 512, D])
    ident, trirev, cind = ext("ident", [128, 128]), ext("trirev", [128, 128]), ext("cind", [128, 2])
    ones1, ones128 = ext("ones1", [1, 128]), ext("ones128", [128, 128])
    out = nc.dram_tensor("out", [4096, D], F32, kind="ExternalOutput").ap()
    yloc, yg = loc("yloc", [SL, 256]), loc("yg", [4 * SL, 256])
    hloc = [loc("hloc%d" % i, [4096, D]) for i in range(3)]
    hg0, hkvg, hg2 = loc("hg0", [SL, D]), loc("hkvg", [SL, D]), loc("hg2", [SL, D])
    GROUPS = [[0, 1, 2, 3], [4, 5, 6, 7]]

    def hperm(n):
        r_, w_ = n // 4096, n % 4096
        return (w_ // 256) * 1024 + r_ * 256 + (w_ % 256)

    def gather_y(S):
        S.coll_multi("AllGather", GROUPS, [(yloc[i * 1024:(i + 1) * 1024, :], yg[i * 4096:(i + 1) * 4096, :]) for i in range(16)])

    def gather_h(S, src, dst):
        S.coll_multi("AllGather", GROUPS, [(src[i * 256:(i + 1) * 256, :], dst[i * 1024:(i + 1) * 1024, :]) for i in range(16)])
    with ExitStack() as es:
        S = Sched(nc, es)
        ps = [es.enter_context(nc.psum_tensor("ps%d" % i, [128, 512], F32)) for i in range(8)]

        def ydone(i):
            S.coll_async("AllGather", GROUPS, yloc[i * 1024:(i + 1) * 1024, :], yg[i * 4096:(i + 1) * 4096, :], [("yo", 0), ("yo", 1)])

        def post(layer, ysrc, hp, dst, gdst=None):
            hdone = None
            if gdst is not None:
                hdone = lambda i: S.coll_async("AllGather", GROUPS, dst[i * 256:(i + 1) * 256, :], gdst[i * 1024:(i + 1) * 1024, :],
                                               [("ot", 0), ("ot", 1)])
            emit_post(nc, S, ps, {"y": ysrc[:, :], "idx": idx_d[:, :], "hp": hp, "chunk_done": hdone, "wout": p_wout[layer], "lnp": p_lnp[layer], "wr": p_wr[layer], "rb": p_rb[layer],
                                  "wg": p_wg[layer], "wu": p_wu[layer], "wd": p_wd[layer], "ident": ident, "out": dst},
                      "p%d" % layer)

        def gla(layer, h):
            emit_gla(nc, S, ps, {"h": h, "chunk_done": ydone, "hmap": (hperm if layer > 0 else (lambda n: n)), "wcat": g_wcat[layer], "wg2": g_wg2[layer], "bg": g_bg[layer], "gn": g_gn[layer],
                                 "ident": ident, "trirev": trirev, "cind": cind, "ones1": ones1, "yout": yloc}, "g%d" % layer)

        def att(j, h):
            emit_att(nc, S, ps, {"h": h, "hkv": hkvg, "chunk_done": ydone, "hmap": hperm, "wq": a_wq[j], "wk": a_wk, "wv": a_wv, "biasT": a_bt, "cfar": a_cfar,
                                 "lamv": a_lamv[j], "gsub": a_gsub[j], "cst": a_cst[j], "ident": ident, "ones128": ones128,
                                 "yout": yloc}, "a%d" % j)

        import os
        LEVEL = int(os.environ.get("FUSED_LEVEL", "99"))
        steps = [
            lambda: gla(0, xb), lambda: S.barrier(), lambda: post(0, yg, xs, hloc[0], hg0), lambda: S.barrier(),
            lambda: gla(1, hg0), lambda: S.barrier(), lambda: post(1, yg, hloc[0], hloc[1], hkvg), lambda: S.barrier(),
            lambda: att(0, hkvg), lambda: S.barrier(), lambda: post(2, yg, hloc[1], hloc[2], hg2), lambda: S.barrier(),
            lambda: att(1, hg2), lambda: S.barrier(), lambda: post(3, yg, hloc[2], out),
        ]
        for i_, st_ in enumerate(steps):
            if i_ >= LEVEL:
                break
            st_()
        S.barrier()
    return nc


_NC = {}


def kernel(x, a_w_in, a_w_gate2, a_b_gate, a_g_norm, a_w_out, kv_w, b_w_q, b_lam_q1, b_lam_k1,
           b_lam_q2, b_lam_k2, b_g_sub, b_w_out, rel_table, moe_w_group, moe_b_group, moe_w_router,
           moe_b_router, moe_w_gate, moe_w_up, moe_w_down, ln_g, ln_b):
    f32 = np.float32
    A = lambda a: np.ascontiguousarray(np.asarray(a, dtype=f32))
    x = A(x)
    B, S_, D = x.shape
    if "nc" not in _NC:
        _NC["nc"] = build_fused()
    nc = _NC["nc"]
    w_in = A(a_w_in)
    kvw = A(kv_w)
    wqf = A(b_w_q)
    rt = A(rel_table)
    gconst = gla_consts()
    p_wout = A(np.stack([a_w_out[0], a_w_out[1], b_w_out[0], b_w_out[1]]))
    p_lnp = A(np.stack([np.stack([ln_g[l, 0], ln_b[l, 0], ln_g[l, 1], ln_b[l, 1]]) for l in range(4)]))
    p_wr = A(np.concatenate([moe_w_group, moe_w_router], axis=2))
    p_rb = A(np.concatenate([np.asarray(moe_b_group).reshape(4, -1), np.asarray(moe_b_router).reshape(4, -1)], axis=1)[:, None, :])
    p_wg, p_wu, p_wd = A(moe_w_gate), A(moe_w_up), A(moe_w_down)
    linits = [0.8 - 0.6 * math.exp(-0.3 * layer) for layer in (2, 3)]
    a_cst = A(np.stack([np.broadcast_to(np.array([[li, 1.0 - li]], f32), (128, 2)) for li in linits]))
    a_lamv = A(np.stack([np.stack([b_lam_q1[j], b_lam_k1[j], b_lam_q2[j], b_lam_k2[j]]) for j in range(2)]))
    a_gsub = A(np.asarray(b_g_sub)[:, None, :])
    in_maps = []
    for c in range(8):
        b, r = c // 4, c % 4
        hd = r
        g_wcat = np.stack([np.concatenate([w_in[l][:, hd * 128:(hd + 1) * 128], w_in[l][:, 512 + hd * 128:512 + (hd + 1) * 128],
                                           w_in[l][:, 1024 + hd * 256:1024 + (hd + 1) * 256], w_in[l][:, 2048 + hd * 256:2048 + (hd + 1) * 256],
                                           w_in[l][:, 3072:3088]], axis=1) for l in range(2)])
        heads = [2 * r, 2 * r + 1]
        a_wq = np.stack([np.concatenate([wqf[j][:, hh * 64:(hh + 1) * 64] if m_ == 0 else wqf[j][:, 512 + hh * 64:512 + (hh + 1) * 64]
                                         for hh in heads for m_ in range(2)], axis=1) for j in range(2)])
        a_wk = np.concatenate([kvw[:, hh * 64:(hh + 1) * 64] if m_ == 0 else kvw[:, 512 + hh * 64:512 + (hh + 1) * 64]
                               for hh in heads for m_ in range(2)], axis=1)
        a_wv = np.concatenate([kvw[:, 1024 + hh * 128:1024 + (hh + 1) * 128] for hh in heads], axis=1)
        idxv = np.zeros((128, 128), np.uint32)
        for r_ in range(4):
            for t_ in range(32):
                n_ = r * 4096 + t_ * 128
                idxv[:, r_ * 32 + t_] = (n_ // 1024) * 4096 + r_ * 1024 + (n_ % 1024) + np.arange(128)
        m = {"xb": x[b], "xs": np.ascontiguousarray(x[b, r * 4096:(r + 1) * 4096]), "idx": idxv, "g_wcat": A(g_wcat), "g_wg2": A(np.asarray(a_w_gate2)[:, :, hd * 128:(hd + 1) * 128]),
             "g_bg": A(np.asarray(a_b_gate)[:, None, hd * 128:(hd + 1) * 128]), "g_gn": A(np.asarray(a_g_norm)[:, None, hd * 256:(hd + 1) * 256]),
             "a_wq": A(a_wq), "a_wk": A(a_wk), "a_wv": A(a_wv), "a_biasT": att_bias_tiles(rt, heads),
             "a_cfar": A(np.broadcast_to(rt[15, heads][None, :], (128, 2))), "a_lamv": a_lamv, "a_gsub": a_gsub, "a_cst": a_cst,
             "p_wout": p_wout, "p_lnp": p_lnp, "p_wr": p_wr, "p_rb": p_rb, "p_wg": p_wg, "p_wu": p_wu, "p_wd": p_wd,
             "ident": gconst["ident"], "trirev": gconst["trirev"], "cind": gconst["cind"], "ones1": gconst["ones1"],
             "ones128": np.ones((128, 128), f32)}
        in_maps.append(m)
    res = run_bass_kernel_spmd(nc, in_maps, core_ids=list(range(8)))
    return np.concatenate([res.results[c]["out"] for c in range(8)], axis=0).reshape(B, S_, D)
```

```python
from contextlib import ExitStack
import math
import numpy as np
import concourse.bass as bass
import concourse.mybir as mybir
from concourse.bass_utils import run_bass_kernel_spmd

F32 = mybir.dt.float32
BF16 = mybir.dt.bfloat16
AF = mybir.ActivationFunctionType
ALU = mybir.AluOpType
AX = mybir.AxisListType


class Sched:
    ENG = ("pe", "act", "dve", "pool", "sp")

    def __init__(self, nc, es):
        self.nc = nc
        self.es = es
        self.eng = {"pe": nc.tensor, "act": nc.scalar, "dve": nc.vector, "pool": nc.gpsimd, "sp": nc.sync}
        self.sem = {e: es.enter_context(nc.semaphore("s_" + e)) for e in self.ENG}
        self.cnt = {e: 0 for e in self.ENG}
        self.seen = {e: {} for e in self.ENG}
        self.snaps = {e: [None] for e in self.ENG}
        self.dsem = {}
        self.dcnt = {}
        self.lastw = {}
        self.readers = {}
        self.nwait = 0
        self.ninst = 0

    def _deps(self, r, w):
        deps = []
        for k in r:
            t = self.lastw.get(k)
            if t is not None:
                deps.append(t)
        for k in w:
            t = self.lastw.get(k)
            if t is not None:
                deps.append(t)
            deps.extend(self.readers.get(k, ()))
        return deps

    def _wait(self, e, deps, skip_dma_sem=None):
        seen = self.seen[e]
        need = {}
        for (src, val) in deps:
            if src == e and e == "pe":
                continue
            if skip_dma_sem is not None and src == skip_dma_sem:
                continue
            if seen.get(src, 0) >= val:
                continue
            if need.get(src, 0) < val:
                need[src] = val
        if not need:
            return
        seen = dict(seen)
        for src, val in need.items():
            if isinstance(src, tuple):
                self.eng[e].wait_ge(self.dsem[src[1]], val)
            else:
                self.eng[e].wait_ge(self.sem[src], val)
                snap = self.snaps[src][val]
                if snap:
                    for s2, v2 in snap.items():
                        if seen.get(s2, 0) < v2:
                            seen[s2] = v2
            if seen.get(src, 0) < val:
                seen[src] = val
            self.nwait += 1
        self.seen[e] = seen

    def _commit(self, tok, r, w):
        for k in w:
            self.lastw[k] = tok
            self.readers[k] = []
        for k in r:
            self.readers.setdefault(k, []).append(tok)

    def op(self, e, fn, r=(), w=()):
        px = [k for k in r if isinstance(k, tuple) and k[0] == "ps"]
        if px:
            r = [k for k in r if k not in px]
            w = list(w) + px
        self._wait(e, self._deps(r, w))
        ins = fn()
        self.cnt[e] += 1
        ins.then_inc(self.sem[e], 1)
        self.snaps[e].append(self.seen[e])
        self._commit((e, self.cnt[e]), r, w)
        self.ninst += 1
        return ins

    def dma(self, q, out, in_, r=(), w=(), sem=None):
        assert sem is not None
        if sem not in self.dsem:
            self.dsem[sem] = self.es.enter_context(self.nc.semaphore("d_%d" % len(self.dsem)))
            self.dcnt[sem] = 0
        self._wait(q, self._deps(r, w), skip_dma_sem=("dma", sem))
        ins = self.eng[q].dma_start(out=out, in_=in_)
        self.dcnt[sem] += 16
        ins.then_inc(self.dsem[sem], 16)
        self._commit((("dma", sem), self.dcnt[sem]), r, w)
        self.ninst += 1
        return ins

    def finish(self, keys):
        deps = []
        for k in keys:
            t = self.lastw.get(k)
            if t is not None:
                deps.append(t)
            deps.extend(self.readers.get(k, ()))
        self._wait("sp", deps)


def _sched_barrier(self):
    for e in self.ENG:
        deps = [(f, self.cnt[f]) for f in self.ENG if self.cnt[f] > 0]
        deps += [(("dma", k), v) for k, v in self.dcnt.items() if v > 0]
        self._wait(e, deps)
    self.lastw = {}
    self.readers = {}


def _sched_coll(self, kind, groups, in_ap, out_ap):
    self.barrier()
    if "cc" not in self.dsem:
        self.dsem["cc"] = self.es.enter_context(self.nc.semaphore("d_cc"))
        self.dcnt["cc"] = 0
    ins = self.nc.gpsimd.collective_compute(kind, ALU.bypass, replica_groups=groups, ins=[in_ap], outs=[out_ap])
    self.dcnt["cc"] += 1
    ins.then_inc(self.dsem["cc"])
    self.ninst += 1
    self.barrier()


Sched.barrier = _sched_barrier
Sched.coll = _sched_coll


def _sched_dma_fn(self, q, fn, r=(), w=(), sem=None):
    if sem not in self.dsem:
        self.dsem[sem] = self.es.enter_context(self.nc.semaphore("d_%d" % len(self.dsem)))
        self.dcnt[sem] = 0
    self._wait(q, self._deps(r, w), skip_dma_sem=("dma", sem))
    ins = fn()
    self.dcnt[sem] += 16
    ins.then_inc(self.dsem[sem], 16)
    self._commit((("dma", sem), self.dcnt[sem]), r, w)
    self.ninst += 1
    return ins


Sched.dma_fn = _sched_dma_fn


def _sched_coll_multi(self, kind, groups, pairs):
    self.barrier()
    if "cc" not in self.dsem:
        self.dsem["cc"] = self.es.enter_context(self.nc.semaphore("d_cc"))
        self.dcnt["cc"] = 0
    for (in_ap, out_ap) in pairs:
        ins = self.nc.gpsimd.collective_compute(kind, ALU.bypass, replica_groups=groups, ins=[in_ap], outs=[out_ap])
        self.dcnt["cc"] += 1
        ins.then_inc(self.dsem["cc"])
        self.ninst += 1
        self.nc.gpsimd.wait_ge(self.dsem["cc"], self.dcnt["cc"])
    self.barrier()


Sched.coll_multi = _sched_coll_multi


def _sched_coll_async(self, kind, groups, in_ap, out_ap, wait_sems):
    if "cc" not in self.dsem:
        self.dsem["cc"] = self.es.enter_context(self.nc.semaphore("d_cc"))
        self.dcnt["cc"] = 0
    deps = [(("dma", k), self.dcnt[k]) for k in wait_sems if self.dcnt.get(k, 0) > 0]
    if self.dcnt["cc"] > 0:
        deps.append((("dma", "cc"), self.dcnt["cc"]))
    self._wait("pool", deps)
    ins = self.nc.gpsimd.collective_compute(kind, ALU.bypass, replica_groups=groups, ins=[in_ap], outs=[out_ap], dma_qos="P3")
    self.dcnt["cc"] += 1
    ins.then_inc(self.dsem["cc"])
    self.ninst += 1


Sched.coll_async = _sched_coll_async

ALPHA = (2.0 * 4) ** 0.25
LN_EPS = 1e-5
TAU = 16.0
NEGB = -30000.0

LN_EPS = 1e-5
TAU = 16.0


def gla_consts():
    s = np.arange(128)
    same = (s[:, None] // 64) == (s[None, :] // 64)
    trirev = ((s[:, None] > s[None, :]) & same).astype(np.float32) * (-1.0 / TAU)
    cind = np.zeros((128, 2), np.float32)
    cind[:64, 0] = -1.0 / TAU
    cind[64:, 1] = -1.0 / TAU
    return {"ident": np.eye(128, dtype=np.float32), "trirev": trirev, "cind": cind,
            "ones1": np.ones((1, 128), np.float32)}


LN_EPS = 1e-5
NEGB = -30000.0


def rel_bucket_np(rel):
    nb = 16
    max_exact = 8
    base = np.where(rel > 0, nb, 0)
    n = np.abs(rel)
    large = max_exact + (np.log(np.maximum(n, 1).astype(np.float32) / np.float32(max_exact))
                         / np.float32(math.log(128 / max_exact)) * np.float32(nb - max_exact)).astype(np.int32)
    large = np.minimum(large, nb - 1)
    return base + np.where(n < max_exact, n, large)


def att_bias_tiles(rel_table, heads):
    kl = np.arange(128)[:, None]
    ql = np.arange(128)[None, :]
    out = np.zeros((len(heads), 2, 128, 128), np.float32)
    bd = rel_bucket_np(kl - ql)
    bp = rel_bucket_np(kl - ql - 128)
    vis = (kl // 64) <= (ql // 64)
    for i, h in enumerate(heads):
        out[i, 0] = np.where(vis, rel_table[bd, h], np.float32(NEGB))
        out[i, 1] = rel_table[bp, h]
    return out


def emit_post(nc, S, ps, D, tag, NTOK=4096, SG=1024, NEXP=16, do_A=True, do_B=True, do_R=True, stage=9):
    D_ = 1024
    D, DD = D_, D
    FF = 512
    y_d, hp_d, wout_d, lnp_d, wr_d, rb_d = DD["y"], DD["hp"], DD["wout"], DD["lnp"], DD["wr"], DD["rb"]
    wg_d, wu_d, wd_d, id_d, out_d = DD["wg"], DD["wu"], DD["wd"], DD["ident"], DD["out"]
    chunk_done = DD.get("chunk_done")
    NSG = NTOK // SG
    TPS = SG // 128
    GPS = SG // 512
    with ExitStack() as es:
        def sb(name, shape, dt=F32):
            return es.enter_context(nc.sbuf_tensor("sb_" + tag + "_" + name, shape, dt))
        ident = sb("ident", [128, 128])
        idx = sb("idx", [128, 128], mybir.dt.uint32)
        wout = sb("wout", [128, 8, D], BF16)
        wr = sb("wr", [128, 8, 20])
        rb = sb("rb", [128, 20])
        lnp = sb("lnp", [128, 4, D])
        hT = sb("hT", [128, 8, SG], BF16)
        yacc = sb("yacc", [128, TPS, D])
        comb = sb("comb", [128, TPS, 16])
        wg = [sb("wg%d" % i, [128, 8, FF], BF16) for i in range(2)]
        wu = [sb("wu%d" % i, [128, 8, FF], BF16) for i in range(2)]
        wd = [sb("wd%d" % i, [128, 4, D], BF16) for i in range(2)]
        yt = [sb("yt%d" % i, [128, D]) for i in range(2)]
        hpt = [sb("hpt%d" % i, [128, D]) for i in range(2)]
        yT = [sb("yT%d" % i, [128, 8, 128], BF16) for i in range(2)]
        zt = [sb("z%d" % i, [128, D]) for i in range(2)]
        hT32 = [sb("hT32_%d" % i, [128, 8, 128]) for i in range(2)]
        sg = [sb("sg%d" % i, [128, 512]) for i in range(2)]
        hdn = [sb("hdn%d" % i, [128, 4, 512], BF16) for i in range(2)]
        ot = [sb("ot%d" % i, [128, D]) for i in range(2)]
        st = sb("stats", [128, 2, 6])
        mv = sb("mv", [128, 2])
        rstd = sb("rstd", [128, 1])
        lg = sb("lg", [128, 20])
        r_gmax = sb("r_gmax", [128, 1])
        r_gmask = sb("r_gmask", [128, 4])
        r_gt = sb("r_gt", [128, 4])
        r_gsum = sb("r_gsum", [128, 1])
        r_m1 = sb("r_m1", [128, 4])
        r_m2 = sb("r_m2", [128, 4])
        r_is1 = sb("r_is1", [128, 4, 4])
        r_is2 = sb("r_is2", [128, 4, 4])
        r_e2 = sb("r_e2", [128, 4, 4])
        r_w1 = sb("r_w1", [128, 4])
        r_w2 = sb("r_w2", [128, 4])
        r_gs = sb("r_gs", [128, 4])

        V, A, P, G = nc.vector, nc.scalar, nc.tensor, nc.gpsimd

        S.dma("sp", ident[:], id_d[:, :], w=["ident"], sem="ident")
        S.dma("sp", idx[:], DD["idx"], w=["idx"], sem="idx")
        S.dma("pool", wout[:], wout_d.rearrange("(kc p) f -> p kc f", p=128), w=["wout"], sem="wout")
        S.dma("sp", wr[:], wr_d.rearrange("(kc p) f -> p kc f", p=128), w=["wr"], sem="wr")
        S.dma("sp", rb[:], rb_d[0:1, :].partition_broadcast(128), w=["rb"], sem="rb")
        S.dma("sp", lnp[:], lnp_d.partition_broadcast(128), w=["lnp"], sem="lnp")

        def load_expert(e, slot):
            S.dma("pool", wg[slot][:], wg_d[e].rearrange("(kc p) f -> p kc f", p=128), w=[("wg", slot)], sem=("wg", slot))
            S.dma("pool", wu[slot][:], wu_d[e].rearrange("(kc p) f -> p kc f", p=128), w=[("wu", slot)], sem=("wu", slot))
            S.dma("pool", wd[slot][:], wd_d[e].rearrange("(kc p) f -> p kc f", p=128), w=[("wd", slot)], sem=("wd", slot))

        def layernorm(src, dst, gi, sl):
            for c in range(2):
                S.op("dve", lambda c=c: V.bn_stats(out=st[:, c, :], in_=src[:, c * 512:(c + 1) * 512]), r=[sl], w=["st"])
            S.op("dve", lambda: V.bn_aggr(out=mv[:], in_=st[:].rearrange("p a b -> p (a b)")), r=["st"], w=["mv"])
            S.op("dve", lambda: V.tensor_scalar_add(out=rstd[:], in0=mv[:, 1:2], scalar1=LN_EPS), r=["mv"], w=["rstd"])
            S.op("act", lambda: A.activation(out=rstd[:], in_=rstd[:], func=AF.Ln), r=["rstd"], w=["rstd"])
            S.op("act", lambda: A.activation(out=rstd[:], in_=rstd[:], func=AF.Exp, scale=-0.5), r=["rstd"], w=["rstd"])
            S.op("dve", lambda: V.tensor_scalar(out=dst, in0=src, scalar1=mv[:, 0:1], scalar2=rstd[:],
                                                op0=ALU.subtract, op1=ALU.mult), r=["mv", "rstd", sl], w=[sl])
            S.op("pool", lambda: G.tensor_tensor(out=dst, in0=dst, in1=lnp[:, gi, :], op=ALU.mult), r=["lnp", sl], w=[sl])
            S.op("pool", lambda: G.tensor_tensor(out=dst, in0=dst, in1=lnp[:, gi + 1, :], op=ALU.add), r=["lnp", sl], w=[sl])

        pending = []
        cur_e = [0]
        nload = 0
        for s in range(NSG):
            if do_B:
                load_expert(0, nload % 2)
            for t in range(TPS if do_A else 0):
                tok0 = s * SG + t * 128
                sl = t % 2
                tg = s * TPS + t
                for r_ in range(4):
                    S.dma_fn("pool", lambda: G.indirect_dma_start(out=yt[sl][:, r_ * 256:(r_ + 1) * 256], out_offset=None, in_=y_d,
                                                                  in_offset=bass.IndirectOffsetOnAxis(ap=idx[:, r_ * 32 + tg:r_ * 32 + tg + 1], axis=0)),
                             r=["idx"], w=[("yt", sl)], sem=("yt", sl))
                S.dma("sp", hpt[sl][:], hp_d[tok0:tok0 + 128, :], w=[("hp", sl)], sem=("hp", sl))
                for hb in range(2):
                    for j in range(4):
                        kc = hb * 4 + j
                        S.op("pe", lambda kc=kc, j=j, hb=hb: P.transpose(out=ps[hb][:, j * 128:(j + 1) * 128],
                                                                        in_=yt[sl][:, kc * 128:(kc + 1) * 128], identity=ident[:]),
                             r=[("yt", sl), "ident"], w=[("ps", hb)])
                S.op("act", lambda: A.copy(out=yT[sl][:, 0:4, :], in_=ps[0][:].rearrange("p (a b) -> p a b", a=4)),
                     r=[("ps", 0)], w=[("yT", sl, 0)])
                S.op("dve", lambda: V.tensor_copy(out=yT[sl][:, 4:8, :], in_=ps[1][:].rearrange("p (a b) -> p a b", a=4)),
                     r=[("ps", 1)], w=[("yT", sl, 1)])
                if stage < 2:
                    continue
                for half in range(2):
                    for kc in range(8):
                        S.op("pe", lambda kc=kc, half=half: P.matmul(ps[2 + half][:], lhsT=yT[sl][:, kc, :],
                                                                     rhs=wout[:, kc, half * 512:(half + 1) * 512],
                                                                     start=(kc == 0), stop=(kc == 7)),
                             r=[("yT", sl, kc // 4), "wout"], w=[("ps", 2 + half)])
                if stage < 3:
                    continue
                for half in range(2):
                    S.op("dve", lambda half=half: V.scalar_tensor_tensor(out=zt[sl][:, half * 512:(half + 1) * 512],
                                                                         in0=hpt[sl][:, half * 512:(half + 1) * 512], scalar=ALPHA,
                                                                         in1=ps[2 + half][:], op0=ALU.mult, op1=ALU.add),
                         r=[("hp", sl), ("ps", 2 + half)], w=[("z", sl)])
                layernorm(zt[sl][:], zt[sl][:], 0, ("z", sl))
                if stage < 4:
                    continue
                S.op("act", lambda: A.mul(out=yacc[:, t, :], in_=zt[sl][:], mul=ALPHA), r=[("z", sl)], w=[("yacc", t)])
                for hb in range(2):
                    for j in range(4):
                        kc = hb * 4 + j
                        S.op("pe", lambda kc=kc, j=j, hb=hb: P.transpose(out=ps[4 + hb][:, j * 128:(j + 1) * 128],
                                                                        in_=zt[sl][:, kc * 128:(kc + 1) * 128], identity=ident[:]),
                             r=[("z", sl), "ident"], w=[("ps", 4 + hb)])
                for hb in range(2):
                    S.op("act", lambda hb=hb: A.copy(out=hT32[sl][:, hb * 4:(hb + 1) * 4, :],
                                                     in_=ps[4 + hb][:].rearrange("p (a b) -> p a b", a=4)),
                         r=[("ps", 4 + hb)], w=[("hT32", sl, hb)])
                    S.op("dve", lambda hb=hb: V.tensor_copy(out=hT[:, hb * 4:(hb + 1) * 4, t * 128:(t + 1) * 128],
                                                            in_=ps[4 + hb][:].rearrange("p (a b) -> p a b", a=4)),
                         r=[("ps", 4 + hb)], w=[("hT", t)])
                if stage < 5:
                    continue
                for kc in range(8):
                    S.op("pe", lambda kc=kc: P.matmul(ps[6][:, 0:20], lhsT=hT32[sl][:, kc, :], rhs=wr[:, kc, :],
                                                      start=(kc == 0), stop=(kc == 7)),
                         r=[("hT32", sl, kc // 4), "wr"], w=[("ps", 6)])
                if not do_R:
                    continue
                RT = ["rt"]
                S.op("dve", lambda: V.tensor_tensor(out=lg[:], in0=ps[6][:, 0:20], in1=rb[:], op=ALU.add),
                     r=[("ps", 6), "rb"], w=RT)
                S.op("dve", lambda: V.tensor_reduce(out=r_gmax[:], in_=lg[:, 0:4], axis=AX.X, op=ALU.max), r=RT, w=RT)
                S.op("dve", lambda: V.tensor_scalar(out=r_gmask[:], in0=lg[:, 0:4], scalar1=r_gmax[:], scalar2=None,
                                                    op0=ALU.is_ge), r=RT, w=RT)
                S.op("dve", lambda: V.tensor_scalar(out=r_gt[:], in0=lg[:, 0:4], scalar1=r_gmax[:], scalar2=None,
                                                    op0=ALU.subtract), r=RT, w=RT)
                S.op("act", lambda: A.activation(out=r_gt[:], in_=r_gt[:], func=AF.Exp, accum_out=r_gsum[:]), r=RT, w=RT)
                S.op("dve", lambda: V.reciprocal(out=r_gsum[:], in_=r_gsum[:]), r=RT, w=RT)
                S.op("dve", lambda: V.tensor_scalar(out=r_gs[:], in0=r_gmask[:], scalar1=r_gsum[:], scalar2=None,
                                                    op0=ALU.mult), r=RT, w=RT)
                ev = lg[:, 4:20].rearrange("p (g j) -> p g j", g=4)
                S.op("dve", lambda: V.tensor_reduce(out=r_m1[:], in_=ev, axis=AX.X, op=ALU.max), r=RT, w=RT)
                S.op("dve", lambda: V.tensor_tensor(out=r_is1[:], in0=ev, in1=r_m1[:].unsqueeze(2).to_broadcast([128, 4, 4]),
                                                    op=ALU.is_equal), r=RT, w=RT)
                S.op("dve", lambda: V.scalar_tensor_tensor(out=r_e2[:], in0=r_is1[:], scalar=-1e30, in1=ev,
                                                           op0=ALU.mult, op1=ALU.add), r=RT, w=RT)
                S.op("dve", lambda: V.tensor_reduce(out=r_m2[:], in_=r_e2[:], axis=AX.X, op=ALU.max), r=RT, w=RT)
                S.op("dve", lambda: V.tensor_tensor(out=r_is2[:], in0=r_e2[:], in1=r_m2[:].unsqueeze(2).to_broadcast([128, 4, 4]),
                                                    op=ALU.is_equal), r=RT, w=RT)
                S.op("dve", lambda: V.tensor_tensor(out=r_w1[:], in0=r_m2[:], in1=r_m1[:], op=ALU.subtract), r=RT, w=RT)
                S.op("act", lambda: A.activation(out=r_w1[:], in_=r_w1[:], func=AF.Exp), r=RT, w=RT)
                S.op("dve", lambda: V.tensor_scalar_add(out=r_w1[:], in0=r_w1[:], scalar1=1.0), r=RT, w=RT)
                S.op("dve", lambda: V.reciprocal(out=r_w1[:], in_=r_w1[:]), r=RT, w=RT)
                S.op("dve", lambda: V.tensor_scalar(out=r_w2[:], in0=r_w1[:], scalar1=-1.0, scalar2=1.0,
                                                    op0=ALU.mult, op1=ALU.add), r=RT, w=RT)
                S.op("dve", lambda: V.tensor_tensor(out=r_w1[:], in0=r_w1[:], in1=r_gs[:], op=ALU.mult), r=RT, w=RT)
                S.op("dve", lambda: V.tensor_tensor(out=r_w2[:], in0=r_w2[:], in1=r_gs[:], op=ALU.mult), r=RT, w=RT)
                S.op("dve", lambda: V.tensor_tensor(out=r_is1[:], in0=r_is1[:], in1=r_w1[:].unsqueeze(2).to_broadcast([128, 4, 4]),
                                                    op=ALU.mult), r=RT, w=RT)
                S.op("dve", lambda: V.tensor_tensor(out=r_is2[:], in0=r_is2[:], in1=r_w2[:].unsqueeze(2).to_broadcast([128, 4, 4]),
                                                    op=ALU.mult), r=RT, w=RT)
                S.op("dve", lambda: V.tensor_tensor(out=comb[:, t, :].rearrange("p (g j) -> p g j", g=4), in0=r_is1[:], in1=r_is2[:],
                                                    op=ALU.add), r=RT, w=[("comb", t)])
            for e in range(NEXP if do_B else 0):
                slot = nload % 2
                if pending and e in (3, 6, 9, 12):
                    chunk_done(pending.pop(0))
                nload += 1
                if e + 1 < NEXP:
                    load_expert(e + 1, nload % 2)
                for g in range(GPS):
                    hs = g % 2
                    for fc in range(4):
                        pg = fc % 2
                        for kc in range(8):
                            S.op("pe", lambda kc=kc, fc=fc, pg=pg: P.matmul(ps[pg][:], lhsT=wg[slot][:, kc, fc * 128:(fc + 1) * 128],
                                                                          rhs=hT[:, kc, g * 512:(g + 1) * 512],
                                                                          start=(kc == 0), stop=(kc == 7)),
                                 r=[("wg", slot)] + [("hT", g * 4 + i) for i in range(4)], w=[("ps", pg)])
                        for kc in range(8):
                            S.op("pe", lambda kc=kc, fc=fc, pg=pg: P.matmul(ps[2 + pg][:], lhsT=wu[slot][:, kc, fc * 128:(fc + 1) * 128],
                                                                          rhs=hT[:, kc, g * 512:(g + 1) * 512],
                                                                          start=(kc == 0), stop=(kc == 7)),
                                 r=[("wu", slot)] + [("hT", g * 4 + i) for i in range(4)], w=[("ps", 2 + pg)])
                        S.op("act", lambda pg=pg: A.activation(out=sg[pg][:], in_=ps[pg][:], func=AF.Silu),
                             r=[("ps", pg)], w=[("sg", pg)])
                        S.op("dve", lambda pg=pg, fc=fc: V.tensor_tensor(out=hdn[hs][:, fc, :], in0=ps[2 + pg][:], in1=sg[pg][:], op=ALU.mult),
                             r=[("ps", 2 + pg), ("sg", pg)], w=[("hdn", hs, fc)])
                    for tt in range(4):
                        t = g * 4 + tt
                        for half in range(2):
                            pb = 4 + (tt * 2 + half) % 4
                            for fc in range(4):
                                S.op("pe", lambda fc=fc, half=half, pb=pb, tt=tt: P.matmul(ps[pb][:], lhsT=hdn[hs][:, fc, tt * 128:(tt + 1) * 128],
                                                                                       rhs=wd[slot][:, fc, half * 512:(half + 1) * 512],
                                                                                       start=(fc == 0), stop=(fc == 3)),
                                     r=[("hdn", hs, fc), ("wd", slot)], w=[("ps", pb)])
                            S.op("dve", lambda half=half, pb=pb, t=t: V.scalar_tensor_tensor(
                                out=yacc[:, t, half * 512:(half + 1) * 512], in0=ps[pb][:], scalar=comb[:, t, e:e + 1],
                                in1=yacc[:, t, half * 512:(half + 1) * 512], op0=ALU.mult, op1=ALU.add),
                                r=[("ps", pb), ("comb", t), ("yacc", t)], w=[("yacc", t)])
            for t in range(TPS):
                tok0 = s * SG + t * 128
                sl = t % 2
                src = yacc[:, t, :]
                for c in range(2):
                    S.op("dve", lambda c=c: V.bn_stats(out=st[:, c, :], in_=src[:, c * 512:(c + 1) * 512]), r=[("yacc", t)], w=["st"])
                S.op("dve", lambda: V.bn_aggr(out=mv[:], in_=st[:].rearrange("p a b -> p (a b)")), r=["st"], w=["mv"])
                S.op("dve", lambda: V.tensor_scalar_add(out=rstd[:], in0=mv[:, 1:2], scalar1=LN_EPS), r=["mv"], w=["rstd"])
                S.op("act", lambda: A.activation(out=rstd[:], in_=rstd[:], func=AF.Ln), r=["rstd"], w=["rstd"])
                S.op("act", lambda: A.activation(out=rstd[:], in_=rstd[:], func=AF.Exp, scale=-0.5), r=["rstd"], w=["rstd"])
                S.op("dve", lambda: V.tensor_scalar(out=ot[sl][:], in0=src, scalar1=mv[:, 0:1], scalar2=rstd[:],
                                                    op0=ALU.subtract, op1=ALU.mult), r=["mv", "rstd", ("yacc", t)], w=[("ot", sl)])
                S.op("pool", lambda: G.tensor_tensor(out=ot[sl][:], in0=ot[sl][:], in1=lnp[:, 2, :], op=ALU.mult), r=["lnp", ("ot", sl)], w=[("ot", sl)])
                S.op("pool", lambda: G.tensor_tensor(out=ot[sl][:], in0=ot[sl][:], in1=lnp[:, 3, :], op=ALU.add), r=["lnp", ("ot", sl)], w=[("ot", sl)])
                S.dma("sp", out_d[tok0:tok0 + 128, :], ot[sl][:], r=[("ot", sl)], sem=("ot", sl))
                if t % 2 == 1 and chunk_done:
                    pending.append(tok0 // 256)
        while pending:
            chunk_done(pending.pop(0))


def emit_gla(nc, S, ps, DD, tag, S_LEN=16384):
    D = 1024
    NT = S_LEN // 128
    h_d, wcat_d, wg2_d, bg_d, gn_d = DD["h"], DD["wcat"], DD["wg2"], DD["bg"], DD["gn"]
    id_d, tr_d, ci_d, on_d, y_d = DD["ident"], DD["trirev"], DD["cind"], DD["ones1"], DD["yout"]
    hmap = DD.get("hmap", lambda n: n)
    chunk_done = DD.get("chunk_done")
    with ExitStack() as es:
        def sb(name, shape, dt=F32):
            return es.enter_context(nc.sbuf_tensor("sb_" + tag + "_" + name, shape, dt))
        ident = sb("ident", [128, 128])
        trirev = sb("trirev", [128, 128])
        cind = sb("cind", [128, 2])
        ones1 = sb("ones1", [1, 128])
        wcat = sb("wcat", [128, 8, 784], BF16)
        wg2 = sb("wg2", [16, 128])
        bg = sb("bg", [1, 128])
        gn = sb("gn", [128, 256])
        state = sb("state", [128, 256])
        qlo = sb("qlo", [128, 128])
        qhi = sb("qhi", [128, 128])
        ht = [sb("ht%d" % i, [128, D]) for i in range(2)]
        hT = [sb("hT%d" % i, [128, 8, 128], BF16) for i in range(2)]
        lrT = sb("lrT", [16, 128])
        la = sb("la", [128, 128])
        kd = sb("kd", [128, 128])
        dec = sb("dec", [128, 2])
        kdec = sb("kdec", [128, 128], BF16)
        vbf = sb("vbf", [128, 256], BF16)
        er = sb("er", [128, 256])
        junk = sb("junk", [128, 256])
        ss = sb("ss", [128, 1])
        yo = [sb("yo%d" % i, [128, 256]) for i in range(2)]
        V, A, P, G = nc.vector, nc.scalar, nc.tensor, nc.gpsimd

        S.dma("sp", ident[:], id_d[:, :], w=["ident"], sem="ident")
        S.dma("sp", trirev[:], tr_d[:, :], w=["trirev"], sem="trirev")
        S.dma("sp", cind[:], ci_d[:, :], w=["cind"], sem="cind")
        S.dma("sp", ones1[:], on_d[:, :], w=["ones1"], sem="ones1")
        S.dma("pool", wcat[:], wcat_d.rearrange("(kc p) f -> p kc f", p=128), w=["wcat"], sem="wcat")
        S.dma("sp", wg2[:], wg2_d[:, :], w=["wg2"], sem="wg2")
        S.dma("sp", bg[:], bg_d[:, :], w=["bg"], sem="bg")
        S.dma("sp", gn[:], gn_d[0:1, :].partition_broadcast(128), w=["gn"], sem="gn")
        S.op("dve", lambda: V.memset(state[:], 0.0), w=["state"])
        S.op("dve", lambda: V.memset(qlo[:], 0.0), w=["qlo"])
        S.op("dve", lambda: V.memset(qhi[:], 0.0), w=["qhi"])

        for t in range(NT):
            tok0 = t * 128
            sl = t % 2
            if t == 0:
                S.dma("sp", ht[0][:], h_d[hmap(0):hmap(0) + 128, :], w=[("ht", 0)], sem=("ht", 0))
            if t + 1 < NT:
                tn = (t + 1) * 128
                S.dma("sp", ht[(t + 1) % 2][:], h_d[hmap(tn):hmap(tn) + 128, :], w=[("ht", (t + 1) % 2)], sem=("ht", (t + 1) % 2))
            for hb in range(2):
                for j in range(4):
                    kc = hb * 4 + j
                    S.op("pe", lambda: P.transpose(out=ps[hb][:, j * 128:(j + 1) * 128],
                                                   in_=ht[sl][:, kc * 128:(kc + 1) * 128], identity=ident[:]),
                         r=[("ht", sl), "ident"], w=[("ps", hb)])
            S.op("act", lambda: A.copy(out=hT[sl][:, 0:4, :], in_=ps[0][:].rearrange("p (a b) -> p a b", a=4)),
                 r=[("ps", 0)], w=[("hT", sl, 0)])
            S.op("dve", lambda: V.tensor_copy(out=hT[sl][:, 4:8, :], in_=ps[1][:].rearrange("p (a b) -> p a b", a=4)),
                 r=[("ps", 1)], w=[("hT", sl, 1)])
            for kc in range(8):
                S.op("pe", lambda: P.matmul(ps[2][:, 0:128], lhsT=wcat[:, kc, 0:128], rhs=hT[sl][:, kc, :],
                                            start=(kc == 0), stop=(kc == 7)),
                     r=["wcat", ("hT", sl, kc // 4)], w=[("ps", 2)])
            for kc in range(8):
                S.op("pe", lambda: P.matmul(ps[2][0:16, 128:256], lhsT=wcat[:, kc, 768:784], rhs=hT[sl][:, kc, :],
                                            start=(kc == 0), stop=(kc == 7)),
                     r=["wcat", ("hT", sl, kc // 4)], w=[("ps", 2)])
            for kc in range(8):
                S.op("pe", lambda: P.matmul(ps[3][:, 0:384], lhsT=hT[sl][:, kc, :], rhs=wcat[:, kc, 128:512],
                                            start=(kc == 0), stop=(kc == 7)),
                     r=["wcat", ("hT", sl, kc // 4)], w=[("ps", 3)])
            for kc in range(8):
                S.op("pe", lambda: P.matmul(ps[4][:, 0:256], lhsT=hT[sl][:, kc, :], rhs=wcat[:, kc, 512:768],
                                            start=(kc == 0), stop=(kc == 7)),
                     r=["wcat", ("hT", sl, kc // 4)], w=[("ps", 4)])
            S.op("act", lambda: A.mul(out=qlo[:, 0:64], in_=ps[2][:, 0:64], mul=128 ** -0.5), r=[("ps", 2)], w=["qlo"])
            S.op("act", lambda: A.mul(out=qhi[:, 64:128], in_=ps[2][:, 64:128], mul=128 ** -0.5), r=[("ps", 2)], w=["qhi"])
            S.op("dve", lambda: V.tensor_copy(out=lrT[:], in_=ps[2][0:16, 128:256]), r=[("ps", 2)], w=["lrT"])
            S.op("pe", lambda: P.matmul(ps[2][:, 0:128], lhsT=lrT[:], rhs=wg2[:], start=True, stop=False),
                 r=["lrT", "wg2"], w=[("ps", 2)])
            S.op("pe", lambda: P.matmul(ps[2][:, 0:128], lhsT=ones1[:], rhs=bg[:], start=False, stop=True),
                 r=["ones1", "bg"], w=[("ps", 2)])
            S.op("act", lambda: A.activation(out=la[:], in_=ps[2][:, 0:128], func=AF.Exp, scale=-1.0), r=[("ps", 2)], w=["la"])
            S.op("dve", lambda: V.tensor_scalar_add(out=la[:], in0=la[:], scalar1=1.0), r=["la"], w=["la"])
            S.op("act", lambda: A.activation(out=la[:], in_=la[:], func=AF.Ln), r=["la"], w=["la"])
            S.op("pe", lambda: P.matmul(ps[2][:, 128:256], lhsT=trirev[:], rhs=la[:], start=True, stop=True),
                 r=["trirev", "la"], w=[("ps", 2)])
            S.op("pe", lambda: P.matmul(ps[2][:, 256:258], lhsT=la[:], rhs=cind[:], start=True, stop=True),
                 r=["cind", "la"], w=[("ps", 2)])
            S.op("act", lambda: A.activation(out=kd[:], in_=ps[2][:, 128:256], func=AF.Exp), r=[("ps", 2)], w=["kd"])
            S.op("act", lambda: A.activation(out=dec[:], in_=ps[2][:, 256:258], func=AF.Exp), r=[("ps", 2)], w=["dec"])
            S.op("dve", lambda: V.tensor_tensor(out=kdec[:], in0=ps[3][:, 0:128], in1=kd[:], op=ALU.mult),
                 r=[("ps", 3), "kd"], w=["kdec"])
            S.op("act", lambda: A.copy(out=vbf[:], in_=ps[3][:, 128:384]), r=[("ps", 3)], w=["vbf"])
            for c in range(2):
                pb = 5 + c
                S.op("pe", lambda: P.matmul(ps[pb][:, 0:256], lhsT=kdec[c * 64:(c + 1) * 64, :], rhs=vbf[c * 64:(c + 1) * 64, :],
                                            start=True, stop=True),
                     r=["kdec", "vbf"], w=[("ps", pb)])
            for c in range(2):
                pb = 5 + c
                S.op("dve", lambda: V.scalar_tensor_tensor(out=state[:], in0=state[:], scalar=dec[:, c:c + 1], in1=ps[pb][:, 0:256],
                                                           op0=ALU.mult, op1=ALU.add),
                     r=["dec", ("ps", pb), "state"], w=["state"])
                S.op("pe", lambda: P.matmul(ps[7][:, 0:256], lhsT=(qlo if c == 0 else qhi)[:], rhs=state[:],
                                            start=(c == 0), stop=(c == 1)),
                     r=["state", "qlo" if c == 0 else "qhi"], w=[("ps", 7)])
            S.op("act", lambda: A.activation(out=er[:], in_=ps[4][:, 0:256], func=AF.Exp, scale=-1.0), r=[("ps", 4)], w=["er"])
            S.op("dve", lambda: V.tensor_scalar_add(out=er[:], in0=er[:], scalar1=1.0), r=["er"], w=["er"])
            S.op("dve", lambda: V.reciprocal(out=er[:], in_=er[:]), r=["er"], w=["er"])
            S.op("dve", lambda: V.tensor_tensor(out=er[:], in0=ps[4][:, 0:256], in1=er[:], op=ALU.mult), r=[("ps", 4), "er"], w=["er"])
            S.op("dve", lambda: V.tensor_tensor(out=er[:], in0=er[:], in1=gn[:], op=ALU.mult), r=["er", "gn"], w=["er"])
            S.op("act", lambda: A.activation(out=junk[:], in_=ps[7][:, 0:256], func=AF.Square, accum_out=ss[:]),
                 r=[("ps", 7)], w=["junk", "ss"])
            S.op("dve", lambda: V.tensor_scalar(out=ss[:], in0=ss[:], scalar1=1.0 / 256, scalar2=LN_EPS, op0=ALU.mult, op1=ALU.add),
                 r=["ss"], w=["ss"])
            S.op("act", lambda: A.activation(out=ss[:], in_=ss[:], func=AF.Ln), r=["ss"], w=["ss"])
            S.op("act", lambda: A.activation(out=ss[:], in_=ss[:], func=AF.Exp, scale=-0.5), r=["ss"], w=["ss"])
            S.op("dve", lambda: V.scalar_tensor_tensor(out=yo[sl][:], in0=ps[7][:, 0:256], scalar=ss[:], in1=er[:],
                                                       op0=ALU.mult, op1=ALU.mult),
                 r=[("ps", 7), "ss", "er"], w=[("yo", sl)])
            S.dma("sp", y_d[tok0:tok0 + 128, :], yo[sl][:], r=[("yo", sl)], sem=("yo", sl))
            if (t + 1) % 8 == 0 and chunk_done:
                chunk_done(t // 8)


def emit_att(nc, S, ps, DD, tag, S_LEN=16384):
    D = 1024
    NT = S_LEN // 128
    NG = S_LEN // 512
    h_d, hkv_d, wq_d, wk_d, wv_d = DD["h"], DD["hkv"], DD["wq"], DD["wk"], DD["wv"]
    bt_d, cfar_d, lam_d, gsub_d, cst_d = DD["biasT"], DD["cfar"], DD["lamv"], DD["gsub"], DD["cst"]
    id_d, on_d, y_d = DD["ident"], DD["ones128"], DD["yout"]
    hmap = DD.get("hmap", lambda n: n)
    chunk_done = DD.get("chunk_done")
    with ExitStack() as es:
        def sb(name, shape, dt=F32):
            return es.enter_context(nc.sbuf_tensor("sb_" + tag + "_" + name, shape, dt))
        ident = sb("ident", [128, 128])
        wq = sb("wq", [128, 8, 256], BF16)
        wk = sb("wk", [128, 8, 256], BF16)
        wv = sb("wv", [128, 8, 256], BF16)
        bt = sb("bt", [128, 4, 128])
        cfar = sb("cfar", [128, 2])
        lamv = sb("lamv", [128, 4, 64])
        lam = sb("lam", [128, 4])
        cst = sb("cst", [128, 2])
        KT = [sb("KT%d" % i, [128, S_LEN], BF16) for i in range(2)]
        VV = [sb("V%d" % i, [128, NT, 128], BF16) for i in range(2)]
        QT = [[sb("QT%d_%d" % (i, s), [128, 512], BF16) for s in range(2)] for i in range(2)]
        ht = [sb("ht%d" % i, [128, D]) for i in range(2)]
        hT = sb("hT", [128, 8, 512], BF16)
        NPT = 6
        PT = [sb("PT%d" % i, [128, 512], BF16) for i in range(NPT)]
        tmp = [sb("tmp%d" % i, [128, 128]) for i in range(2)]
        ones = sb("ones", [128, 128])
        gcol = sb("gcol", [128, 1])
        Pacc = [[sb("Pacc%d_%d" % (m, i), [128, 512]) for i in range(2)] for m in range(2)]
        rinv = [sb("rinv%d" % m, [128, 512]) for m in range(2)]
        oT = sb("oT", [128, 512])
        o2 = sb("o2", [128, 512])
        yT = [sb("yT%d" % i, [128, 512]) for i in range(2)]
        yo = [sb("yo%d" % i, [128, 4, 128]) for i in range(2)]
        V, A, P, G = nc.vector, nc.scalar, nc.tensor, nc.gpsimd

        S.dma("sp", ident[:], id_d[:, :], w=["ident"], sem="ident")
        S.dma("pool", wq[:], wq_d.rearrange("(kc p) f -> p kc f", p=128), w=["wq"], sem="wq")
        S.dma("pool", wk[:], wk_d.rearrange("(kc p) f -> p kc f", p=128), w=["wk"], sem="wk")
        S.dma("pool", wv[:], wv_d.rearrange("(kc p) f -> p kc f", p=128), w=["wv"], sem="wv")
        S.dma("sp", bt[:], bt_d.rearrange("h t k q -> k (h t) q"), w=["bt"], sem="bt")
        S.dma("sp", cfar[:], cfar_d[:, :], w=["cfar"], sem="cfar")
        S.dma("sp", cst[:], cst_d[:, :], w=["cst"], sem="cst")
        S.dma("sp", lamv[:], lam_d.partition_broadcast(128), w=["lamv"], sem="lamv")
        S.dma("sp", ones[:], on_d[:, :], w=["ones"], sem="ones")
        S.dma("sp", gcol[:], gsub_d.rearrange("o d -> d o"), w=["gcol"], sem="gcol")
        L = ["lam"]
        S.op("dve", lambda: V.tensor_tensor(out=lamv[:, 0, :], in0=lamv[:, 0, :], in1=lamv[:, 1, :], op=ALU.mult), r=["lamv"], w=["lamv"])
        S.op("dve", lambda: V.tensor_tensor(out=lamv[:, 2, :], in0=lamv[:, 2, :], in1=lamv[:, 3, :], op=ALU.mult), r=["lamv"], w=["lamv"])
        S.op("dve", lambda: V.tensor_reduce(out=lam[:, 0:1], in_=lamv[:, 0, :], axis=AX.X, op=ALU.add), r=["lamv"], w=L)
        S.op("dve", lambda: V.tensor_reduce(out=lam[:, 1:2], in_=lamv[:, 2, :], axis=AX.X, op=ALU.add), r=["lamv"], w=L)
        S.op("act", lambda: A.activation(out=lam[:, 0:2], in_=lam[:, 0:2], func=AF.Exp), r=L, w=L)
        S.op("dve", lambda: V.tensor_tensor(out=lam[:, 2:3], in0=lam[:, 1:2], in1=lam[:, 0:1], op=ALU.subtract), r=L, w=L)
        S.op("dve", lambda: V.tensor_tensor(out=lam[:, 3:4], in0=lam[:, 2:3], in1=cst[:, 0:1], op=ALU.subtract), r=L + ["cst"], w=L)
        S.op("dve", lambda: V.tensor_tensor(out=gcol[:], in0=gcol[:], in1=cst[:, 1:2], op=ALU.mult), r=["gcol", "cst"], w=["gcol"])

        def load_hT(src_d, g):
            for tt in range(4):
                tok0 = g * 512 + tt * 128
                sl = tt % 2
                S.dma("sp", ht[sl][:], src_d[hmap(tok0):hmap(tok0) + 128, :], w=[("ht", sl)], sem=("ht", sl))
                for hb in range(2):
                    for j in range(4):
                        kc = hb * 4 + j
                        S.op("pe", lambda: P.transpose(out=ps[6 + hb][:, j * 128:(j + 1) * 128],
                                                       in_=ht[sl][:, kc * 128:(kc + 1) * 128], identity=ident[:]),
                             r=[("ht", sl), "ident"], w=[("ps", 6 + hb)])
                S.op("act", lambda: A.copy(out=hT[:, 0:4, tt * 128:(tt + 1) * 128], in_=ps[6][:].rearrange("p (a b) -> p a b", a=4)),
                     r=[("ps", 6)], w=[("hT", tt)])
                S.op("dve", lambda: V.tensor_copy(out=hT[:, 4:8, tt * 128:(tt + 1) * 128], in_=ps[7][:].rearrange("p (a b) -> p a b", a=4)),
                     r=[("ps", 7)], w=[("hT", tt)])
        HTK = [("hT", i) for i in range(4)]

        for g in range(NG):
            load_hT(hkv_d, g)
            for i in range(2):
                for kc in range(8):
                    S.op("pe", lambda: P.matmul(ps[6][:], lhsT=wk[:, kc, i * 128:(i + 1) * 128], rhs=hT[:, kc, :],
                                                start=(kc == 0), stop=(kc == 7)), r=["wk"] + HTK, w=[("ps", 6)])
                S.op("act" if i == 0 else "dve",
                     (lambda: A.copy(out=KT[i][:, g * 512:(g + 1) * 512], in_=ps[6][:])) if i == 0 else
                     (lambda: V.tensor_copy(out=KT[i][:, g * 512:(g + 1) * 512], in_=ps[6][:])),
                     r=[("ps", 6)], w=[("KT", i)])
            for tt in range(4):
                for kc in range(8):
                    S.op("pe", lambda: P.matmul(ps[7][:, 0:256], lhsT=hT[:, kc, tt * 128:(tt + 1) * 128], rhs=wv[:, kc, :],
                                                start=(kc == 0), stop=(kc == 7)), r=["wv"] + HTK, w=[("ps", 7)])
                S.op("act", lambda: A.copy(out=VV[0][:, g * 4 + tt, 0:128], in_=ps[7][:, 0:128]), r=[("ps", 7)], w=[("V", 0)])
                S.op("dve", lambda: V.tensor_copy(out=VV[1][:, g * 4 + tt, 0:128], in_=ps[7][:, 128:256]), r=[("ps", 7)], w=[("V", 1)])

        state = {"pt": 0, "sb": 0}
        SBANK = [(0, 1), (4, 5)]
        RS = 7
        pinit = {}

        def emit_S(it):
            (g, hd, j, qs) = it
            c0 = max(0, j - 4 * g)
            banks = SBANK[state["sb"] % 2]
            state["sb"] += 1
            ptis = (state["pt"] % NPT, (state["pt"] + 1) % NPT)
            state["pt"] += 2
            for mp in range(2):
                lo = mp * 64
                S.op("pe", lambda: P.matmul(ps[banks[mp]][:, c0 * 128:512], lhsT=KT[hd][lo:lo + 64, j * 128:(j + 1) * 128],
                                            rhs=QT[hd][qs][lo:lo + 64, c0 * 128:512], start=True, stop=True),
                     r=[("KT", hd), ("QT", hd, qs)], w=[("ps", banks[mp])])
            for mp in range(2):
                sbk = banks[mp]
                pti = ptis[mp]
                if j < 4 * g - 1:
                    S.op("act", lambda: A.activation(out=PT[pti][:], in_=ps[sbk][:], func=AF.Exp, scale=0.125, bias=cfar[:, hd:hd + 1]),
                         r=[("ps", sbk), "cfar"], w=[("PT", pti)])
                else:
                    for c in range(c0, 4):
                        i = 4 * g + c
                        cs = slice(c * 128, (c + 1) * 128)
                        if j < i - 1:
                            S.op("act", lambda: A.activation(out=PT[pti][:, cs], in_=ps[sbk][:, cs], func=AF.Exp, scale=0.125,
                                                             bias=cfar[:, hd:hd + 1]),
                                 r=[("ps", sbk), "cfar"], w=[("PT", pti)])
                        else:
                            ty = 0 if j == i else 1
                            tb = (c + mp) % 2
                            S.op("dve", lambda: V.scalar_tensor_tensor(out=tmp[tb][:], in0=ps[sbk][:, cs], scalar=0.125,
                                                                       in1=bt[:, hd * 2 + ty, :], op0=ALU.mult, op1=ALU.add),
                                 r=[("ps", sbk), "bt"], w=[("tmp", tb)])
                            S.op("act", lambda: A.activation(out=PT[pti][:, cs], in_=tmp[tb][:], func=AF.Exp),
                                 r=[("tmp", tb)], w=[("PT", pti)])
            return (c0, ptis)

        def emit_PV(it, info):
            (g, hd, j, qs) = it
            (c0, ptis) = info
            cs = slice(c0 * 128, 512)
            for mp in range(2):
                pti = ptis[mp]
                S.op("pe", lambda: P.matmul(ps[2 + mp][:, cs], lhsT=VV[hd][:, j, :], rhs=PT[pti][:, cs],
                                            start=(j == 0), stop=(j == 4 * g + 3), skip_group_check=True),
                     r=[("PT", pti), ("V", hd)], w=[("ps", 2 + mp)])
            for mp in range(2):
                pti = ptis[mp]
                a = 1 if (2 * j + mp) % 3 == 0 else 0
                eng, E = ("dve", V) if a == 0 else ("pool", G)
                key = (g, hd, mp, a)
                if key not in pinit:
                    pinit[key] = True
                    if c0 > 0:
                        S.op(eng, lambda: E.memset(Pacc[mp][a][:, 0:c0 * 128], 0.0), w=[("Pacc", mp, a)])
                    S.op(eng, lambda: E.tensor_copy(out=Pacc[mp][a][:, cs], in_=PT[pti][:, cs]), r=[("PT", pti)], w=[("Pacc", mp, a)])
                else:
                    S.op(eng, lambda: E.tensor_tensor(out=Pacc[mp][a][:, cs], in0=Pacc[mp][a][:, cs], in1=PT[pti][:, cs], op=ALU.add),
                         r=[("PT", pti), ("Pacc", mp, a)], w=[("Pacc", mp, a)])
            if j == 4 * g + 3:
                for mp in range(2):
                    accs = [a2 for a2 in range(2) if (g, hd, mp, a2) in pinit]
                    for n2, a2 in enumerate(accs):
                        S.op("pe", lambda: P.matmul(ps[RS][:], lhsT=ones[:], rhs=Pacc[mp][a2][:], start=(n2 == 0), stop=(n2 == len(accs) - 1)),
                             r=["ones", ("Pacc", mp, a2)], w=[("ps", RS)])
                    S.op("dve", lambda: V.reciprocal(out=rinv[mp][:], in_=ps[RS][:]), r=[("ps", RS)], w=[("rinv", mp)])

        def epilogue(g, hd, ys):
            S.op("dve", lambda: V.tensor_tensor(out=oT[:], in0=ps[2][:], in1=rinv[0][:], op=ALU.mult), r=[("ps", 2), ("rinv", 0)], w=["oT"])
            S.op("dve", lambda: V.tensor_tensor(out=o2[:], in0=ps[3][:], in1=rinv[1][:], op=ALU.mult), r=[("ps", 3), ("rinv", 1)], w=["o2"])
            S.op("dve", lambda: V.scalar_tensor_tensor(out=oT[:], in0=o2[:], scalar=lam[:, 3:4], in1=oT[:], op0=ALU.mult, op1=ALU.add),
                 r=["o2", "oT", "lam"], w=["oT"])
            S.op("act", lambda: A.activation(out=o2[:], in_=oT[:], func=AF.Square), r=["oT"], w=["o2"])
            S.op("pe", lambda: P.matmul(ps[RS][:], lhsT=ones[:], rhs=o2[:], start=True, stop=True), r=["ones", "o2"], w=[("ps", RS)])
            S.op("dve", lambda: V.tensor_scalar(out=o2[:], in0=ps[RS][:], scalar1=1.0 / 128, scalar2=LN_EPS, op0=ALU.mult, op1=ALU.add),
                 r=[("ps", RS)], w=["o2"])
            S.op("act", lambda: A.activation(out=o2[:], in_=o2[:], func=AF.Ln), r=["o2"], w=["o2"])
            S.op("act", lambda: A.activation(out=o2[:], in_=o2[:], func=AF.Exp, scale=-0.5), r=["o2"], w=["o2"])
            S.op("dve", lambda: V.scalar_tensor_tensor(out=yT[hd][:], in0=oT[:], scalar=gcol[:, 0:1], in1=o2[:], op0=ALU.mult, op1=ALU.mult),
                 r=["oT", "o2", "gcol"], w=[("yT", hd)])
            for c in range(4):
                S.op("pe", lambda: P.transpose(out=ps[6][:, c * 128:(c + 1) * 128], in_=yT[hd][:, c * 128:(c + 1) * 128], identity=ident[:]),
                     r=[("yT", hd), "ident"], w=[("ps", 6)])
            S.op("act", lambda: A.copy(out=yo[hd][:], in_=ps[6][:].rearrange("p (c f) -> p c f", c=4)), r=[("ps", 6)], w=[("yo", hd)])
            S.dma("sp", y_d[g * 512:(g + 1) * 512, hd * 128:(hd + 1) * 128].rearrange("(c p) f -> p c f", p=128), yo[hd][:],
                  r=[("yo", hd)], sem=("yo", hd))

        def prep(g):
            qs = g % 2
            load_hT(h_d, g)
            for hd in range(2):
                for kc in range(8):
                    S.op("pe", lambda: P.matmul(ps[6][:], lhsT=wq[:, kc, hd * 128:(hd + 1) * 128], rhs=hT[:, kc, :],
                                                start=(kc == 0), stop=(kc == 7)), r=["wq"] + HTK, w=[("ps", 6)])
                S.op("dve", lambda: V.tensor_copy(out=QT[hd][qs][:], in_=ps[6][:]), r=[("ps", 6)], w=[("QT", hd, qs)])

        prep(0)
        for g in range(NG):
            qs = g % 2
            ys = g % 2
            if g >= 2 and g % 2 == 0 and chunk_done:
                chunk_done(g // 2 - 1)
            for hd in range(2):
                items = [(g, hd, j, qs) for j in range(4 * g + 4)]
                info = emit_S(items[0])
                for n in range(len(items)):
                    nxt = emit_S(items[n + 1]) if n + 1 < len(items) else None
                    emit_PV(items[n], info)
                    info = nxt
                    if hd == 1 and n == len(items) // 2 and g + 1 < NG:
                        prep(g + 1)
                epilogue(g, hd, ys)
        if chunk_done:
            chunk_done(NG // 2 - 1)


def build_fused():
    nc = bass.Bass("TRN2", target_bir_lowering=False)
    SL, D = 16384, 1024
    ext = lambda name, shape: nc.dram_tensor(name, shape, F32, kind="ExternalInput").ap()
    loc = lambda name, shape: nc.dram_tensor(name, shape, F32).ap()
    xb = ext("xb", [SL, D])
    xs = ext("xs", [4096, D])
    idx_d = nc.dram_tensor("idx", [128, 128], mybir.dt.uint32, kind="ExternalInput").ap()
    g_wcat, g_wg2, g_bg, g_gn = ext("g_wcat", [2, D, 784]), ext("g_wg2", [2, 16, 128]), ext("g_bg", [2, 1, 128]), ext("g_gn", [2, 1, 256])
    a_wq, a_wk, a_wv = ext("a_wq", [2, D, 256]), ext("a_wk", [D, 256]), ext("a_wv", [D, 256])
    a_bt, a_cfar, a_lamv = ext("a_biasT", [2, 2, 128, 128]), ext("a_cfar", [128, 2]), ext("a_lamv", [2, 4, 64])
    a_gsub, a_cst = ext("a_gsub", [2, 1, 128]), ext("a_cst", [2, 128, 2])
    p_wout, p_lnp, p_wr, p_rb = ext("p_wout", [4, D, D]), ext("p_lnp", [4, 4, D]), ext("p_wr", [4, D, 20]), ext("p_rb", [4, 1, 20])
    p_wg, p_wu, p_wd = ext("p_wg", [4, 16, D, 512]), ext("p_wu", [4, 16, D, 512]), ext("p_wd", [4, 16, 512, D])
    ident, trirev, cind = ext("ident", [128, 128]), ext("trirev", [128, 128]), ext("cind", [128, 2])
    ones1, ones128 = ext("ones1", [1, 128]), ext("ones128", [128, 128])
    out = nc.dram_tensor("out", [4096, D], F32, kind="ExternalOutput").ap()
    yloc, yg = loc("yloc", [SL, 256]), loc("yg", [4 * SL, 256])
    hloc = [loc("hloc%d" % i, [4096, D]) for i in range(3)]
    hg0, hkvg, hg2 = loc("hg0", [SL, D]), loc("hkvg", [SL, D]), loc("hg2", [SL, D])
    GROUPS = [[0, 1, 2, 3], [4, 5, 6, 7]]

    def hperm(n):
        r_, w_ = n // 4096, n % 4096
        return (w_ // 256) * 1024 + r_ * 256 + (w_ % 256)

    def gather_y(S):
        S.coll_multi("AllGather", GROUPS, [(yloc[i * 1024:(i + 1) * 1024, :], yg[i * 4096:(i + 1) * 4096, :]) for i in range(16)])

    def gather_h(S, src, dst):
        S.coll_multi("AllGather", GROUPS, [(src[i * 256:(i + 1) * 256, :], dst[i * 1024:(i + 1) * 1024, :]) for i in range(16)])
    with ExitStack() as es:
        S = Sched(nc, es)
        ps = [es.enter_context(nc.psum_tensor("ps%d" % i, [128, 512], F32)) for i in range(8)]

        def ydone(i):
            S.coll_async("AllGather", GROUPS, yloc[i * 1024:(i + 1) * 1024, :], yg[i * 4096:(i + 1) * 4096, :], [("yo", 0), ("yo", 1)])

        def post(layer, ysrc, hp, dst, gdst=None):
            hdone = None
            if gdst is not None:
                hdone = lambda i: S.coll_async("AllGather", GROUPS, dst[i * 256:(i + 1) * 256, :], gdst[i * 1024:(i + 1) * 1024, :],
                                               [("ot", 0), ("ot", 1)])
            emit_post(nc, S, ps, {"y": ysrc[:, :], "idx": idx_d[:, :], "hp": hp, "chunk_done": hdone, "wout": p_wout[layer], "lnp": p_lnp[layer], "wr": p_wr[layer], "rb": p_rb[layer],
                                  "wg": p_wg[layer], "wu": p_wu[layer], "wd": p_wd[layer], "ident": ident, "out": dst},
                      "p%d" % layer)

        def gla(layer, h):
            emit_gla(nc, S, ps, {"h": h, "chunk_done": ydone, "hmap": (hperm if layer > 0 else (lambda n: n)), "wcat": g_wcat[layer], "wg2": g_wg2[layer], "bg": g_bg[layer], "gn": g_gn[layer],
                                 "ident": ident, "trirev": trirev, "cind": cind, "ones1": ones1, "yout": yloc}, "g%d" % layer)

        def att(j, h):
            emit_att(nc, S, ps, {"h": h, "hkv": hkvg, "chunk_done": ydone, "hmap": hperm, "wq": a_wq[j], "wk": a_wk, "wv": a_wv, "biasT": a_bt, "cfar": a_cfar,
                                 "lamv": a_lamv[j], "gsub": a_gsub[j], "cst": a_cst[j], "ident": ident, "ones128": ones128,
                                 "yout": yloc}, "a%d" % j)

        import os
        LEVEL = int(os.environ.get("FUSED_LEVEL", "99"))
        steps = [
            lambda: gla(0, xb), lambda: S.barrier(), lambda: post(0, yg, xs, hloc[0], hg0), lambda: S.barrier(),
            lambda: gla(1, hg0), lambda: S.barrier(), lambda: post(1, yg, hloc[0], hloc[1], hkvg), lambda: S.barrier(),
            lambda: att(0, hkvg), lambda: S.barrier(), lambda: post(2, yg, hloc[1], hloc[2], hg2), lambda: S.barrier(),
            lambda: att(1, hg2), lambda: S.barrier(), lambda: post(3, yg, hloc[2], out),
        ]
        for i_, st_ in enumerate(steps):
            if i_ >= LEVEL:
                break
            st_()
        S.barrier()
    return nc


_NC = {}


def kernel(x, a_w_in, a_w_gate2, a_b_gate, a_g_norm, a_w_out, kv_w, b_w_q, b_lam_q1, b_lam_k1,
           b_lam_q2, b_lam_k2, b_g_sub, b_w_out, rel_table, moe_w_group, moe_b_group, moe_w_router,
           moe_b_router, moe_w_gate, moe_w_up, moe_w_down, ln_g, ln_b):
    f32 = np.float32
    A = lambda a: np.ascontiguousarray(np.asarray(a, dtype=f32))
    x = A(x)
    B, S_, D = x.shape
    if "nc" not in _NC:
        _NC["nc"] = build_fused()
    nc = _NC["nc"]
    w_in = A(a_w_in)
    kvw = A(kv_w)
    wqf = A(b_w_q)
    rt = A(rel_table)
    gconst = gla_consts()
    p_wout = A(np.stack([a_w_out[0], a_w_out[1], b_w_out[0], b_w_out[1]]))
    p_lnp = A(np.stack([np.stack([ln_g[l, 0], ln_b[l, 0], ln_g[l, 1], ln_b[l, 1]]) for l in range(4)]))
    p_wr = A(np.concatenate([moe_w_group, moe_w_router], axis=2))
    p_rb = A(np.concatenate([np.asarray(moe_b_group).reshape(4, -1), np.asarray(moe_b_router).reshape(4, -1)], axis=1)[:, None, :])
    p_wg, p_wu, p_wd = A(moe_w_gate), A(moe_w_up), A(moe_w_down)
    linits = [0.8 - 0.6 * math.exp(-0.3 * layer) for layer in (2, 3)]
    a_cst = A(np.stack([np.broadcast_to(np.array([[li, 1.0 - li]], f32), (128, 2)) for li in linits]))
    a_lamv = A(np.stack([np.stack([b_lam_q1[j], b_lam_k1[j], b_lam_q2[j], b_lam_k2[j]]) for j in range(2)]))
    a_gsub = A(np.asarray(b_g_sub)[:, None, :])
    in_maps = []
    for c in range(8):
        b, r = c // 4, c % 4
        hd = r
        g_wcat = np.stack([np.concatenate([w_in[l][:, hd * 128:(hd + 1) * 128], w_in[l][:, 512 + hd * 128:512 + (hd + 1) * 128],
                                           w_in[l][:, 1024 + hd * 256:1024 + (hd + 1) * 256], w_in[l][:, 2048 + hd * 256:2048 + (hd + 1) * 256],
                                           w_in[l][:, 3072:3088]], axis=1) for l in range(2)])
        heads = [2 * r, 2 * r + 1]
        a_wq = np.stack([np.concatenate([wqf[j][:, hh * 64:(hh + 1) * 64] if m_ == 0 else wqf[j][:, 512 + hh * 64:512 + (hh + 1) * 64]
                                         for hh in heads for m_ in range(2)], axis=1) for j in range(2)])
        a_wk = np.concatenate([kvw[:, hh * 64:(hh + 1) * 64] if m_ == 0 else kvw[:, 512 + hh * 64:512 + (hh + 1) * 64]
                               for hh in heads for m_ in range(2)], axis=1)
        a_wv = np.concatenate([kvw[:, 1024 + hh * 128:1024 + (hh + 1) * 128] for hh in heads], axis=1)
        idxv = np.zeros((128, 128), np.uint32)
        for r_ in range(4):
            for t_ in range(32):
                n_ = r * 4096 + t_ * 128
                idxv[:, r_ * 32 + t_] = (n_ // 1024) * 4096 + r_ * 1024 + (n_ % 1024) + np.arange(128)
        m = {"xb": x[b], "xs": np.ascontiguousarray(x[b, r * 4096:(r + 1) * 4096]), "idx": idxv, "g_wcat": A(g_wcat), "g_wg2": A(np.asarray(a_w_gate2)[:, :, hd * 128:(hd + 1) * 128]),
             "g_bg": A(np.asarray(a_b_gate)[:, None, hd * 128:(hd + 1) * 128]), "g_gn": A(np.asarray(a_g_norm)[:, None, hd * 256:(hd + 1) * 256]),
             "a_wq": A(a_wq), "a_wk": A(a_wk), "a_wv": A(a_wv), "a_biasT": att_bias_tiles(rt, heads),
             "a_cfar": A(np.broadcast_to(rt[15, heads][None, :], (128, 2))), "a_lamv": a_lamv, "a_gsub": a_gsub, "a_cst": a_cst,
             "p_wout": p_wout, "p_lnp": p_lnp, "p_wr": p_wr, "p_rb": p_rb, "p_wg": p_wg, "p_wu": p_wu, "p_wd": p_wd,
             "ident": gconst["ident"], "trirev": gconst["trirev"], "cind": gconst["cind"], "ones1": gconst["ones1"],
             "ones128": np.ones((128, 128), f32)}
        in_maps.append(m)
    res = run_bass_kernel_spmd(nc, in_maps, core_ids=list(range(8)))
    return np.concatenate([res.results[c]["out"] for c in range(8)], axis=0).reshape(B, S_, D)
```

```python
from contextlib import ExitStack
import math
import numpy as np
import concourse.bass as bass
import concourse.mybir as mybir
from concourse.bass_utils import run_bass_kernel_spmd

F32 = mybir.dt.float32
BF16 = mybir.dt.bfloat16
AF = mybir.ActivationFunctionType
ALU = mybir.AluOpType
AX = mybir.AxisListType


class Sched:
    ENG = ("pe", "act", "dve", "pool", "sp")

    def __init__(self, nc, es):
        self.nc = nc
        self.es = es
        self.eng = {"pe": nc.tensor, "act": nc.scalar, "dve": nc.vector, "pool": nc.gpsimd, "sp": nc.sync}
        self.sem = {e: es.enter_context(nc.semaphore("s_" + e)) for e in self.ENG}
        self.cnt = {e: 0 for e in self.ENG}
        self.seen = {e: {} for e in self.ENG}
        self.snaps = {e: [None] for e in self.ENG}
        self.dsem = {}
        self.dcnt = {}
        self.lastw = {}
        self.readers = {}
        self.nwait = 0
        self.ninst = 0

    def _deps(self, r, w):
        deps = []
        for k in r:
            t = self.lastw.get(k)
            if t is not None:
                deps.append(t)
        for k in w:
            t = self.lastw.get(k)
            if t is not None:
                deps.append(t)
            deps.extend(self.readers.get(k, ()))
        return deps

    def _wait(self, e, deps, skip_dma_sem=None):
        seen = self.seen[e]
        need = {}
        for (src, val) in deps:
            if src == e and e == "pe":
                continue
            if skip_dma_sem is not None and src == skip_dma_sem:
                continue
            if seen.get(src, 0) >= val:
                continue
            if need.get(src, 0) < val:
                need[src] = val
        if not need:
            return
        seen = dict(seen)
        for src, val in need.items():
            if isinstance(src, tuple):
                self.eng[e].wait_ge(self.dsem[src[1]], val)
            else:
                self.eng[e].wait_ge(self.sem[src], val)
                snap = self.snaps[src][val]
                if snap:
                    for s2, v2 in snap.items():
                        if seen.get(s2, 0) < v2:
                            seen[s2] = v2
            if seen.get(src, 0) < val:
                seen[src] = val
            self.nwait += 1
        self.seen[e] = seen

    def _commit(self, tok, r, w):
        for k in w:
            self.lastw[k] = tok
            self.readers[k] = []
        for k in r:
            self.readers.setdefault(k, []).append(tok)

    def op(self, e, fn, r=(), w=()):
        px = [k for k in r if isinstance(k, tuple) and k[0] == "ps"]
        if px:
            r = [k for k in r if k not in px]
            w = list(w) + px
        self._wait(e, self._deps(r, w))
        ins = fn()
        self.cnt[e] += 1
        ins.then_inc(self.sem[e], 1)
        self.snaps[e].append(self.seen[e])
        self._commit((e, self.cnt[e]), r, w)
        self.ninst += 1
        return ins

    def dma(self, q, out, in_, r=(), w=(), sem=None):
        assert sem is not None
        if sem not in self.dsem:
            self.dsem[sem] = self.es.enter_context(self.nc.semaphore("d_%d" % len(self.dsem)))
            self.dcnt[sem] = 0
        self._wait(q, self._deps(r, w), skip_dma_sem=("dma", sem))
        ins = self.eng[q].dma_start(out=out, in_=in_)
        self.dcnt[sem] += 16
        ins.then_inc(self.dsem[sem], 16)
        self._commit((("dma", sem), self.dcnt[sem]), r, w)
        self.ninst += 1
        return ins

    def finish(self, keys):
        deps = []
        for k in keys:
            t = self.lastw.get(k)
            if t is not None:
                deps.append(t)
            deps.extend(self.readers.get(k, ()))
        self._wait("sp", deps)


def _sched_barrier(self):
    for e in self.ENG:
        deps = [(f, self.cnt[f]) for f in self.ENG if self.cnt[f] > 0]
        deps += [(("dma", k), v) for k, v in self.dcnt.items() if v > 0]
        self._wait(e, deps)
    self.lastw = {}
    self.readers = {}


def _sched_coll(self, kind, groups, in_ap, out_ap):
    self.barrier()
    if "cc" not in self.dsem:
        self.dsem["cc"] = self.es.enter_context(self.nc.semaphore("d_cc"))
        self.dcnt["cc"] = 0
    ins = self.nc.gpsimd.collective_compute(kind, ALU.bypass, replica_groups=groups, ins=[in_ap], outs=[out_ap])
    self.dcnt["cc"] += 1
    ins.then_inc(self.dsem["cc"])
    self.ninst += 1
    self.barrier()


Sched.barrier = _sched_barrier
Sched.coll = _sched_coll


def _sched_dma_fn(self, q, fn, r=(), w=(), sem=None):
    if sem not in self.dsem:
        self.dsem[sem] = self.es.enter_context(self.nc.semaphore("d_%d" % len(self.dsem)))
        self.dcnt[sem] = 0
    self._wait(q, self._deps(r, w), skip_dma_sem=("dma", sem))
    ins = fn()
    self.dcnt[sem] += 16
    ins.then_inc(self.dsem[sem], 16)
    self._commit((("dma", sem), self.dcnt[sem]), r, w)
    self.ninst += 1
    return ins


Sched.dma_fn = _sched_dma_fn


def _sched_coll_multi(self, kind, groups, pairs):
    self.barrier()
    if "cc" not in self.dsem:
        self.dsem["cc"] = self.es.enter_context(self.nc.semaphore("d_cc"))
        self.dcnt["cc"] = 0
    for (in_ap, out_ap) in pairs:
        ins = self.nc.gpsimd.collective_compute(kind, ALU.bypass, replica_groups=groups, ins=[in_ap], outs=[out_ap])
        self.dcnt["cc"] += 1
        ins.then_inc(self.dsem["cc"])
        self.ninst += 1
        self.nc.gpsimd.wait_ge(self.dsem["cc"], self.dcnt["cc"])
    self.barrier()


Sched.coll_multi = _sched_coll_multi


def _sched_coll_async(self, kind, groups, in_ap, out_ap, wait_sems):
    if "cc" not in self.dsem:
        self.dsem["cc"] = self.es.enter_context(self.nc.semaphore("d_cc"))
        self.dcnt["cc"] = 0
    deps = [(("dma", k), self.dcnt[k]) for k in wait_sems if self.dcnt.get(k, 0) > 0]
    if self.dcnt["cc"] > 0:
        deps.append((("dma", "cc"), self.dcnt["cc"]))
    self._wait("pool", deps)
    ins = self.nc.gpsimd.collective_compute(kind, ALU.bypass, replica_groups=groups, ins=[in_ap], outs=[out_ap], dma_qos="P3")
    self.dcnt["cc"] += 1
    ins.then_inc(self.dsem["cc"])
    self.ninst += 1


Sched.coll_async = _sched_coll_async

ALPHA = (2.0 * 4) ** 0.25
LN_EPS = 1e-5
TAU = 16.0
NEGB = -30000.0

LN_EPS = 1e-5
TAU = 16.0


def gla_consts():
    s = np.arange(128)
    same = (s[:, None] // 64) == (s[None, :] // 64)
    trirev = ((s[:, None] > s[None, :]) & same).astype(np.float32) * (-1.0 / TAU)
    cind = np.zeros((128, 2), np.float32)
    cind[:64, 0] = -1.0 / TAU
    cind[64:, 1] = -1.0 / TAU
    return {"ident": np.eye(128, dtype=np.float32), "trirev": trirev, "cind": cind,
            "ones1": np.ones((1, 128), np.float32)}


LN_EPS = 1e-5
NEGB = -30000.0


def rel_bucket_np(rel):
    nb = 16
    max_exact = 8
    base = np.where(rel > 0, nb, 0)
    n = np.abs(rel)
    large = max_exact + (np.log(np.maximum(n, 1).astype(np.float32) / np.float32(max_exact))
                         / np.float32(math.log(128 / max_exact)) * np.float32(nb - max_exact)).astype(np.int32)
    large = np.minimum(large, nb - 1)
    return base + np.where(n < max_exact, n, large)


def att_bias_tiles(rel_table, heads):
    kl = np.arange(128)[:, None]
    ql = np.arange(128)[None, :]
    out = np.zeros((len(heads), 2, 128, 128), np.float32)
    bd = rel_bucket_np(kl - ql)
    bp = rel_bucket_np(kl - ql - 128)
    vis = (kl // 64) <= (ql // 64)
    for i, h in enumerate(heads):
        out[i, 0] = np.where(vis, rel_table[bd, h], np.float32(NEGB))
        out[i, 1] = rel_table[bp, h]
    return out


def emit_post(nc, S, ps, D, tag, NTOK=4096, SG=1024, NEXP=16, do_A=True, do_B=True, do_R=True, stage=9):
    D_ = 1024
    D, DD = D_, D
    FF = 512
    y_d, hp_d, wout_d, lnp_d, wr_d, rb_d = DD["y"], DD["hp"], DD["wout"], DD["lnp"], DD["wr"], DD["rb"]
    wg_d, wu_d, wd_d, id_d, out_d = DD["wg"], DD["wu"], DD["wd"], DD["ident"], DD["out"]
    chunk_done = DD.get("chunk_done")
    NSG = NTOK // SG
    TPS = SG // 128
    GPS = SG // 512
    with ExitStack() as es:
        def sb(name, shape, dt=F32):
            return es.enter_context(nc.sbuf_tensor("sb_" + tag + "_" + name, shape, dt))
        ident = sb("ident", [128, 128])
        idx = sb("idx", [128, 128], mybir.dt.uint32)
        wout = sb("wout", [128, 8, D], BF16)
        wr = sb("wr", [128, 8, 20])
        rb = sb("rb", [128, 20])
        lnp = sb("lnp", [128, 4, D])
        hT = sb("hT", [128, 8, SG], BF16)
        yacc = sb("yacc", [128, TPS, D])
        comb = sb("comb", [128, TPS, 16])
        wg = [sb("wg%d" % i, [128, 8, FF], BF16) for i in range(2)]
        wu = [sb("wu%d" % i, [128, 8, FF], BF16) for i in range(2)]
        wd = [sb("wd%d" % i, [128, 4, D], BF16) for i in range(2)]
        yt = [sb("yt%d" % i, [128, D]) for i in range(2)]
        hpt = [sb("hpt%d" % i, [128, D]) for i in range(2)]
        yT = [sb("yT%d" % i, [128, 8, 128], BF16) for i in range(2)]
        zt = [sb("z%d" % i, [128, D]) for i in range(2)]
        hT32 = [sb("hT32_%d" % i, [128, 8, 128]) for i in range(2)]
        sg = [sb("sg%d" % i, [128, 512]) for i in range(2)]
        hdn = [sb("hdn%d" % i, [128, 4, 512], BF16) for i in range(2)]
        ot = [sb("ot%d" % i, [128, D]) for i in range(2)]
        st = sb("stats", [128, 2, 6])
        mv = sb("mv", [128, 2])
        rstd = sb("rstd", [128, 1])
        lg = sb("lg", [128, 20])
        r_gmax = sb("r_gmax", [128, 1])
        r_gmask = sb("r_gmask", [128, 4])
        r_gt = sb("r_gt", [128, 4])
        r_gsum = sb("r_gsum", [128, 1])
        r_m1 = sb("r_m1", [128, 4])
        r_m2 = sb("r_m2", [128, 4])
        r_is1 = sb("r_is1", [128, 4, 4])
        r_is2 = sb("r_is2", [128, 4, 4])
        r_e2 = sb("r_e2", [128, 4, 4])
        r_w1 = sb("r_w1", [128, 4])
        r_w2 = sb("r_w2", [128, 4])
        r_gs = sb("r_gs", [128, 4])

        V, A, P, G = nc.vector, nc.scalar, nc.tensor, nc.gpsimd

        S.dma("sp", ident[:], id_d[:, :], w=["ident"], sem="ident")
        S.dma("sp", idx[:], DD["idx"], w=["idx"], sem="idx")
        S.dma("pool", wout[:], wout_d.rearrange("(kc p) f -> p kc f", p=128), w=["wout"], sem="wout")
        S.dma("sp", wr[:], wr_d.rearrange("(kc p) f -> p kc f", p=128), w=["wr"], sem="wr")
        S.dma("sp", rb[:], rb_d[0:1, :].partition_broadcast(128), w=["rb"], sem="rb")
        S.dma("sp", lnp[:], lnp_d.partition_broadcast(128), w=["lnp"], sem="lnp")

        def load_expert(e, slot):
            S.dma("pool", wg[slot][:], wg_d[e].rearrange("(kc p) f -> p kc f", p=128), w=[("wg", slot)], sem=("wg", slot))
            S.dma("pool", wu[slot][:], wu_d[e].rearrange("(kc p) f -> p kc f", p=128), w=[("wu", slot)], sem=("wu", slot))
            S.dma("pool", wd[slot][:], wd_d[e].rearrange("(kc p) f -> p kc f", p=128), w=[("wd", slot)], sem=("wd", slot))

        def layernorm(src, dst, gi, sl):
            for c in range(2):
                S.op("dve", lambda c=c: V.bn_stats(out=st[:, c, :], in_=src[:, c * 512:(c + 1) * 512]), r=[sl], w=["st"])
            S.op("dve", lambda: V.bn_aggr(out=mv[:], in_=st[:].rearrange("p a b -> p (a b)")), r=["st"], w=["mv"])
            S.op("dve", lambda: V.tensor_scalar_add(out=rstd[:], in0=mv[:, 1:2], scalar1=LN_EPS), r=["mv"], w=["rstd"])
            S.op("act", lambda: A.activation(out=rstd[:], in_=rstd[:], func=AF.Ln), r=["rstd"], w=["rstd"])
            S.op("act", lambda: A.activation(out=rstd[:], in_=rstd[:], func=AF.Exp, scale=-0.5), r=["rstd"], w=["rstd"])
            S.op("dve", lambda: V.tensor_scalar(out=dst, in0=src, scalar1=mv[:, 0:1], scalar2=rstd[:],
                                                op0=ALU.subtract, op1=ALU.mult), r=["mv", "rstd", sl], w=[sl])
            S.op("pool", lambda: G.tensor_tensor(out=dst, in0=dst, in1=lnp[:, gi, :], op=ALU.mult), r=["lnp", sl], w=[sl])
            S.op("pool", lambda: G.tensor_tensor(out=dst, in0=dst, in1=lnp[:, gi + 1, :], op=ALU.add), r=["lnp", sl], w=[sl])

        pending = []
        cur_e = [0]
        nload = 0
        for s in range(NSG):
            if do_B:
                load_expert(0, nload % 2)
            for t in range(TPS if do_A else 0):
                tok0 = s * SG + t * 128
                sl = t % 2
                tg = s * TPS + t
                for r_ in range(4):
                    S.dma_fn("pool", lambda: G.indirect_dma_start(out=yt[sl][:, r_ * 256:(r_ + 1) * 256], out_offset=None, in_=y_d,
                                                                  in_offset=bass.IndirectOffsetOnAxis(ap=idx[:, r_ * 32 + tg:r_ * 32 + tg + 1], axis=0)),
                             r=["idx"], w=[("yt", sl)], sem=("yt", sl))
                S.dma("sp", hpt[sl][:], hp_d[tok0:tok0 + 128, :], w=[("hp", sl)], sem=("hp", sl))
                for hb in range(2):
                    for j in range(4):
                        kc = hb * 4 + j
                        S.op("pe", lambda kc=kc, j=j, hb=hb: P.transpose(out=ps[hb][:, j * 128:(j + 1) * 128],
                                                                        in_=yt[sl][:, kc * 128:(kc + 1) * 128], identity=ident[:]),
                             r=[("yt", sl), "ident"], w=[("ps", hb)])
                S.op("act", lambda: A.copy(out=yT[sl][:, 0:4, :], in_=ps[0][:].rearrange("p (a b) -> p a b", a=4)),
                     r=[("ps", 0)], w=[("yT", sl, 0)])
                S.op("dve", lambda: V.tensor_copy(out=yT[sl][:, 4:8, :], in_=ps[1][:].rearrange("p (a b) -> p a b", a=4)),
                     r=[("ps", 1)], w=[("yT", sl, 1)])
                if stage < 2:
                    continue
                for half in range(2):
                    for kc in range(8):
                        S.op("pe", lambda kc=kc, half=half: P.matmul(ps[2 + half][:], lhsT=yT[sl][:, kc, :],
                                                                     rhs=wout[:, kc, half * 512:(half + 1) * 512],
                                                                     start=(kc == 0), stop=(kc == 7)),
                             r=[("yT", sl, kc // 4), "wout"], w=[("ps", 2 + half)])
                if stage < 3:
                    continue
                for half in range(2):
                    S.op("dve", lambda half=half: V.scalar_tensor_tensor(out=zt[sl][:, half * 512:(half + 1) * 512],
                                                                         in0=hpt[sl][:, half * 512:(half + 1) * 512], scalar=ALPHA,
                                                                         in1=ps[2 + half][:], op0=ALU.mult, op1=ALU.add),
                         r=[("hp", sl), ("ps", 2 + half)], w=[("z", sl)])
                layernorm(zt[sl][:], zt[sl][:], 0, ("z", sl))
                if stage < 4:
                    continue
                S.op("act", lambda: A.mul(out=yacc[:, t, :], in_=zt[sl][:], mul=ALPHA), r=[("z", sl)], w=[("yacc", t)])
                for hb in range(2):
                    for j in range(4):
                        kc = hb * 4 + j
                        S.op("pe", lambda kc=kc, j=j, hb=hb: P.transpose(out=ps[4 + hb][:, j * 128:(j + 1) * 128],
                                                                        in_=zt[sl][:, kc * 128:(kc + 1) * 128], identity=ident[:]),
                             r=[("z", sl), "ident"], w=[("ps", 4 + hb)])
                for hb in range(2):
                    S.op("act", lambda hb=hb: A.copy(out=hT32[sl][:, hb * 4:(hb + 1) * 4, :],
                                                     in_=ps[4 + hb][:].rearrange("p (a b) -> p a b", a=4)),
                         r=[("ps", 4 + hb)], w=[("hT32", sl, hb)])
                    S.op("dve", lambda hb=hb: V.tensor_copy(out=hT[:, hb * 4:(hb + 1) * 4, t * 128:(t + 1) * 128],
                                                            in_=ps[4 + hb][:].rearrange("p (a b) -> p a b", a=4)),
                         r=[("ps", 4 + hb)], w=[("hT", t)])
                if stage < 5:
                    continue
                for kc in range(8):
                    S.op("pe", lambda kc=kc: P.matmul(ps[6][:, 0:20], lhsT=hT32[sl][:, kc, :], rhs=wr[:, kc, :],
                                                      start=(kc == 0), stop=(kc == 7)),
                         r=[("hT32", sl, kc // 4), "wr"], w=[("ps", 6)])
                if not do_R:
                    continue
                RT = ["rt"]
                S.op("dve", lambda: V.tensor_tensor(out=lg[:], in0=ps[6][:, 0:20], in1=rb[:], op=ALU.add),
                     r=[("ps", 6), "rb"], w=RT)
                S.op("dve", lambda: V.tensor_reduce(out=r_gmax[:], in_=lg[:, 0:4], axis=AX.X, op=ALU.max), r=RT, w=RT)
                S.op("dve", lambda: V.tensor_scalar(out=r_gmask[:], in0=lg[:, 0:4], scalar1=r_gmax[:], scalar2=None,
                                                    op0=ALU.is_ge), r=RT, w=RT)
                S.op("dve", lambda: V.tensor_scalar(out=r_gt[:], in0=lg[:, 0:4], scalar1=r_gmax[:], scalar2=None,
                                                    op0=ALU.subtract), r=RT, w=RT)
                S.op("act", lambda: A.activation(out=r_gt[:], in_=r_gt[:], func=AF.Exp, accum_out=r_gsum[:]), r=RT, w=RT)
                S.op("dve", lambda: V.reciprocal(out=r_gsum[:], in_=r_gsum[:]), r=RT, w=RT)
                S.op("dve", lambda: V.tensor_scalar(out=r_gs[:], in0=r_gmask[:], scalar1=r_gsum[:], scalar2=None,
                                                    op0=ALU.mult), r=RT, w=RT)
                ev = lg[:, 4:20].rearrange("p (g j) -> p g j", g=4)
                S.op("dve", lambda: V.tensor_reduce(out=r_m1[:], in_=ev, axis=AX.X, op=ALU.max), r=RT, w=RT)
                S.op("dve", lambda: V.tensor_tensor(out=r_is1[:], in0=ev, in1=r_m1[:].unsqueeze(2).to_broadcast([128, 4, 4]),
                                                    op=ALU.is_equal), r=RT, w=RT)
                S.op("dve", lambda: V.scalar_tensor_tensor(out=r_e2[:], in0=r_is1[:], scalar=-1e30, in1=ev,
                                                           op0=ALU.mult, op1=ALU.add), r=RT, w=RT)
                S.op("dve", lambda: V.tensor_reduce(out=r_m2[:], in_=r_e2[:], axis=AX.X, op=ALU.max), r=RT, w=RT)
                S.op("dve", lambda: V.tensor_tensor(out=r_is2[:], in0=r_e2[:], in1=r_m2[:].unsqueeze(2).to_broadcast([128, 4, 4]),
                                                    op=ALU.is_equal), r=RT, w=RT)
                S.op("dve", lambda: V.tensor_tensor(out=r_w1[:], in0=r_m2[:], in1=r_m1[:], op=ALU.subtract), r=RT, w=RT)
                S.op("act", lambda: A.activation(out=r_w1[:], in_=r_w1[:], func=AF.Exp), r=RT, w=RT)
                S.op("dve", lambda: V.tensor_scalar_add(out=r_w1[:], in0=r_w1[:], scalar1=1.0), r=RT, w=RT)
                S.op("dve", lambda: V.reciprocal(out=r_w1[:], in_=r_w1[:]), r=RT, w=RT)
                S.op("dve", lambda: V.tensor_scalar(out=r_w2[:], in0=r_w1[:], scalar1=-1.0, scalar2=1.0,
                                                    op0=ALU.mult, op1=ALU.add), r=RT, w=RT)
                S.op("dve", lambda: V.tensor_tensor(out=r_w1[:], in0=r_w1[:], in1=r_gs[:], op=ALU.mult), r=RT, w=RT)
                S.op("dve", lambda: V.tensor_tensor(out=r_w2[:], in0=r_w2[:], in1=r_gs[:], op=ALU.mult), r=RT, w=RT)
                S.op("dve", lambda: V.tensor_tensor(out=r_is1[:], in0=r_is1[:], in1=r_w1[:].unsqueeze(2).to_broadcast([128, 4, 4]),
                                                    op=ALU.mult), r=RT, w=RT)
                S.op("dve", lambda: V.tensor_tensor(out=r_is2[:], in0=r_is2[:], in1=r_w2[:].unsqueeze(2).to_broadcast([128, 4, 4]),
                                                    op=ALU.mult), r=RT, w=RT)
                S.op("dve", lambda: V.tensor_tensor(out=comb[:, t, :].rearrange("p (g j) -> p g j", g=4), in0=r_is1[:], in1=r_is2[:],
                                                    op=ALU.add), r=RT, w=[("comb", t)])
            for e in range(NEXP if do_B else 0):
                slot = nload % 2
                if pending and e in (3, 6, 9, 12):
                    chunk_done(pending.pop(0))
                nload += 1
                if e + 1 < NEXP:
                    load_expert(e + 1, nload % 2)
                for g in range(GPS):
                    hs = g % 2
                    for fc in range(4):
                        pg = fc % 2
                        for kc in range(8):
                            S.op("pe", lambda kc=kc, fc=fc, pg=pg: P.matmul(ps[pg][:], lhsT=wg[slot][:, kc, fc * 128:(fc + 1) * 128],
                                                                          rhs=hT[:, kc, g * 512:(g + 1) * 512],
                                                                          start=(kc == 0), stop=(kc == 7)),
                                 r=[("wg", slot)] + [("hT", g * 4 + i) for i in range(4)], w=[("ps", pg)])
                        for kc in range(8):
                            S.op("pe", lambda kc=kc, fc=fc, pg=pg: P.matmul(ps[2 + pg][:], lhsT=wu[slot][:, kc, fc * 128:(fc + 1) * 128],
                                                                          rhs=hT[:, kc, g * 512:(g + 1) * 512],
                                                                          start=(kc == 0), stop=(kc == 7)),
                                 r=[("wu", slot)] + [("hT", g * 4 + i) for i in range(4)], w=[("ps", 2 + pg)])
                        S.op("act", lambda pg=pg: A.activation(out=sg[pg][:], in_=ps[pg][:], func=AF.Silu),
                             r=[("ps", pg)], w=[("sg", pg)])
                        S.op("dve", lambda pg=pg, fc=fc: V.tensor_tensor(out=hdn[hs][:, fc, :], in0=ps[2 + pg][:], in1=sg[pg][:], op=ALU.mult),
                             r=[("ps", 2 + pg), ("sg", pg)], w=[("hdn", hs, fc)])
                    for tt in range(4):
                        t = g * 4 + tt
                        for half in range(2):
                            pb = 4 + (tt * 2 + half) % 4
                            for fc in range(4):
                                S.op("pe", lambda fc=fc, half=half, pb=pb, tt=tt: P.matmul(ps[pb][:], lhsT=hdn[hs][:, fc, tt * 128:(tt + 1) * 128],
                                                                                       rhs=wd[slot][:, fc, half * 512:(half + 1) * 512],
                                                                                       start=(fc == 0), stop=(fc == 3)),
                                     r=[("hdn", hs, fc), ("wd", slot)], w=[("ps", pb)])
                            S.op("dve", lambda half=half, pb=pb, t=t: V.scalar_tensor_tensor(
                                out=yacc[:, t, half * 512:(half + 1) * 512], in0=ps[pb][:], scalar=comb[:, t, e:e + 1],
                                in1=yacc[:, t, half * 512:(half + 1) * 512], op0=ALU.mult, op1=ALU.add),
                                r=[("ps", pb), ("comb", t), ("yacc", t)], w=[("yacc", t)])
            for t in range(TPS):
                tok0 = s * SG + t * 128
                sl = t % 2
                src = yacc[:, t, :]
                for c in range(2):
                    S.op("dve", lambda c=c: V.bn_stats(out=st[:, c, :], in_=src[:, c * 512:(c + 1) * 512]), r=[("yacc", t)], w=["st"])
                S.op("dve", lambda: V.bn_aggr(out=mv[:], in_=st[:].rearrange("p a b -> p (a b)")), r=["st"], w=["mv"])
                S.op("dve", lambda: V.tensor_scalar_add(out=rstd[:], in0=mv[:, 1:2], scalar1=LN_EPS), r=["mv"], w=["rstd"])
                S.op("act", lambda: A.activation(out=rstd[:], in_=rstd[:], func=AF.Ln), r=["rstd"], w=["rstd"])
                S.op("act", lambda: A.activation(out=rstd[:], in_=rstd[:], func=AF.Exp, scale=-0.5), r=["rstd"], w=["rstd"])
                S.op("dve", lambda: V.tensor_scalar(out=ot[sl][:], in0=src, scalar1=mv[:, 0:1], scalar2=rstd[:],
                                                    op0=ALU.subtract, op1=ALU.mult), r=["mv", "rstd", ("yacc", t)], w=[("ot", sl)])
                S.op("pool", lambda: G.tensor_tensor(out=ot[sl][:], in0=ot[sl][:], in1=lnp[:, 2, :], op=ALU.mult), r=["lnp", ("ot", sl)], w=[("ot", sl)])
                S.op("pool", lambda: G.tensor_tensor(out=ot[sl][:], in0=ot[sl][:], in1=lnp[:, 3, :], op=ALU.add), r=["lnp", ("ot", sl)], w=[("ot", sl)])
                S.dma("sp", out_d[tok0:tok0 + 128, :], ot[sl][:], r=[("ot", sl)], sem=("ot", sl))
                if t % 2 == 1 and chunk_done:
                    pending.append(tok0 // 256)
        while pending:
            chunk_done(pending.pop(0))


def emit_gla(nc, S, ps, DD, tag, S_LEN=16384):
    D = 1024
    NT = S_LEN // 128
    h_d, wcat_d, wg2_d, bg_d, gn_d = DD["h"], DD["wcat"], DD["wg2"], DD["bg"], DD["gn"]
    id_d, tr_d, ci_d, on_d, y_d = DD["ident"], DD["trirev"], DD["cind"], DD["ones1"], DD["yout"]
    hmap = DD.get("hmap", lambda n: n)
    chunk_done = DD.get("chunk_done")
    with ExitStack() as es:
        def sb(name, shape, dt=F32):
            return es.enter_context(nc.sbuf_tensor("sb_" + tag + "_" + name, shape, dt))
        ident = sb("ident", [128, 128])
        trirev = sb("trirev", [128, 128])
        cind = sb("cind", [128, 2])
        ones1 = sb("ones1", [1, 128])
        wcat = sb("wcat", [128, 8, 784], BF16)
        wg2 = sb("wg2", [16, 128])
        bg = sb("bg", [1, 128])
        gn = sb("gn", [128, 256])
        state = sb("state", [128, 256])
        qlo = sb("qlo", [128, 128])
        qhi = sb("qhi", [128, 128])
        ht = [sb("ht%d" % i, [128, D]) for i in range(2)]
        hT = [sb("hT%d" % i, [128, 8, 128], BF16) for i in range(2)]
        lrT = sb("lrT", [16, 128])
        la = sb("la", [128, 128])
        kd = sb("kd", [128, 128])
        dec = sb("dec", [128, 2])
        kdec = sb("kdec", [128, 128], BF16)
        vbf = sb("vbf", [128, 256], BF16)
        er = sb("er", [128, 256])
        junk = sb("junk", [128, 256])
        ss = sb("ss", [128, 1])
        yo = [sb("yo%d" % i, [128, 256]) for i in range(2)]
        V, A, P, G = nc.vector, nc.scalar, nc.tensor, nc.gpsimd

        S.dma("sp", ident[:], id_d[:, :], w=["ident"], sem="ident")
        S.dma("sp", trirev[:], tr_d[:, :], w=["trirev"], sem="trirev")
        S.dma("sp", cind[:], ci_d[:, :], w=["cind"], sem="cind")
        S.dma("sp", ones1[:], on_d[:, :], w=["ones1"], sem="ones1")
        S.dma("pool", wcat[:], wcat_d.rearrange("(kc p) f -> p kc f", p=128), w=["wcat"], sem="wcat")
        S.dma("sp", wg2[:], wg2_d[:, :], w=["wg2"], sem="wg2")
        S.dma("sp", bg[:], bg_d[:, :], w=["bg"], sem="bg")
        S.dma("sp", gn[:], gn_d[0:1, :].partition_broadcast(128), w=["gn"], sem="gn")
        S.op("dve", lambda: V.memset(state[:], 0.0), w=["state"])
        S.op("dve", lambda: V.memset(qlo[:], 0.0), w=["qlo"])
        S.op("dve", lambda: V.memset(qhi[:], 0.0), w=["qhi"])

        for t in range(NT):
            tok0 = t * 128
            sl = t % 2
            if t == 0:
                S.dma("sp", ht[0][:], h_d[hmap(0):hmap(0) + 128, :], w=[("ht", 0)], sem=("ht", 0))
            if t + 1 < NT:
                tn = (t + 1) * 128
                S.dma("sp", ht[(t + 1) % 2][:], h_d[hmap(tn):hmap(tn) + 128, :], w=[("ht", (t + 1) % 2)], sem=("ht", (t + 1) % 2))
            for hb in range(2):
                for j in range(4):
                    kc = hb * 4 + j
                    S.op("pe", lambda: P.transpose(out=ps[hb][:, j * 128:(j + 1) * 128],
                                                   in_=ht[sl][:, kc * 128:(kc + 1) * 128], identity=ident[:]),
                         r=[("ht", sl), "ident"], w=[("ps", hb)])
            S.op("act", lambda: A.copy(out=hT[sl][:, 0:4, :], in_=ps[0][:].rearrange("p (a b) -> p a b", a=4)),
                 r=[("ps", 0)], w=[("hT", sl, 0)])
            S.op("dve", lambda: V.tensor_copy(out=hT[sl][:, 4:8, :], in_=ps[1][:].rearrange("p (a b) -> p a b", a=4)),
                 r=[("ps", 1)], w=[("hT", sl, 1)])
            for kc in range(8):
                S.op("pe", lambda: P.matmul(ps[2][:, 0:128], lhsT=wcat[:, kc, 0:128], rhs=hT[sl][:, kc, :],
                                            start=(kc == 0), stop=(kc == 7)),
                     r=["wcat", ("hT", sl, kc // 4)], w=[("ps", 2)])
            for kc in range(8):
                S.op("pe", lambda: P.matmul(ps[2][0:16, 128:256], lhsT=wcat[:, kc, 768:784], rhs=hT[sl][:, kc, :],
                                            start=(kc == 0), stop=(kc == 7)),
                     r=["wcat", ("hT", sl, kc // 4)], w=[("ps", 2)])
            for kc in range(8):
                S.op("pe", lambda: P.matmul(ps[3][:, 0:384], lhsT=hT[sl][:, kc, :], rhs=wcat[:, kc, 128:512],
                                            start=(kc == 0), stop=(kc == 7)),
                     r=["wcat", ("hT", sl, kc // 4)], w=[("ps", 3)])
            for kc in range(8):
                S.op("pe", lambda: P.matmul(ps[4][:, 0:256], lhsT=hT[sl][:, kc, :], rhs=wcat[:, kc, 512:768],
                                            start=(kc == 0), stop=(kc == 7)),
                     r=["wcat", ("hT", sl, kc // 4)], w=[("ps", 4)])
            S.op("act", lambda: A.mul(out=qlo[:, 0:64], in_=ps[2][:, 0:64], mul=128 ** -0.5), r=[("ps", 2)], w=["qlo"])
            S.op("act", lambda: A.mul(out=qhi[:, 64:128], in_=ps[2][:, 64:128], mul=128 ** -0.5), r=[("ps", 2)], w=["qhi"])
            S.op("dve", lambda: V.tensor_copy(out=lrT[:], in_=ps[2][0:16, 128:256]), r=[("ps", 2)], w=["lrT"])
            S.op("pe", lambda: P.matmul(ps[2][:, 0:128], lhsT=lrT[:], rhs=wg2[:], start=True, stop=False),
                 r=["lrT", "wg2"], w=[("ps", 2)])
            S.op("pe", lambda: P.matmul(ps[2][:, 0:128], lhsT=ones1[:], rhs=bg[:], start=False, stop=True),
                 r=["ones1", "bg"], w=[("ps", 2)])
            S.op("act", lambda: A.activation(out=la[:], in_=ps[2][:, 0:128], func=AF.Exp, scale=-1.0), r=[("ps", 2)], w=["la"])
            S.op("dve", lambda: V.tensor_scalar_add(out=la[:], in0=la[:], scalar1=1.0), r=["la"], w=["la"])
            S.op("act", lambda: A.activation(out=la[:], in_=la[:], func=AF.Ln), r=["la"], w=["la"])
            S.op("pe", lambda: P.matmul(ps[2][:, 128:256], lhsT=trirev[:], rhs=la[:], start=True, stop=True),
                 r=["trirev", "la"], w=[("ps", 2)])
            S.op("pe", lambda: P.matmul(ps[2][:, 256:258], lhsT=la[:], rhs=cind[:], start=True, stop=True),
                 r=["cind", "la"], w=[("ps", 2)])
            S.op("act", lambda: A.activation(out=kd[:], in_=ps[2][:, 128:256], func=AF.Exp), r=[("ps", 2)], w=["kd"])
            S.op("act", lambda: A.activation(out=dec[:], in_=ps[2][:, 256:258], func=AF.Exp), r=[("ps", 2)], w=["dec"])
            S.op("dve", lambda: V.tensor_tensor(out=kdec[:], in0=ps[3][:, 0:128], in1=kd[:], op=ALU.mult),
                 r=[("ps", 3), "kd"], w=["kdec"])
            S.op("act", lambda: A.copy(out=vbf[:], in_=ps[3][:, 128:384]), r=[("ps", 3)], w=["vbf"])
            for c in range(2):
                pb = 5 + c
                S.op("pe", lambda: P.matmul(ps[pb][:, 0:256], lhsT=kdec[c * 64:(c + 1) * 64, :], rhs=vbf[c * 64:(c + 1) * 64, :],
                                            start=True, stop=True),
                     r=["kdec", "vbf"], w=[("ps", pb)])
            for c in range(2):
                pb = 5 + c
                S.op("dve", lambda: V.scalar_tensor_tensor(out=state[:], in0=state[:], scalar=dec[:, c:c + 1], in1=ps[pb][:, 0:256],
                                                           op0=ALU.mult, op1=ALU.add),
                     r=["dec", ("ps", pb), "state"], w=["state"])
                S.op("pe", lambda: P.matmul(ps[7][:, 0:256], lhsT=(qlo if c == 0 else qhi)[:], rhs=state[:],
                                            start=(c == 0), stop=(c == 1)),
                     r=["state", "qlo" if c == 0 else "qhi"], w=[("ps", 7)])
            S.op("act", lambda: A.activation(out=er[:], in_=ps[4][:, 0:256], func=AF.Exp, scale=-1.0), r=[("ps", 4)], w=["er"])
            S.op("dve", lambda: V.tensor_scalar_add(out=er[:], in0=er[:], scalar1=1.0), r=["er"], w=["er"])
            S.op("dve", lambda: V.reciprocal(out=er[:], in_=er[:]), r=["er"], w=["er"])
            S.op("dve", lambda: V.tensor_tensor(out=er[:], in0=ps[4][:, 0:256], in1=er[:], op=ALU.mult), r=[("ps", 4), "er"], w=["er"])
            S.op("dve", lambda: V.tensor_tensor(out=er[:], in0=er[:], in1=gn[:], op=ALU.mult), r=["er", "gn"], w=["er"])
            S.op("act", lambda: A.activation(out=junk[:], in_=ps[7][:, 0:256], func=AF.Square, accum_out=ss[:]),
                 r=[("ps", 7)], w=["junk", "ss"])
            S.op("dve", lambda: V.tensor_scalar(out=ss[:], in0=ss[:], scalar1=1.0 / 256, scalar2=LN_EPS, op0=ALU.mult, op1=ALU.add),
                 r=["ss"], w=["ss"])
            S.op("act", lambda: A.activation(out=ss[:], in_=ss[:], func=AF.Ln), r=["ss"], w=["ss"])
            S.op("act", lambda: A.activation(out=ss[:], in_=ss[:], func=AF.Exp, scale=-0.5), r=["ss"], w=["ss"])
            S.op("dve", lambda: V.scalar_tensor_tensor(out=yo[sl][:], in0=ps[7][:, 0:256], scalar=ss[:], in1=er[:],
                                                       op0=ALU.mult, op1=ALU.mult),
                 r=[("ps", 7), "ss", "er"], w=[("yo", sl)])
            S.dma("sp", y_d[tok0:tok0 + 128, :], yo[sl][:], r=[("yo", sl)], sem=("yo", sl))
            if (t + 1) % 8 == 0 and chunk_done:
                chunk_done(t // 8)


def emit_att(nc, S, ps, DD, tag, S_LEN=16384):
    D = 1024
    NT = S_LEN // 128
    NG = S_LEN // 512
    h_d, hkv_d, wq_d, wk_d, wv_d = DD["h"], DD["hkv"], DD["wq"], DD["wk"], DD["wv"]
    bt_d, cfar_d, lam_d, gsub_d, cst_d = DD["biasT"], DD["cfar"], DD["lamv"], DD["gsub"], DD["cst"]
    id_d, on_d, y_d = DD["ident"], DD["ones128"], DD["yout"]
    hmap = DD.get("hmap", lambda n: n)
    chunk_done = DD.get("chunk_done")
    with ExitStack() as es:
        def sb(name, shape, dt=F32):
            return es.enter_context(nc.sbuf_tensor("sb_" + tag + "_" + name, shape, dt))
        ident = sb("ident", [128, 128])
        wq = sb("wq", [128, 8, 256], BF16)
        wk = sb("wk", [128, 8, 256], BF16)
        wv = sb("wv", [128, 8, 256], BF16)
        bt = sb("bt", [128, 4, 128])
        cfar = sb("cfar", [128, 2])
        lamv = sb("lamv", [128, 4, 64])
        lam = sb("lam", [128, 4])
        cst = sb("cst", [128, 2])
        KT = [sb("KT%d" % i, [128, S_LEN], BF16) for i in range(2)]
        VV = [sb("V%d" % i, [128, NT, 128], BF16) for i in range(2)]
        QT = [[sb("QT%d_%d" % (i, s), [128, 512], BF16) for s in range(2)] for i in range(2)]
        ht = [sb("ht%d" % i, [128, D]) for i in range(2)]
        hT = sb("hT", [128, 8, 512], BF16)
        NPT = 6
        PT = [sb("PT%d" % i, [128, 512], BF16) for i in range(NPT)]
        tmp = [sb("tmp%d" % i, [128, 128]) for i in range(2)]
        ones = sb("ones", [128, 128])
        gcol = sb("gcol", [128, 1])
        Pacc = [[sb("Pacc%d_%d" % (m, i), [128, 512]) for i in range(2)] for m in range(2)]
        rinv = [sb("rinv%d" % m, [128, 512]) for m in range(2)]
        oT = sb("oT", [128, 512])
        o2 = sb("o2", [128, 512])
        yT = [sb("yT%d" % i, [128, 512]) for i in range(2)]
        yo = [sb("yo%d" % i, [128, 4, 128]) for i in range(2)]
        V, A, P, G = nc.vector, nc.scalar, nc.tensor, nc.gpsimd

        S.dma("sp", ident[:], id_d[:, :], w=["ident"], sem="ident")
        S.dma("pool", wq[:], wq_d.rearrange("(kc p) f -> p kc f", p=128), w=["wq"], sem="wq")
        S.dma("pool", wk[:], wk_d.rearrange("(kc p) f -> p kc f", p=128), w=["wk"], sem="wk")
        S.dma("pool", wv[:], wv_d.rearrange("(kc p) f -> p kc f", p=128), w=["wv"], sem="wv")
        S.dma("sp", bt[:], bt_d.rearrange("h t k q -> k (h t) q"), w=["bt"], sem="bt")
        S.dma("sp", cfar[:], cfar_d[:, :], w=["cfar"], sem="cfar")
        S.dma("sp", cst[:], cst_d[:, :], w=["cst"], sem="cst")
        S.dma("sp", lamv[:], lam_d.partition_broadcast(128), w=["lamv"], sem="lamv")
        S.dma("sp", ones[:], on_d[:, :], w=["ones"], sem="ones")
        S.dma("sp", gcol[:], gsub_d.rearrange("o d -> d o"), w=["gcol"], sem="gcol")
        L = ["lam"]
        S.op("dve", lambda: V.tensor_tensor(out=lamv[:, 0, :], in0=lamv[:, 0, :], in1=lamv[:, 1, :], op=ALU.mult), r=["lamv"], w=["lamv"])
        S.op("dve", lambda: V.tensor_tensor(out=lamv[:, 2, :], in0=lamv[:, 2, :], in1=lamv[:, 3, :], op=ALU.mult), r=["lamv"], w=["lamv"])
        S.op("dve", lambda: V.tensor_reduce(out=lam[:, 0:1], in_=lamv[:, 0, :], axis=AX.X, op=ALU.add), r=["lamv"], w=L)
        S.op("dve", lambda: V.tensor_reduce(out=lam[:, 1:2], in_=lamv[:, 2, :], axis=AX.X, op=ALU.add), r=["lamv"], w=L)
        S.op("act", lambda: A.activation(out=lam[:, 0:2], in_=lam[:, 0:2], func=AF.Exp), r=L, w=L)
        S.op("dve", lambda: V.tensor_tensor(out=lam[:, 2:3], in0=lam[:, 1:2], in1=lam[:, 0:1], op=ALU.subtract), r=L, w=L)
        S.op("dve", lambda: V.tensor_tensor(out=lam[:, 3:4], in0=lam[:, 2:3], in1=cst[:, 0:1], op=ALU.subtract), r=L + ["cst"], w=L)
        S.op("dve", lambda: V.tensor_tensor(out=gcol[:], in0=gcol[:], in1=cst[:, 1:2], op=ALU.mult), r=["gcol", "cst"], w=["gcol"])

        def load_hT(src_d, g):
            for tt in range(4):
                tok0 = g * 512 + tt * 128
                sl = tt % 2
                S.dma("sp", ht[sl][:], src_d[hmap(tok0):hmap(tok0) + 128, :], w=[("ht", sl)], sem=("ht", sl))
                for hb in range(2):
                    for j in range(4):
                        kc = hb * 4 + j
                        S.op("pe", lambda: P.transpose(out=ps[6 + hb][:, j * 128:(j + 1) * 128],
                                                       in_=ht[sl][:, kc * 128:(kc + 1) * 128], identity=ident[:]),
                             r=[("ht", sl), "ident"], w=[("ps", 6 + hb)])
                S.op("act", lambda: A.copy(out=hT[:, 0:4, tt * 128:(tt + 1) * 128], in_=ps[6][:].rearrange("p (a b) -> p a b", a=4)),
                     r=[("ps", 6)], w=[("hT", tt)])
                S.op("dve", lambda: V.tensor_copy(out=hT[:, 4:8, tt * 128:(tt + 1) * 128], in_=ps[7][:].rearrange("p (a b) -> p a b", a=4)),
                     r=[("ps", 7)], w=[("hT", tt)])
        HTK = [("hT", i) for i in range(4)]

        kv_load, kv_store = DD.get("kv_load"), DD.get("kv_store")
        if kv_load is None:
            for g in range(NG):
                load_hT(hkv_d, g)
                for i in range(2):
                    for kc in range(8):
                        S.op("pe", lambda: P.matmul(ps[6][:], lhsT=wk[:, kc, i * 128:(i + 1) * 128], rhs=hT[:, kc, :],
                                                    start=(kc == 0), stop=(kc == 7)), r=["wk"] + HTK, w=[("ps", 6)])
                    S.op("act" if i == 0 else "dve",
                         (lambda: A.copy(out=KT[i][:, g * 512:(g + 1) * 512], in_=ps[6][:])) if i == 0 else
                         (lambda: V.tensor_copy(out=KT[i][:, g * 512:(g + 1) * 512], in_=ps[6][:])),
                         r=[("ps", 6)], w=[("KT", i)])
                for tt in range(4):
                    for kc in range(8):
                        S.op("pe", lambda: P.matmul(ps[7][:, 0:256], lhsT=hT[:, kc, tt * 128:(tt + 1) * 128], rhs=wv[:, kc, :],
                                                    start=(kc == 0), stop=(kc == 7)), r=["wv"] + HTK, w=[("ps", 7)])
                    S.op("act", lambda: A.copy(out=VV[0][:, g * 4 + tt, 0:128], in_=ps[7][:, 0:128]), r=[("ps", 7)], w=[("V", 0)])
                    S.op("dve", lambda: V.tensor_copy(out=VV[1][:, g * 4 + tt, 0:128], in_=ps[7][:, 128:256]), r=[("ps", 7)], w=[("V", 1)])


            if kv_store is not None:
                for i in range(2):
                    S.dma("sp", kv_store[i], KT[i][:], r=[("KT", i)], sem=("kvst", i))
                    S.dma("sp", kv_store[2 + i].rearrange("p (t d) -> p t d", d=128), VV[i][:], r=[("V", i)], sem=("kvst", 2 + i))
        else:
            for i in range(2):
                S.dma("sp", KT[i][:], kv_load[i], w=[("KT", i)], sem=("kvld", i))
                S.dma("sp", VV[i][:], kv_load[2 + i].rearrange("p (t d) -> p t d", d=128), w=[("V", i)], sem=("kvld", 2 + i))
        state = {"pt": 0, "sb": 0}
        SBANK = [(0, 1), (4, 5)]
        RS = 7
        pinit = {}

        def emit_S(it):
            (g, hd, j, qs) = it
            c0 = max(0, j - 4 * g)
            banks = SBANK[state["sb"] % 2]
            state["sb"] += 1
            ptis = (state["pt"] % NPT, (state["pt"] + 1) % NPT)
            state["pt"] += 2
            for mp in range(2):
                lo = mp * 64
                S.op("pe", lambda: P.matmul(ps[banks[mp]][:, c0 * 128:512], lhsT=KT[hd][lo:lo + 64, j * 128:(j + 1) * 128],
                                            rhs=QT[hd][qs][lo:lo + 64, c0 * 128:512], start=True, stop=True),
                     r=[("KT", hd), ("QT", hd, qs)], w=[("ps", banks[mp])])
            for mp in range(2):
                sbk = banks[mp]
                pti = ptis[mp]
                if j < 4 * g - 1:
                    S.op("act", lambda: A.activation(out=PT[pti][:], in_=ps[sbk][:], func=AF.Exp, scale=0.125, bias=cfar[:, hd:hd + 1]),
                         r=[("ps", sbk), "cfar"], w=[("PT", pti)])
                else:
                    for c in range(c0, 4):
                        i = 4 * g + c
                        cs = slice(c * 128, (c + 1) * 128)
                        if j < i - 1:
                            S.op("act", lambda: A.activation(out=PT[pti][:, cs], in_=ps[sbk][:, cs], func=AF.Exp, scale=0.125,
                                                             bias=cfar[:, hd:hd + 1]),
                                 r=[("ps", sbk), "cfar"], w=[("PT", pti)])
                        else:
                            ty = 0 if j == i else 1
                            tb = (c + mp) % 2
                            S.op("dve", lambda: V.scalar_tensor_tensor(out=tmp[tb][:], in0=ps[sbk][:, cs], scalar=0.125,
                                                                       in1=bt[:, hd * 2 + ty, :], op0=ALU.mult, op1=ALU.add),
                                 r=[("ps", sbk), "bt"], w=[("tmp", tb)])
                            S.op("act", lambda: A.activation(out=PT[pti][:, cs], in_=tmp[tb][:], func=AF.Exp),
                                 r=[("tmp", tb)], w=[("PT", pti)])
            return (c0, ptis)

        def emit_PV(it, info):
            (g, hd, j, qs) = it
            (c0, ptis) = info
            cs = slice(c0 * 128, 512)
            for mp in range(2):
                pti = ptis[mp]
                S.op("pe", lambda: P.matmul(ps[2 + mp][:, cs], lhsT=VV[hd][:, j, :], rhs=PT[pti][:, cs],
                                            start=(j == 0), stop=(j == 4 * g + 3), skip_group_check=True),
                     r=[("PT", pti), ("V", hd)], w=[("ps", 2 + mp)])
            for mp in range(2):
                pti = ptis[mp]
                a = 1 if (2 * j + mp) % 3 == 0 else 0
                eng, E = ("dve", V) if a == 0 else ("pool", G)
                key = (g, hd, mp, a)
                if key not in pinit:
                    pinit[key] = True
                    if c0 > 0:
                        S.op(eng, lambda: E.memset(Pacc[mp][a][:, 0:c0 * 128], 0.0), w=[("Pacc", mp, a)])
                    S.op(eng, lambda: E.tensor_copy(out=Pacc[mp][a][:, cs], in_=PT[pti][:, cs]), r=[("PT", pti)], w=[("Pacc", mp, a)])
                else:
                    S.op(eng, lambda: E.tensor_tensor(out=Pacc[mp][a][:, cs], in0=Pacc[mp][a][:, cs], in1=PT[pti][:, cs], op=ALU.add),
                         r=[("PT", pti), ("Pacc", mp, a)], w=[("Pacc", mp, a)])
            if j == 4 * g + 3:
                for mp in range(2):
                    accs = [a2 for a2 in range(2) if (g, hd, mp, a2) in pinit]
                    for n2, a2 in enumerate(accs):
                        S.op("pe", lambda: P.matmul(ps[RS][:], lhsT=ones[:], rhs=Pacc[mp][a2][:], start=(n2 == 0), stop=(n2 == len(accs) - 1)),
                             r=["ones", ("Pacc", mp, a2)], w=[("ps", RS)])
                    S.op("dve", lambda: V.reciprocal(out=rinv[mp][:], in_=ps[RS][:]), r=[("ps", RS)], w=[("rinv", mp)])

        def epilogue(g, hd, ys):
            S.op("dve", lambda: V.tensor_tensor(out=oT[:], in0=ps[2][:], in1=rinv[0][:], op=ALU.mult), r=[("ps", 2), ("rinv", 0)], w=["oT"])
            S.op("dve", lambda: V.tensor_tensor(out=o2[:], in0=ps[3][:], in1=rinv[1][:], op=ALU.mult), r=[("ps", 3), ("rinv", 1)], w=["o2"])
            S.op("dve", lambda: V.scalar_tensor_tensor(out=oT[:], in0=o2[:], scalar=lam[:, 3:4], in1=oT[:], op0=ALU.mult, op1=ALU.add),
                 r=["o2", "oT", "lam"], w=["oT"])
            S.op("act", lambda: A.activation(out=o2[:], in_=oT[:], func=AF.Square), r=["oT"], w=["o2"])
            S.op("pe", lambda: P.matmul(ps[RS][:], lhsT=ones[:], rhs=o2[:], start=True, stop=True), r=["ones", "o2"], w=[("ps", RS)])
            S.op("dve", lambda: V.tensor_scalar(out=o2[:], in0=ps[RS][:], scalar1=1.0 / 128, scalar2=LN_EPS, op0=ALU.mult, op1=ALU.add),
                 r=[("ps", RS)], w=["o2"])
            S.op("act", lambda: A.activation(out=o2[:], in_=o2[:], func=AF.Ln), r=["o2"], w=["o2"])
            S.op("act", lambda: A.activation(out=o2[:], in_=o2[:], func=AF.Exp, scale=-0.5), r=["o2"], w=["o2"])
            S.op("dve", lambda: V.scalar_tensor_tensor(out=yT[hd][:], in0=oT[:], scalar=gcol[:, 0:1], in1=o2[:], op0=ALU.mult, op1=ALU.mult),
                 r=["oT", "o2", "gcol"], w=[("yT", hd)])
            for c in range(4):
                S.op("pe", lambda: P.transpose(out=ps[6][:, c * 128:(c + 1) * 128], in_=yT[hd][:, c * 128:(c + 1) * 128], identity=ident[:]),
                     r=[("yT", hd), "ident"], w=[("ps", 6)])
            S.op("act", lambda: A.copy(out=yo[hd][:], in_=ps[6][:].rearrange("p (c f) -> p c f", c=4)), r=[("ps", 6)], w=[("yo", hd)])
            S.dma("sp", y_d[g * 512:(g + 1) * 512, hd * 128:(hd + 1) * 128].rearrange("(c p) f -> p c f", p=128), yo[hd][:],
                  r=[("yo", hd)], sem=("yo", hd))

        def prep(g):
            qs = g % 2
            load_hT(h_d, g)
            for hd in range(2):
                for kc in range(8):
                    S.op("pe", lambda: P.matmul(ps[6][:], lhsT=wq[:, kc, hd * 128:(hd + 1) * 128], rhs=hT[:, kc, :],
                                                start=(kc == 0), stop=(kc == 7)), r=["wq"] + HTK, w=[("ps", 6)])
                S.op("dve", lambda: V.tensor_copy(out=QT[hd][qs][:], in_=ps[6][:]), r=[("ps", 6)], w=[("QT", hd, qs)])

        prep(0)
        for g in range(NG):
            qs = g % 2
            ys = g % 2
            if g >= 2 and g % 2 == 0 and chunk_done:
                chunk_done(g // 2 - 1)
            for hd in range(2):
                items = [(g, hd, j, qs) for j in range(4 * g + 4)]
                info = emit_S(items[0])
                for n in range(len(items)):
                    nxt = emit_S(items[n + 1]) if n + 1 < len(items) else None
                    emit_PV(items[n], info)
                    info = nxt
                    if hd == 1 and n == len(items) // 2 and g + 1 < NG:
                        prep(g + 1)
                epilogue(g, hd, ys)
        if chunk_done:
            chunk_done(NG // 2 - 1)


def build_fused():
    nc = bass.Bass("TRN2", target_bir_lowering=False)
    SL, D = 16384, 1024
    ext = lambda name, shape: nc.dram_tensor(name, shape, F32, kind="ExternalInput").ap()
    loc = lambda name, shape: nc.dram_tensor(name, shape, F32).ap()
    xb = ext("xb", [SL, D])
    xs = ext("xs", [4096, D])
    idx_d = nc.dram_tensor("idx", [128, 128], mybir.dt.uint32, kind="ExternalInput").ap()
    g_wcat, g_wg2, g_bg, g_gn = ext("g_wcat", [2, D, 784]), ext("g_wg2", [2, 16, 128]), ext("g_bg", [2, 1, 128]), ext("g_gn", [2, 1, 256])
    a_wq, a_wk, a_wv = ext("a_wq", [2, D, 256]), ext("a_wk", [D, 256]), ext("a_wv", [D, 256])
    a_bt, a_cfar, a_lamv = ext("a_biasT", [2, 2, 128, 128]), ext("a_cfar", [128, 2]), ext("a_lamv", [2, 4, 64])
    a_gsub, a_cst = ext("a_gsub", [2, 1, 128]), ext("a_cst", [2, 128, 2])
    p_wout, p_lnp, p_wr, p_rb = ext("p_wout", [4, D, D]), ext("p_lnp", [4, 4, D]), ext("p_wr", [4, D, 20]), ext("p_rb", [4, 1, 20])
    p_wg, p_wu, p_wd = ext("p_wg", [4, 16, D, 512]), ext("p_wu", [4, 16, D, 512]), ext("p_wd", [4, 16, 512, D])
    ident, trirev, cind = ext("ident", [128, 128]), ext("trirev", [128, 128]), ext("cind", [128, 2])
    ones1, ones128 = ext("ones1", [1, 128]), ext("ones128", [128, 128])
    out = nc.dram_tensor("out", [4096, D], F32, kind="ExternalOutput").ap()
    yloc, yg = loc("yloc", [SL, 256]), loc("yg", [4 * SL, 256])
    hloc = [loc("hloc%d" % i, [4096, D]) for i in range(3)]
    hg0, hkvg, hg2 = loc("hg0", [SL, D]), loc("hkvg", [SL, D]), loc("hg2", [SL, D])
    kvs = [nc.dram_tensor("kvs%d" % i, [128, SL], BF16).ap() for i in range(4)]
    GROUPS = [[0, 1, 2, 3], [4, 5, 6, 7]]

    def hperm(n):
        r_, w_ = n // 4096, n % 4096
        return (w_ // 256) * 1024 + r_ * 256 + (w_ % 256)

    def gather_y(S):
        S.coll_multi("AllGather", GROUPS, [(yloc[i * 1024:(i + 1) * 1024, :], yg[i * 4096:(i + 1) * 4096, :]) for i in range(16)])

    def gather_h(S, src, dst):
        S.coll_multi("AllGather", GROUPS, [(src[i * 256:(i + 1) * 256, :], dst[i * 1024:(i + 1) * 1024, :]) for i in range(16)])
    with ExitStack() as es:
        S = Sched(nc, es)
        ps = [es.enter_context(nc.psum_tensor("ps%d" % i, [128, 512], F32)) for i in range(8)]

        def ydone(i):
            S.coll_async("AllGather", GROUPS, yloc[i * 1024:(i + 1) * 1024, :], yg[i * 4096:(i + 1) * 4096, :], [("yo", 0), ("yo", 1)])

        def post(layer, ysrc, hp, dst, gdst=None):
            hdone = None
            if gdst is not None:
                hdone = lambda i: S.coll_async("AllGather", GROUPS, dst[i * 256:(i + 1) * 256, :], gdst[i * 1024:(i + 1) * 1024, :],
                                               [("ot", 0), ("ot", 1)])
            emit_post(nc, S, ps, {"y": ysrc[:, :], "idx": idx_d[:, :], "hp": hp, "chunk_done": hdone, "wout": p_wout[layer], "lnp": p_lnp[layer], "wr": p_wr[layer], "rb": p_rb[layer],
                                  "wg": p_wg[layer], "wu": p_wu[layer], "wd": p_wd[layer], "ident": ident, "out": dst},
                      "p%d" % layer)

        def gla(layer, h):
            emit_gla(nc, S, ps, {"h": h, "chunk_done": ydone, "hmap": (hperm if layer > 0 else (lambda n: n)), "wcat": g_wcat[layer], "wg2": g_wg2[layer], "bg": g_bg[layer], "gn": g_gn[layer],
                                 "ident": ident, "trirev": trirev, "cind": cind, "ones1": ones1, "yout": yloc}, "g%d" % layer)

        def att(j, h):
            emit_att(nc, S, ps, {"h": h, "hkv": hkvg, "chunk_done": ydone, "hmap": hperm, ("kv_store" if j == 0 else "kv_load"): kvs, "wq": a_wq[j], "wk": a_wk, "wv": a_wv, "biasT": a_bt, "cfar": a_cfar,
                                 "lamv": a_lamv[j], "gsub": a_gsub[j], "cst": a_cst[j], "ident": ident, "ones128": ones128,
                                 "yout": yloc}, "a%d" % j)

        import os
        LEVEL = int(os.environ.get("FUSED_LEVEL", "99"))
        steps = [
            lambda: gla(0, xb), lambda: S.barrier(), lambda: post(0, yg, xs, hloc[0], hg0), lambda: S.barrier(),
            lambda: gla(1, hg0), lambda: S.barrier(), lambda: post(1, yg, hloc[0], hloc[1], hkvg), lambda: S.barrier(),
            lambda: att(0, hkvg), lambda: S.barrier(), lambda: post(2, yg, hloc[1], hloc[2], hg2), lambda: S.barrier(),
            lambda: att(1, hg2), lambda: S.barrier(), lambda: post(3, yg, hloc[2], out),
        ]
        for i_, st_ in enumerate(steps):
            if i_ >= LEVEL:
                break
            st_()
        S.barrier()
    return nc


_NC = {}


def kernel(x, a_w_in, a_w_gate2, a_b_gate, a_g_norm, a_w_out, kv_w, b_w_q, b_lam_q1, b_lam_k1,
           b_lam_q2, b_lam_k2, b_g_sub, b_w_out, rel_table, moe_w_group, moe_b_group, moe_w_router,
           moe_b_router, moe_w_gate, moe_w_up, moe_w_down, ln_g, ln_b):
    f32 = np.float32
    A = lambda a: np.ascontiguousarray(np.asarray(a, dtype=f32))
    x = A(x)
    B, S_, D = x.shape
    if "nc" not in _NC:
        _NC["nc"] = build_fused()
    nc = _NC["nc"]
    w_in = A(a_w_in)
    kvw = A(kv_w)
    wqf = A(b_w_q)
    rt = A(rel_table)
    gconst = gla_consts()
    p_wout = A(np.stack([a_w_out[0], a_w_out[1], b_w_out[0], b_w_out[1]]))
    p_lnp = A(np.stack([np.stack([ln_g[l, 0], ln_b[l, 0], ln_g[l, 1], ln_b[l, 1]]) for l in range(4)]))
    p_wr = A(np.concatenate([moe_w_group, moe_w_router], axis=2))
    p_rb = A(np.concatenate([np.asarray(moe_b_group).reshape(4, -1), np.asarray(moe_b_router).reshape(4, -1)], axis=1)[:, None, :])
    p_wg, p_wu, p_wd = A(moe_w_gate), A(moe_w_up), A(moe_w_down)
    linits = [0.8 - 0.6 * math.exp(-0.3 * layer) for layer in (2, 3)]
    a_cst = A(np.stack([np.broadcast_to(np.array([[li, 1.0 - li]], f32), (128, 2)) for li in linits]))
    a_lamv = A(np.stack([np.stack([b_lam_q1[j], b_lam_k1[j], b_lam_q2[j], b_lam_k2[j]]) for j in range(2)]))
    a_gsub = A(np.asarray(b_g_sub)[:, None, :])
    in_maps = []
    for c in range(8):
        b, r = c // 4, c % 4
        hd = r
        g_wcat = np.stack([np.concatenate([w_in[l][:, hd * 128:(hd + 1) * 128], w_in[l][:, 512 + hd * 128:512 + (hd + 1) * 128],
                                           w_in[l][:, 1024 + hd * 256:1024 + (hd + 1) * 256], w_in[l][:, 2048 + hd * 256:2048 + (hd + 1) * 256],
                                           w_in[l][:, 3072:3088]], axis=1) for l in range(2)])
        heads = [2 * r, 2 * r + 1]
        a_wq = np.stack([np.concatenate([wqf[j][:, hh * 64:(hh + 1) * 64] if m_ == 0 else wqf[j][:, 512 + hh * 64:512 + (hh + 1) * 64]
                                         for hh in heads for m_ in range(2)], axis=1) for j in range(2)])
        a_wk = np.concatenate([kvw[:, hh * 64:(hh + 1) * 64] if m_ == 0 else kvw[:, 512 + hh * 64:512 + (hh + 1) * 64]
                               for hh in heads for m_ in range(2)], axis=1)
        a_wv = np.concatenate([kvw[:, 1024 + hh * 128:1024 + (hh + 1) * 128] for hh in heads], axis=1)
        idxv = np.zeros((128, 128), np.uint32)
        for r_ in range(4):
            for t_ in range(32):
                n_ = r * 4096 + t_ * 128
                idxv[:, r_ * 32 + t_] = (n_ // 1024) * 4096 + r_ * 1024 + (n_ % 1024) + np.arange(128)
        m = {"xb": x[b], "xs": np.ascontiguousarray(x[b, r * 4096:(r + 1) * 4096]), "idx": idxv, "g_wcat": A(g_wcat), "g_wg2": A(np.asarray(a_w_gate2)[:, :, hd * 128:(hd + 1) * 128]),
             "g_bg": A(np.asarray(a_b_gate)[:, None, hd * 128:(hd + 1) * 128]), "g_gn": A(np.asarray(a_g_norm)[:, None, hd * 256:(hd + 1) * 256]),
             "a_wq": A(a_wq), "a_wk": A(a_wk), "a_wv": A(a_wv), "a_biasT": att_bias_tiles(rt, heads),
             "a_cfar": A(np.broadcast_to(rt[15, heads][None, :], (128, 2))), "a_lamv": a_lamv, "a_gsub": a_gsub, "a_cst": a_cst,
             "p_wout": p_wout, "p_lnp": p_lnp, "p_wr": p_wr, "p_rb": p_rb, "p_wg": p_wg, "p_wu": p_wu, "p_wd": p_wd,
             "ident": gconst["ident"], "trirev": gconst["trirev"], "cind": gconst["cind"], "ones1": gconst["ones1"],
             "ones128": np.ones((128, 128), f32)}
        in_maps.append(m)
    res = run_bass_kernel_spmd(nc, in_maps, core_ids=list(range(8)))
    return np.concatenate([res.results[c]["out"] for c in range(8)], axis=0).reshape(B, S_, D)
```

```python
from contextlib import ExitStack
import math
import numpy as np
import concourse.bass as bass
import concourse.mybir as mybir
from concourse.bass_utils import run_bass_kernel_spmd

F32 = mybir.dt.float32
BF16 = mybir.dt.bfloat16
AF = mybir.ActivationFunctionType
ALU = mybir.AluOpType
AX = mybir.AxisListType


class Sched:
    ENG = ("pe", "act", "dve", "pool", "sp")

    def __init__(self, nc, es):
        self.nc = nc
        self.es = es
        self.eng = {"pe": nc.tensor, "act": nc.scalar, "dve": nc.vector, "pool": nc.gpsimd, "sp": nc.sync}
        self.sem = {e: es.enter_context(nc.semaphore("s_" + e)) for e in self.ENG}
        self.cnt = {e: 0 for e in self.ENG}
        self.seen = {e: {} for e in self.ENG}
        self.snaps = {e: [None] for e in self.ENG}
        self.dsem = {}
        self.dcnt = {}
        self.lastw = {}
        self.readers = {}
        self.nwait = 0
        self.ninst = 0

    def _deps(self, r, w):
        deps = []
        for k in r:
            t = self.lastw.get(k)
            if t is not None:
                deps.append(t)
        for k in w:
            t = self.lastw.get(k)
            if t is not None:
                deps.append(t)
            deps.extend(self.readers.get(k, ()))
        return deps

    def _wait(self, e, deps, skip_dma_sem=None):
        seen = self.seen[e]
        need = {}
        for (src, val) in deps:
            if src == e and e == "pe":
                continue
            if skip_dma_sem is not None and src == skip_dma_sem:
                continue
            if seen.get(src, 0) >= val:
                continue
            if need.get(src, 0) < val:
                need[src] = val
        if not need:
            return
        seen = dict(seen)
        for src, val in need.items():
            if isinstance(src, tuple):
                self.eng[e].wait_ge(self.dsem[src[1]], val)
            else:
                self.eng[e].wait_ge(self.sem[src], val)
                snap = self.snaps[src][val]
                if snap:
                    for s2, v2 in snap.items():
                        if seen.get(s2, 0) < v2:
                            seen[s2] = v2
            if seen.get(src, 0) < val:
                seen[src] = val
            self.nwait += 1
        self.seen[e] = seen

    def _commit(self, tok, r, w):
        for k in w:
            self.lastw[k] = tok
            self.readers[k] = []
        for k in r:
            self.readers.setdefault(k, []).append(tok)

    def op(self, e, fn, r=(), w=()):
        px = [k for k in r if isinstance(k, tuple) and k[0] == "ps"]
        if px:
            r = [k for k in r if k not in px]
            w = list(w) + px
        self._wait(e, self._deps(r, w))
        ins = fn()
        self.cnt[e] += 1
        ins.then_inc(self.sem[e], 1)
        self.snaps[e].append(self.seen[e])
        self._commit((e, self.cnt[e]), r, w)
        self.ninst += 1
        return ins

    def dma(self, q, out, in_, r=(), w=(), sem=None):
        assert sem is not None
        if sem not in self.dsem:
            self.dsem[sem] = self.es.enter_context(self.nc.semaphore("d_%d" % len(self.dsem)))
            self.dcnt[sem] = 0
        self._wait(q, self._deps(r, w), skip_dma_sem=("dma", sem))
        ins = self.eng[q].dma_start(out=out, in_=in_)
        self.dcnt[sem] += 16
        ins.then_inc(self.dsem[sem], 16)
        self._commit((("dma", sem), self.dcnt[sem]), r, w)
        self.ninst += 1
        return ins

    def finish(self, keys):
        deps = []
        for k in keys:
            t = self.lastw.get(k)
            if t is not None:
                deps.append(t)
            deps.extend(self.readers.get(k, ()))
        self._wait("sp", deps)


def _sched_barrier(self):
    for e in self.ENG:
        deps = [(f, self.cnt[f]) for f in self.ENG if self.cnt[f] > 0]
        deps += [(("dma", k), v) for k, v in self.dcnt.items() if v > 0]
        self._wait(e, deps)
    self.lastw = {}
    self.readers = {}


def _sched_coll(self, kind, groups, in_ap, out_ap):
    self.barrier()
    if "cc" not in self.dsem:
        self.dsem["cc"] = self.es.enter_context(self.nc.semaphore("d_cc"))
        self.dcnt["cc"] = 0
    ins = self.nc.gpsimd.collective_compute(kind, ALU.bypass, replica_groups=groups, ins=[in_ap], outs=[out_ap])
    self.dcnt["cc"] += 1
    ins.then_inc(self.dsem["cc"])
    self.ninst += 1
    self.barrier()


Sched.barrier = _sched_barrier
Sched.coll = _sched_coll


def _sched_dma_fn(self, q, fn, r=(), w=(), sem=None):
    if sem not in self.dsem:
        self.dsem[sem] = self.es.enter_context(self.nc.semaphore("d_%d" % len(self.dsem)))
        self.dcnt[sem] = 0
    self._wait(q, self._deps(r, w), skip_dma_sem=("dma", sem))
    ins = fn()
    self.dcnt[sem] += 16
    ins.then_inc(self.dsem[sem], 16)
    self._commit((("dma", sem), self.dcnt[sem]), r, w)
    self.ninst += 1
    return ins


Sched.dma_fn = _sched_dma_fn


def _sched_coll_multi(self, kind, groups, pairs):
    self.barrier()
    if "cc" not in self.dsem:
        self.dsem["cc"] = self.es.enter_context(self.nc.semaphore("d_cc"))
        self.dcnt["cc"] = 0
    for (in_ap, out_ap) in pairs:
        ins = self.nc.gpsimd.collective_compute(kind, ALU.bypass, replica_groups=groups, ins=[in_ap], outs=[out_ap])
        self.dcnt["cc"] += 1
        ins.then_inc(self.dsem["cc"])
        self.ninst += 1
        self.nc.gpsimd.wait_ge(self.dsem["cc"], self.dcnt["cc"])
    self.barrier()


Sched.coll_multi = _sched_coll_multi


def _sched_coll_async(self, kind, groups, in_ap, out_ap, wait_sems):
    if "cc" not in self.dsem:
        self.dsem["cc"] = self.es.enter_context(self.nc.semaphore("d_cc"))
        self.dcnt["cc"] = 0
    deps = [(("dma", k), self.dcnt[k]) for k in wait_sems if self.dcnt.get(k, 0) > 0]
    if self.dcnt["cc"] > 0:
        deps.append((("dma", "cc"), self.dcnt["cc"]))
    self._wait("pool", deps)
    ins = self.nc.gpsimd.collective_compute(kind, ALU.bypass, replica_groups=groups, ins=[in_ap], outs=[out_ap], dma_qos="P3")
    self.dcnt["cc"] += 1
    ins.then_inc(self.dsem["cc"])
    self.ninst += 1


Sched.coll_async = _sched_coll_async

ALPHA = (2.0 * 4) ** 0.25
LN_EPS = 1e-5
TAU = 16.0
NEGB = -30000.0

LN_EPS = 1e-5
TAU = 16.0


def gla_consts():
    s = np.arange(128)
    same = (s[:, None] // 64) == (s[None, :] // 64)
    trirev = ((s[:, None] > s[None, :]) & same).astype(np.float32) * (-1.0 / TAU)
    cind = np.zeros((128, 2), np.float32)
    cind[:64, 0] = -1.0 / TAU
    cind[64:, 1] = -1.0 / TAU
    return {"ident": np.eye(128, dtype=np.float32), "trirev": trirev, "cind": cind,
            "ones1": np.ones((1, 128), np.float32)}


LN_EPS = 1e-5
NEGB = -30000.0


def rel_bucket_np(rel):
    nb = 16
    max_exact = 8
    base = np.where(rel > 0, nb, 0)
    n = np.abs(rel)
    large = max_exact + (np.log(np.maximum(n, 1).astype(np.float32) / np.float32(max_exact))
                         / np.float32(math.log(128 / max_exact)) * np.float32(nb - max_exact)).astype(np.int32)
    large = np.minimum(large, nb - 1)
    return base + np.where(n < max_exact, n, large)


def att_bias_tiles(rel_table, heads):
    kl = np.arange(128)[:, None]
    ql = np.arange(128)[None, :]
    out = np.zeros((len(heads), 2, 128, 128), np.float32)
    bd = rel_bucket_np(kl - ql)
    bp = rel_bucket_np(kl - ql - 128)
    vis = (kl // 64) <= (ql // 64)
    for i, h in enumerate(heads):
        out[i, 0] = np.where(vis, rel_table[bd, h], np.float32(NEGB))
        out[i, 1] = rel_table[bp, h]
    return out


def emit_post(nc, S, ps, D, tag, NTOK=4096, SG=1024, NEXP=16, do_A=True, do_B=True, do_R=True, stage=9):
    D_ = 1024
    D, DD = D_, D
    FF = 512
    y_d, hp_d, wout_d, lnp_d, wr_d, rb_d = DD["y"], DD["hp"], DD["wout"], DD["lnp"], DD["wr"], DD["rb"]
    wg_d, wu_d, wd_d, id_d, out_d = DD["wg"], DD["wu"], DD["wd"], DD["ident"], DD["out"]
    chunk_done = DD.get("chunk_done")
    NSG = NTOK // SG
    TPS = SG // 128
    GPS = SG // 512
    with ExitStack() as es:
        def sb(name, shape, dt=F32):
            return es.enter_context(nc.sbuf_tensor("sb_" + tag + "_" + name, shape, dt))
        ident = sb("ident", [128, 128])
        idx = sb("idx", [128, 128], mybir.dt.uint32)
        wout = sb("wout", [128, 8, D], BF16)
        wr = sb("wr", [128, 8, 20])
        rb = sb("rb", [128, 20])
        lnp = sb("lnp", [128, 4, D])
        hT = sb("hT", [128, 8, SG], BF16)
        yacc = sb("yacc", [128, TPS, D])
        comb = sb("comb", [128, TPS, 16])
        wg = [sb("wg%d" % i, [128, 8, FF], BF16) for i in range(2)]
        wu = [sb("wu%d" % i, [128, 8, FF], BF16) for i in range(2)]
        wd = [sb("wd%d" % i, [128, 4, D], BF16) for i in range(2)]
        yt = [sb("yt%d" % i, [128, D]) for i in range(2)]
        hpt = [sb("hpt%d" % i, [128, D]) for i in range(2)]
        yT = [sb("yT%d" % i, [128, 8, 128], BF16) for i in range(2)]
        zt = [sb("z%d" % i, [128, D]) for i in range(2)]
        hT32 = [sb("hT32_%d" % i, [128, 8, 128]) for i in range(2)]
        sg = [sb("sg%d" % i, [128, 512]) for i in range(2)]
        hdn = [sb("hdn%d" % i, [128, 4, 512], BF16) for i in range(2)]
        ot = [sb("ot%d" % i, [128, D]) for i in range(2)]
        st = sb("stats", [128, 2, 6])
        mv = sb("mv", [128, 2])
        rstd = sb("rstd", [128, 1])
        lg = sb("lg", [128, 20])
        r_gmax = sb("r_gmax", [128, 1])
        r_gmask = sb("r_gmask", [128, 4])
        r_gt = sb("r_gt", [128, 4])
        r_gsum = sb("r_gsum", [128, 1])
        r_m1 = sb("r_m1", [128, 4])
        r_m2 = sb("r_m2", [128, 4])
        r_is1 = sb("r_is1", [128, 4, 4])
        r_is2 = sb("r_is2", [128, 4, 4])
        r_e2 = sb("r_e2", [128, 4, 4])
        r_w1 = sb("r_w1", [128, 4])
        r_w2 = sb("r_w2", [128, 4])
        r_gs = sb("r_gs", [128, 4])

        V, A, P, G = nc.vector, nc.scalar, nc.tensor, nc.gpsimd

        S.dma("sp", ident[:], id_d[:, :], w=["ident"], sem="ident")
        S.dma("sp", idx[:], DD["idx"], w=["idx"], sem="idx")
        S.dma("pool", wout[:], wout_d.rearrange("(kc p) f -> p kc f", p=128), w=["wout"], sem="wout")
        S.dma("sp", wr[:], wr_d.rearrange("(kc p) f -> p kc f", p=128), w=["wr"], sem="wr")
        S.dma("sp", rb[:], rb_d[0:1, :].partition_broadcast(128), w=["rb"], sem="rb")
        S.dma("sp", lnp[:], lnp_d.partition_broadcast(128), w=["lnp"], sem="lnp")

        def load_expert(e, slot):
            S.dma("pool", wg[slot][:], wg_d[e].rearrange("(kc p) f -> p kc f", p=128), w=[("wg", slot)], sem=("wg", slot))
            S.dma("pool", wu[slot][:], wu_d[e].rearrange("(kc p) f -> p kc f", p=128), w=[("wu", slot)], sem=("wu", slot))
            S.dma("pool", wd[slot][:], wd_d[e].rearrange("(kc p) f -> p kc f", p=128), w=[("wd", slot)], sem=("wd", slot))

        def layernorm(src, dst, gi, sl):
            for c in range(2):
                S.op("dve", lambda c=c: V.bn_stats(out=st[:, c, :], in_=src[:, c * 512:(c + 1) * 512]), r=[sl], w=["st"])
            S.op("dve", lambda: V.bn_aggr(out=mv[:], in_=st[:].rearrange("p a b -> p (a b)")), r=["st"], w=["mv"])
            S.op("dve", lambda: V.tensor_scalar_add(out=rstd[:], in0=mv[:, 1:2], scalar1=LN_EPS), r=["mv"], w=["rstd"])
            S.op("act", lambda: A.activation(out=rstd[:], in_=rstd[:], func=AF.Ln), r=["rstd"], w=["rstd"])
            S.op("act", lambda: A.activation(out=rstd[:], in_=rstd[:], func=AF.Exp, scale=-0.5), r=["rstd"], w=["rstd"])
            S.op("dve", lambda: V.scalar_tensor_tensor(out=dst, in0=src, scalar=mv[:, 0:1], in1=lnp[:, gi, :], op0=ALU.subtract, op1=ALU.mult),
                 r=["mv", "lnp", sl], w=[sl])
            S.op("dve", lambda: V.scalar_tensor_tensor(out=dst, in0=dst, scalar=rstd[:], in1=lnp[:, gi + 1, :], op0=ALU.mult, op1=ALU.add),
                 r=["rstd", "lnp", sl], w=[sl])

        pending = []
        cur_e = [0]
        nload = 0
        for s in range(NSG):
            if do_B:
                load_expert(0, nload % 2)
            for t in range(TPS if do_A else 0):
                tok0 = s * SG + t * 128
                sl = t % 2
                tg = s * TPS + t
                for r_ in range(4):
                    S.dma_fn("pool", lambda: G.indirect_dma_start(out=yt[sl][:, r_ * 256:(r_ + 1) * 256], out_offset=None, in_=y_d,
                                                                  in_offset=bass.IndirectOffsetOnAxis(ap=idx[:, r_ * 32 + tg:r_ * 32 + tg + 1], axis=0)),
                             r=["idx"], w=[("yt", sl)], sem=("yt", sl))
                S.dma("sp", hpt[sl][:], hp_d[tok0:tok0 + 128, :], w=[("hp", sl)], sem=("hp", sl))
                for hb in range(2):
                    for j in range(4):
                        kc = hb * 4 + j
                        S.op("pe", lambda kc=kc, j=j, hb=hb: P.transpose(out=ps[hb][:, j * 128:(j + 1) * 128],
                                                                        in_=yt[sl][:, kc * 128:(kc + 1) * 128], identity=ident[:]),
                             r=[("yt", sl), "ident"], w=[("ps", hb)])
                S.op("act", lambda: A.copy(out=yT[sl][:, 0:4, :], in_=ps[0][:].rearrange("p (a b) -> p a b", a=4)),
                     r=[("ps", 0)], w=[("yT", sl, 0)])
                S.op("dve", lambda: V.tensor_copy(out=yT[sl][:, 4:8, :], in_=ps[1][:].rearrange("p (a b) -> p a b", a=4)),
                     r=[("ps", 1)], w=[("yT", sl, 1)])
                if stage < 2:
                    continue
                for half in range(2):
                    for kc in range(8):
                        S.op("pe", lambda kc=kc, half=half: P.matmul(ps[2 + half][:], lhsT=yT[sl][:, kc, :],
                                                                     rhs=wout[:, kc, half * 512:(half + 1) * 512],
                                                                     start=(kc == 0), stop=(kc == 7)),
                             r=[("yT", sl, kc // 4), "wout"], w=[("ps", 2 + half)])
                if stage < 3:
                    continue
                for half in range(2):
                    S.op("dve", lambda half=half: V.scalar_tensor_tensor(out=zt[sl][:, half * 512:(half + 1) * 512],
                                                                         in0=hpt[sl][:, half * 512:(half + 1) * 512], scalar=ALPHA,
                                                                         in1=ps[2 + half][:], op0=ALU.mult, op1=ALU.add),
                         r=[("hp", sl), ("ps", 2 + half)], w=[("z", sl)])
                layernorm(zt[sl][:], zt[sl][:], 0, ("z", sl))
                if stage < 4:
                    continue
                S.op("act", lambda: A.mul(out=yacc[:, t, :], in_=zt[sl][:], mul=ALPHA), r=[("z", sl)], w=[("yacc", t)])
                for hb in range(2):
                    for j in range(4):
                        kc = hb * 4 + j
                        S.op("pe", lambda kc=kc, j=j, hb=hb: P.transpose(out=ps[4 + hb][:, j * 128:(j + 1) * 128],
                                                                        in_=zt[sl][:, kc * 128:(kc + 1) * 128], identity=ident[:]),
                             r=[("z", sl), "ident"], w=[("ps", 4 + hb)])
                for hb in range(2):
                    S.op("act", lambda hb=hb: A.copy(out=hT32[sl][:, hb * 4:(hb + 1) * 4, :],
                                                     in_=ps[4 + hb][:].rearrange("p (a b) -> p a b", a=4)),
                         r=[("ps", 4 + hb)], w=[("hT32", sl, hb)])
                    S.op("dve", lambda hb=hb: V.tensor_copy(out=hT[:, hb * 4:(hb + 1) * 4, t * 128:(t + 1) * 128],
                                                            in_=ps[4 + hb][:].rearrange("p (a b) -> p a b", a=4)),
                         r=[("ps", 4 + hb)], w=[("hT", t)])
                if stage < 5:
                    continue
                for kc in range(8):
                    S.op("pe", lambda kc=kc: P.matmul(ps[6][:, 0:20], lhsT=hT32[sl][:, kc, :], rhs=wr[:, kc, :],
                                                      start=(kc == 0), stop=(kc == 7)),
                         r=[("hT32", sl, kc // 4), "wr"], w=[("ps", 6)])
                if not do_R:
                    continue
                RT = ["rt"]
                S.op("dve", lambda: V.tensor_tensor(out=lg[:], in0=ps[6][:, 0:20], in1=rb[:], op=ALU.add),
                     r=[("ps", 6), "rb"], w=RT)
                S.op("dve", lambda: V.tensor_reduce(out=r_gmax[:], in_=lg[:, 0:4], axis=AX.X, op=ALU.max), r=RT, w=RT)
                S.op("dve", lambda: V.tensor_scalar(out=r_gmask[:], in0=lg[:, 0:4], scalar1=r_gmax[:], scalar2=None,
                                                    op0=ALU.is_ge), r=RT, w=RT)
                S.op("dve", lambda: V.tensor_scalar(out=r_gt[:], in0=lg[:, 0:4], scalar1=r_gmax[:], scalar2=None,
                                                    op0=ALU.subtract), r=RT, w=RT)
                S.op("act", lambda: A.activation(out=r_gt[:], in_=r_gt[:], func=AF.Exp, accum_out=r_gsum[:]), r=RT, w=RT)
                S.op("dve", lambda: V.reciprocal(out=r_gsum[:], in_=r_gsum[:]), r=RT, w=RT)
                S.op("dve", lambda: V.tensor_scalar(out=r_gs[:], in0=r_gmask[:], scalar1=r_gsum[:], scalar2=None,
                                                    op0=ALU.mult), r=RT, w=RT)
                ev = lg[:, 4:20].rearrange("p (g j) -> p g j", g=4)
                S.op("dve", lambda: V.tensor_reduce(out=r_m1[:], in_=ev, axis=AX.X, op=ALU.max), r=RT, w=RT)
                S.op("dve", lambda: V.tensor_tensor(out=r_is1[:], in0=ev, in1=r_m1[:].unsqueeze(2).to_broadcast([128, 4, 4]),
                                                    op=ALU.is_equal), r=RT, w=RT)
                S.op("dve", lambda: V.scalar_tensor_tensor(out=r_e2[:], in0=r_is1[:], scalar=-1e30, in1=ev,
                                                           op0=ALU.mult, op1=ALU.add), r=RT, w=RT)
                S.op("dve", lambda: V.tensor_reduce(out=r_m2[:], in_=r_e2[:], axis=AX.X, op=ALU.max), r=RT, w=RT)
                S.op("dve", lambda: V.tensor_tensor(out=r_is2[:], in0=r_e2[:], in1=r_m2[:].unsqueeze(2).to_broadcast([128, 4, 4]),
                                                    op=ALU.is_equal), r=RT, w=RT)
                S.op("dve", lambda: V.tensor_tensor(out=r_w1[:], in0=r_m2[:], in1=r_m1[:], op=ALU.subtract), r=RT, w=RT)
                S.op("act", lambda: A.activation(out=r_w1[:], in_=r_w1[:], func=AF.Exp), r=RT, w=RT)
                S.op("dve", lambda: V.tensor_scalar_add(out=r_w1[:], in0=r_w1[:], scalar1=1.0), r=RT, w=RT)
                S.op("dve", lambda: V.reciprocal(out=r_w1[:], in_=r_w1[:]), r=RT, w=RT)
                S.op("dve", lambda: V.tensor_scalar(out=r_w2[:], in0=r_w1[:], scalar1=-1.0, scalar2=1.0,
                                                    op0=ALU.mult, op1=ALU.add), r=RT, w=RT)
                S.op("dve", lambda: V.tensor_tensor(out=r_w1[:], in0=r_w1[:], in1=r_gs[:], op=ALU.mult), r=RT, w=RT)
                S.op("dve", lambda: V.tensor_tensor(out=r_w2[:], in0=r_w2[:], in1=r_gs[:], op=ALU.mult), r=RT, w=RT)
                S.op("dve", lambda: V.tensor_tensor(out=r_is1[:], in0=r_is1[:], in1=r_w1[:].unsqueeze(2).to_broadcast([128, 4, 4]),
                                                    op=ALU.mult), r=RT, w=RT)
                S.op("dve", lambda: V.tensor_tensor(out=r_is2[:], in0=r_is2[:], in1=r_w2[:].unsqueeze(2).to_broadcast([128, 4, 4]),
                                                    op=ALU.mult), r=RT, w=RT)
                S.op("dve", lambda: V.tensor_tensor(out=comb[:, t, :].rearrange("p (g j) -> p g j", g=4), in0=r_is1[:], in1=r_is2[:],
                                                    op=ALU.add), r=RT, w=[("comb", t)])
            for e in range(NEXP if do_B else 0):
                slot = nload % 2
                if pending and e in (3, 6, 9, 12):
                    chunk_done(pending.pop(0))
                nload += 1
                if e + 1 < NEXP:
                    load_expert(e + 1, nload % 2)
                for g in range(GPS):
                    hs = g % 2
                    for fc in range(4):
                        pg = fc % 2
                        for kc in range(8):
                            S.op("pe", lambda kc=kc, fc=fc, pg=pg: P.matmul(ps[pg][:], lhsT=wg[slot][:, kc, fc * 128:(fc + 1) * 128],
                                                                          rhs=hT[:, kc, g * 512:(g + 1) * 512],
                                                                          start=(kc == 0), stop=(kc == 7)),
                                 r=[("wg", slot)] + [("hT", g * 4 + i) for i in range(4)], w=[("ps", pg)])
                        for kc in range(8):
                            S.op("pe", lambda kc=kc, fc=fc, pg=pg: P.matmul(ps[2 + pg][:], lhsT=wu[slot][:, kc, fc * 128:(fc + 1) * 128],
                                                                          rhs=hT[:, kc, g * 512:(g + 1) * 512],
                                                                          start=(kc == 0), stop=(kc == 7)),
                                 r=[("wu", slot)] + [("hT", g * 4 + i) for i in range(4)], w=[("ps", 2 + pg)])
                        S.op("act", lambda pg=pg: A.activation(out=sg[pg][:], in_=ps[pg][:], func=AF.Silu),
                             r=[("ps", pg)], w=[("sg", pg)])
                        S.op("dve", lambda pg=pg, fc=fc: V.tensor_tensor(out=hdn[hs][:, fc, :], in0=ps[2 + pg][:], in1=sg[pg][:], op=ALU.mult),
                             r=[("ps", 2 + pg), ("sg", pg)], w=[("hdn", hs, fc)])
                    for tt in range(4):
                        t = g * 4 + tt
                        for half in range(2):
                            pb = 4 + (tt * 2 + half) % 4
                            for fc in range(4):
                                S.op("pe", lambda fc=fc, half=half, pb=pb, tt=tt: P.matmul(ps[pb][:], lhsT=hdn[hs][:, fc, tt * 128:(tt + 1) * 128],
                                                                                       rhs=wd[slot][:, fc, half * 512:(half + 1) * 512],
                                                                                       start=(fc == 0), stop=(fc == 3)),
                                     r=[("hdn", hs, fc), ("wd", slot)], w=[("ps", pb)])
                            S.op("dve", lambda half=half, pb=pb, t=t: V.scalar_tensor_tensor(
                                out=yacc[:, t, half * 512:(half + 1) * 512], in0=ps[pb][:], scalar=comb[:, t, e:e + 1],
                                in1=yacc[:, t, half * 512:(half + 1) * 512], op0=ALU.mult, op1=ALU.add),
                                r=[("ps", pb), ("comb", t), ("yacc", t)], w=[("yacc", t)])
            for t in range(TPS):
                tok0 = s * SG + t * 128
                sl = t % 2
                src = yacc[:, t, :]
                for c in range(2):
                    S.op("dve", lambda c=c: V.bn_stats(out=st[:, c, :], in_=src[:, c * 512:(c + 1) * 512]), r=[("yacc", t)], w=["st"])
                S.op("dve", lambda: V.bn_aggr(out=mv[:], in_=st[:].rearrange("p a b -> p (a b)")), r=["st"], w=["mv"])
                S.op("dve", lambda: V.tensor_scalar_add(out=rstd[:], in0=mv[:, 1:2], scalar1=LN_EPS), r=["mv"], w=["rstd"])
                S.op("act", lambda: A.activation(out=rstd[:], in_=rstd[:], func=AF.Ln), r=["rstd"], w=["rstd"])
                S.op("act", lambda: A.activation(out=rstd[:], in_=rstd[:], func=AF.Exp, scale=-0.5), r=["rstd"], w=["rstd"])
                S.op("dve", lambda: V.scalar_tensor_tensor(out=ot[sl][:], in0=src, scalar=mv[:, 0:1], in1=lnp[:, 2, :], op0=ALU.subtract, op1=ALU.mult),
                     r=["mv", "lnp", ("yacc", t)], w=[("ot", sl)])
                S.op("dve", lambda: V.scalar_tensor_tensor(out=ot[sl][:], in0=ot[sl][:], scalar=rstd[:], in1=lnp[:, 3, :], op0=ALU.mult, op1=ALU.add),
                     r=["rstd", "lnp", ("ot", sl)], w=[("ot", sl)])
                S.dma("sp", out_d[tok0:tok0 + 128, :], ot[sl][:], r=[("ot", sl)], sem=("ot", sl))
                if t % 2 == 1 and chunk_done:
                    pending.append(tok0 // 256)
        while pending:
            chunk_done(pending.pop(0))


def emit_gla(nc, S, ps, DD, tag, S_LEN=16384):
    D = 1024
    NT = S_LEN // 128
    h_d, wcat_d, wg2_d, bg_d, gn_d = DD["h"], DD["wcat"], DD["wg2"], DD["bg"], DD["gn"]
    id_d, tr_d, ci_d, on_d, y_d = DD["ident"], DD["trirev"], DD["cind"], DD["ones1"], DD["yout"]
    hmap = DD.get("hmap", lambda n: n)
    chunk_done = DD.get("chunk_done")
    with ExitStack() as es:
        def sb(name, shape, dt=F32):
            return es.enter_context(nc.sbuf_tensor("sb_" + tag + "_" + name, shape, dt))
        ident = sb("ident", [128, 128])
        trirev = sb("trirev", [128, 128])
        cind = sb("cind", [128, 2])
        ones1 = sb("ones1", [1, 128])
        wcat = sb("wcat", [128, 8, 784], BF16)
        wg2 = sb("wg2", [16, 128])
        bg = sb("bg", [1, 128])
        gn = sb("gn", [128, 256])
        state = sb("state", [128, 256])
        qlo = sb("qlo", [128, 128])
        qhi = sb("qhi", [128, 128])
        ht = [sb("ht%d" % i, [128, D]) for i in range(2)]
        hT = [sb("hT%d" % i, [128, 8, 128], BF16) for i in range(2)]
        lrT = sb("lrT", [16, 128])
        la = sb("la", [128, 128])
        kd = sb("kd", [128, 128])
        dec = sb("dec", [128, 2])
        kdec = sb("kdec", [128, 128], BF16)
        vbf = sb("vbf", [128, 256], BF16)
        er = sb("er", [128, 256])
        junk = sb("junk", [128, 256])
        ss = sb("ss", [128, 1])
        yo = [sb("yo%d" % i, [128, 256]) for i in range(2)]
        V, A, P, G = nc.vector, nc.scalar, nc.tensor, nc.gpsimd

        S.dma("sp", ident[:], id_d[:, :], w=["ident"], sem="ident")
        S.dma("sp", trirev[:], tr_d[:, :], w=["trirev"], sem="trirev")
        S.dma("sp", cind[:], ci_d[:, :], w=["cind"], sem="cind")
        S.dma("sp", ones1[:], on_d[:, :], w=["ones1"], sem="ones1")
        S.dma("pool", wcat[:], wcat_d.rearrange("(kc p) f -> p kc f", p=128), w=["wcat"], sem="wcat")
        S.dma("sp", wg2[:], wg2_d[:, :], w=["wg2"], sem="wg2")
        S.dma("sp", bg[:], bg_d[:, :], w=["bg"], sem="bg")
        S.dma("sp", gn[:], gn_d[0:1, :].partition_broadcast(128), w=["gn"], sem="gn")
        S.op("dve", lambda: V.memset(state[:], 0.0), w=["state"])
        S.op("dve", lambda: V.memset(qlo[:], 0.0), w=["qlo"])
        S.op("dve", lambda: V.memset(qhi[:], 0.0), w=["qhi"])

        for t in range(NT):
            tok0 = t * 128
            sl = t % 2
            if t == 0:
                S.dma("sp", ht[0][:], h_d[hmap(0):hmap(0) + 128, :], w=[("ht", 0)], sem=("ht", 0))
            if t + 1 < NT:
                tn = (t + 1) * 128
                S.dma("sp", ht[(t + 1) % 2][:], h_d[hmap(tn):hmap(tn) + 128, :], w=[("ht", (t + 1) % 2)], sem=("ht", (t + 1) % 2))
            for hb in range(2):
                for j in range(4):
                    kc = hb * 4 + j
                    S.op("pe", lambda: P.transpose(out=ps[hb][:, j * 128:(j + 1) * 128],
                                                   in_=ht[sl][:, kc * 128:(kc + 1) * 128], identity=ident[:]),
                         r=[("ht", sl), "ident"], w=[("ps", hb)])
            S.op("act", lambda: A.copy(out=hT[sl][:, 0:4, :], in_=ps[0][:].rearrange("p (a b) -> p a b", a=4)),
                 r=[("ps", 0)], w=[("hT", sl, 0)])
            S.op("dve", lambda: V.tensor_copy(out=hT[sl][:, 4:8, :], in_=ps[1][:].rearrange("p (a b) -> p a b", a=4)),
                 r=[("ps", 1)], w=[("hT", sl, 1)])
            for kc in range(8):
                S.op("pe", lambda: P.matmul(ps[2][:, 0:128], lhsT=wcat[:, kc, 0:128], rhs=hT[sl][:, kc, :],
                                            start=(kc == 0), stop=(kc == 7)),
                     r=["wcat", ("hT", sl, kc // 4)], w=[("ps", 2)])
            for kc in range(8):
                S.op("pe", lambda: P.matmul(ps[2][0:16, 128:256], lhsT=wcat[:, kc, 768:784], rhs=hT[sl][:, kc, :],
                                            start=(kc == 0), stop=(kc == 7)),
                     r=["wcat", ("hT", sl, kc // 4)], w=[("ps", 2)])
            for kc in range(8):
                S.op("pe", lambda: P.matmul(ps[3][:, 0:384], lhsT=hT[sl][:, kc, :], rhs=wcat[:, kc, 128:512],
                                            start=(kc == 0), stop=(kc == 7)),
                     r=["wcat", ("hT", sl, kc // 4)], w=[("ps", 3)])
            for kc in range(8):
                S.op("pe", lambda: P.matmul(ps[4][:, 0:256], lhsT=hT[sl][:, kc, :], rhs=wcat[:, kc, 512:768],
                                            start=(kc == 0), stop=(kc == 7)),
                     r=["wcat", ("hT", sl, kc // 4)], w=[("ps", 4)])
            S.op("act", lambda: A.mul(out=qlo[:, 0:64], in_=ps[2][:, 0:64], mul=128 ** -0.5), r=[("ps", 2)], w=["qlo"])
            S.op("act", lambda: A.mul(out=qhi[:, 64:128], in_=ps[2][:, 64:128], mul=128 ** -0.5), r=[("ps", 2)], w=["qhi"])
            S.op("dve", lambda: V.tensor_copy(out=lrT[:], in_=ps[2][0:16, 128:256]), r=[("ps", 2)], w=["lrT"])
            S.op("pe", lambda: P.matmul(ps[2][:, 0:128], lhsT=lrT[:], rhs=wg2[:], start=True, stop=False),
                 r=["lrT", "wg2"], w=[("ps", 2)])
            S.op("pe", lambda: P.matmul(ps[2][:, 0:128], lhsT=ones1[:], rhs=bg[:], start=False, stop=True),
                 r=["ones1", "bg"], w=[("ps", 2)])
            S.op("act", lambda: A.activation(out=la[:], in_=ps[2][:, 0:128], func=AF.Exp, scale=-1.0), r=[("ps", 2)], w=["la"])
            S.op("dve", lambda: V.tensor_scalar_add(out=la[:], in0=la[:], scalar1=1.0), r=["la"], w=["la"])
            S.op("act", lambda: A.activation(out=la[:], in_=la[:], func=AF.Ln), r=["la"], w=["la"])
            S.op("pe", lambda: P.matmul(ps[2][:, 128:256], lhsT=trirev[:], rhs=la[:], start=True, stop=True),
                 r=["trirev", "la"], w=[("ps", 2)])
            S.op("pe", lambda: P.matmul(ps[2][:, 256:258], lhsT=la[:], rhs=cind[:], start=True, stop=True),
                 r=["cind", "la"], w=[("ps", 2)])
            S.op("act", lambda: A.activation(out=kd[:], in_=ps[2][:, 128:256], func=AF.Exp), r=[("ps", 2)], w=["kd"])
            S.op("act", lambda: A.activation(out=dec[:], in_=ps[2][:, 256:258], func=AF.Exp), r=[("ps", 2)], w=["dec"])
            S.op("dve", lambda: V.tensor_tensor(out=kdec[:], in0=ps[3][:, 0:128], in1=kd[:], op=ALU.mult),
                 r=[("ps", 3), "kd"], w=["kdec"])
            S.op("act", lambda: A.copy(out=vbf[:], in_=ps[3][:, 128:384]), r=[("ps", 3)], w=["vbf"])
            for c in range(2):
                pb = 5 + c
                S.op("pe", lambda: P.matmul(ps[pb][:, 0:256], lhsT=kdec[c * 64:(c + 1) * 64, :], rhs=vbf[c * 64:(c + 1) * 64, :],
                                            start=True, stop=True),
                     r=["kdec", "vbf"], w=[("ps", pb)])
            for c in range(2):
                pb = 5 + c
                S.op("dve", lambda: V.scalar_tensor_tensor(out=state[:], in0=state[:], scalar=dec[:, c:c + 1], in1=ps[pb][:, 0:256],
                                                           op0=ALU.mult, op1=ALU.add),
                     r=["dec", ("ps", pb), "state"], w=["state"])
                S.op("pe", lambda: P.matmul(ps[7][:, 0:256], lhsT=(qlo if c == 0 else qhi)[:], rhs=state[:],
                                            start=(c == 0), stop=(c == 1)),
                     r=["state", "qlo" if c == 0 else "qhi"], w=[("ps", 7)])
            S.op("act", lambda: A.activation(out=er[:], in_=ps[4][:, 0:256], func=AF.Exp, scale=-1.0), r=[("ps", 4)], w=["er"])
            S.op("dve", lambda: V.tensor_scalar_add(out=er[:], in0=er[:], scalar1=1.0), r=["er"], w=["er"])
            S.op("dve", lambda: V.reciprocal(out=er[:], in_=er[:]), r=["er"], w=["er"])
            S.op("dve", lambda: V.tensor_tensor(out=er[:], in0=ps[4][:, 0:256], in1=er[:], op=ALU.mult), r=[("ps", 4), "er"], w=["er"])
            S.op("dve", lambda: V.tensor_tensor(out=er[:], in0=er[:], in1=gn[:], op=ALU.mult), r=["er", "gn"], w=["er"])
            S.op("act", lambda: A.activation(out=junk[:], in_=ps[7][:, 0:256], func=AF.Square, accum_out=ss[:]),
                 r=[("ps", 7)], w=["junk", "ss"])
            S.op("dve", lambda: V.tensor_scalar(out=ss[:], in0=ss[:], scalar1=1.0 / 256, scalar2=LN_EPS, op0=ALU.mult, op1=ALU.add),
                 r=["ss"], w=["ss"])
            S.op("act", lambda: A.activation(out=ss[:], in_=ss[:], func=AF.Ln), r=["ss"], w=["ss"])
            S.op("act", lambda: A.activation(out=ss[:], in_=ss[:], func=AF.Exp, scale=-0.5), r=["ss"], w=["ss"])
            S.op("dve", lambda: V.scalar_tensor_tensor(out=yo[sl][:], in0=ps[7][:, 0:256], scalar=ss[:], in1=er[:],
                                                       op0=ALU.mult, op1=ALU.mult),
                 r=[("ps", 7), "ss", "er"], w=[("yo", sl)])
            S.dma("sp", y_d[tok0:tok0 + 128, :], yo[sl][:], r=[("yo", sl)], sem=("yo", sl))
            if (t + 1) % 8 == 0 and chunk_done:
                chunk_done(t // 8)


def emit_att(nc, S, ps, DD, tag, S_LEN=16384):
    D = 1024
    NT = S_LEN // 128
    NG = S_LEN // 512
    h_d, hkv_d, wq_d, wk_d, wv_d = DD["h"], DD["hkv"], DD["wq"], DD["wk"], DD["wv"]
    bt_d, cfar_d, lam_d, gsub_d, cst_d = DD["biasT"], DD["cfar"], DD["lamv"], DD["gsub"], DD["cst"]
    id_d, on_d, y_d = DD["ident"], DD["ones128"], DD["yout"]
    hmap = DD.get("hmap", lambda n: n)
    chunk_done = DD.get("chunk_done")
    with ExitStack() as es:
        def sb(name, shape, dt=F32):
            return es.enter_context(nc.sbuf_tensor("sb_" + tag + "_" + name, shape, dt))
        ident = sb("ident", [128, 128])
        wq = sb("wq", [128, 8, 256], BF16)
        wk = sb("wk", [128, 8, 256], BF16)
        wv = sb("wv", [128, 8, 256], BF16)
        bt = sb("bt", [128, 4, 128])
        cfar = sb("cfar", [128, 2])
        lamv = sb("lamv", [128, 4, 64])
        lam = sb("lam", [128, 4])
        cst = sb("cst", [128, 2])
        KT = [sb("KT%d" % i, [128, S_LEN], BF16) for i in range(2)]
        VV = [sb("V%d" % i, [128, NT, 128], BF16) for i in range(2)]
        QT = [[sb("QT%d_%d" % (i, s), [128, 512], BF16) for s in range(2)] for i in range(2)]
        ht = [sb("ht%d" % i, [128, D]) for i in range(2)]
        hT = sb("hT", [128, 8, 512], BF16)
        NPT = 6
        PT = [sb("PT%d" % i, [128, 512], BF16) for i in range(NPT)]
        tmp = [sb("tmp%d" % i, [128, 128]) for i in range(2)]
        ones = sb("ones", [128, 128])
        gcol = sb("gcol", [128, 1])
        Pacc = [[sb("Pacc%d_%d" % (m, i), [128, 512]) for i in range(2)] for m in range(2)]
        rinv = [sb("rinv%d" % m, [128, 512]) for m in range(2)]
        oT = sb("oT", [128, 512])
        o2 = sb("o2", [128, 512])
        yT = [sb("yT%d" % i, [128, 512]) for i in range(2)]
        yo = [sb("yo%d" % i, [128, 4, 128]) for i in range(2)]
        V, A, P, G = nc.vector, nc.scalar, nc.tensor, nc.gpsimd

        S.dma("sp", ident[:], id_d[:, :], w=["ident"], sem="ident")
        S.dma("pool", wq[:], wq_d.rearrange("(kc p) f -> p kc f", p=128), w=["wq"], sem="wq")
        S.dma("pool", wk[:], wk_d.rearrange("(kc p) f -> p kc f", p=128), w=["wk"], sem="wk")
        S.dma("pool", wv[:], wv_d.rearrange("(kc p) f -> p kc f", p=128), w=["wv"], sem="wv")
        S.dma("sp", bt[:], bt_d.rearrange("h t k q -> k (h t) q"), w=["bt"], sem="bt")
        S.dma("sp", cfar[:], cfar_d[:, :], w=["cfar"], sem="cfar")
        S.dma("sp", cst[:], cst_d[:, :], w=["cst"], sem="cst")
        S.dma("sp", lamv[:], lam_d.partition_broadcast(128), w=["lamv"], sem="lamv")
        S.dma("sp", ones[:], on_d[:, :], w=["ones"], sem="ones")
        S.dma("sp", gcol[:], gsub_d.rearrange("o d -> d o"), w=["gcol"], sem="gcol")
        L = ["lam"]
        S.op("dve", lambda: V.tensor_tensor(out=lamv[:, 0, :], in0=lamv[:, 0, :], in1=lamv[:, 1, :], op=ALU.mult), r=["lamv"], w=["lamv"])
        S.op("dve", lambda: V.tensor_tensor(out=lamv[:, 2, :], in0=lamv[:, 2, :], in1=lamv[:, 3, :], op=ALU.mult), r=["lamv"], w=["lamv"])
        S.op("dve", lambda: V.tensor_reduce(out=lam[:, 0:1], in_=lamv[:, 0, :], axis=AX.X, op=ALU.add), r=["lamv"], w=L)
        S.op("dve", lambda: V.tensor_reduce(out=lam[:, 1:2], in_=lamv[:, 2, :], axis=AX.X, op=ALU.add), r=["lamv"], w=L)
        S.op("act", lambda: A.activation(out=lam[:, 0:2], in_=lam[:, 0:2], func=AF.Exp), r=L, w=L)
        S.op("dve", lambda: V.tensor_tensor(out=lam[:, 2:3], in0=lam[:, 1:2], in1=lam[:, 0:1], op=ALU.subtract), r=L, w=L)
        S.op("dve", lambda: V.tensor_tensor(out=lam[:, 3:4], in0=lam[:, 2:3], in1=cst[:, 0:1], op=ALU.subtract), r=L + ["cst"], w=L)
        S.op("dve", lambda: V.tensor_tensor(out=gcol[:], in0=gcol[:], in1=cst[:, 1:2], op=ALU.mult), r=["gcol", "cst"], w=["gcol"])

        def load_hT(src_d, g):
            for tt in range(4):
                tok0 = g * 512 + tt * 128
                sl = tt % 2
                S.dma("sp", ht[sl][:], src_d[hmap(tok0):hmap(tok0) + 128, :], w=[("ht", sl)], sem=("ht", sl))
                for hb in range(2):
                    for j in range(4):
                        kc = hb * 4 + j
                        S.op("pe", lambda: P.transpose(out=ps[6 + hb][:, j * 128:(j + 1) * 128],
                                                       in_=ht[sl][:, kc * 128:(kc + 1) * 128], identity=ident[:]),
                             r=[("ht", sl), "ident"], w=[("ps", 6 + hb)])
                S.op("act", lambda: A.copy(out=hT[:, 0:4, tt * 128:(tt + 1) * 128], in_=ps[6][:].rearrange("p (a b) -> p a b", a=4)),
                     r=[("ps", 6)], w=[("hT", tt)])
                S.op("dve", lambda: V.tensor_copy(out=hT[:, 4:8, tt * 128:(tt + 1) * 128], in_=ps[7][:].rearrange("p (a b) -> p a b", a=4)),
                     r=[("ps", 7)], w=[("hT", tt)])
        HTK = [("hT", i) for i in range(4)]

        kv_load, kv_store = DD.get("kv_load"), DD.get("kv_store")
        if kv_load is None:
            for g in range(NG):
                load_hT(hkv_d, g)
                for i in range(2):
                    for kc in range(8):
                        S.op("pe", lambda: P.matmul(ps[6][:], lhsT=wk[:, kc, i * 128:(i + 1) * 128], rhs=hT[:, kc, :],
                                                    start=(kc == 0), stop=(kc == 7)), r=["wk"] + HTK, w=[("ps", 6)])
                    S.op("act" if i == 0 else "dve",
                         (lambda: A.copy(out=KT[i][:, g * 512:(g + 1) * 512], in_=ps[6][:])) if i == 0 else
                         (lambda: V.tensor_copy(out=KT[i][:, g * 512:(g + 1) * 512], in_=ps[6][:])),
                         r=[("ps", 6)], w=[("KT", i)])
                for tt in range(4):
                    for kc in range(8):
                        S.op("pe", lambda: P.matmul(ps[7][:, 0:256], lhsT=hT[:, kc, tt * 128:(tt + 1) * 128], rhs=wv[:, kc, :],
                                                    start=(kc == 0), stop=(kc == 7)), r=["wv"] + HTK, w=[("ps", 7)])
                    S.op("act", lambda: A.copy(out=VV[0][:, g * 4 + tt, 0:128], in_=ps[7][:, 0:128]), r=[("ps", 7)], w=[("V", 0)])
                    S.op("dve", lambda: V.tensor_copy(out=VV[1][:, g * 4 + tt, 0:128], in_=ps[7][:, 128:256]), r=[("ps", 7)], w=[("V", 1)])


            if kv_store is not None:
                for i in range(2):
                    S.dma("sp", kv_store[i], KT[i][:], r=[("KT", i)], sem=("kvst", i))
                    S.dma("sp", kv_store[2 + i].rearrange("p (t d) -> p t d", d=128), VV[i][:], r=[("V", i)], sem=("kvst", 2 + i))
        else:
            for i in range(2):
                S.dma("sp", KT[i][:], kv_load[i], w=[("KT", i)], sem=("kvld", i))
                S.dma("sp", VV[i][:], kv_load[2 + i].rearrange("p (t d) -> p t d", d=128), w=[("V", i)], sem=("kvld", 2 + i))
        state = {"pt": 0, "sb": 0}
        SBANK = [(0, 1), (4, 5)]
        RS = 7
        pinit = {}

        def emit_S(it):
            (g, hd, j, qs) = it
            c0 = max(0, j - 4 * g)
            banks = SBANK[state["sb"] % 2]
            state["sb"] += 1
            ptis = (state["pt"] % NPT, (state["pt"] + 1) % NPT)
            state["pt"] += 2
            for mp in range(2):
                lo = mp * 64
                S.op("pe", lambda: P.matmul(ps[banks[mp]][:, c0 * 128:512], lhsT=KT[hd][lo:lo + 64, j * 128:(j + 1) * 128],
                                            rhs=QT[hd][qs][lo:lo + 64, c0 * 128:512], start=True, stop=True),
                     r=[("KT", hd), ("QT", hd, qs)], w=[("ps", banks[mp])])
            for mp in range(2):
                sbk = banks[mp]
                pti = ptis[mp]
                if j < 4 * g - 1:
                    S.op("act", lambda: A.activation(out=PT[pti][:], in_=ps[sbk][:], func=AF.Exp, scale=0.125, bias=cfar[:, hd:hd + 1]),
                         r=[("ps", sbk), "cfar"], w=[("PT", pti)])
                else:
                    for c in range(c0, 4):
                        i = 4 * g + c
                        cs = slice(c * 128, (c + 1) * 128)
                        if j < i - 1:
                            S.op("act", lambda: A.activation(out=PT[pti][:, cs], in_=ps[sbk][:, cs], func=AF.Exp, scale=0.125,
                                                             bias=cfar[:, hd:hd + 1]),
                                 r=[("ps", sbk), "cfar"], w=[("PT", pti)])
                        else:
                            ty = 0 if j == i else 1
                            tb = (c + mp) % 2
                            S.op("dve", lambda: V.scalar_tensor_tensor(out=tmp[tb][:], in0=ps[sbk][:, cs], scalar=0.125,
                                                                       in1=bt[:, hd * 2 + ty, :], op0=ALU.mult, op1=ALU.add),
                                 r=[("ps", sbk), "bt"], w=[("tmp", tb)])
                            S.op("act", lambda: A.activation(out=PT[pti][:, cs], in_=tmp[tb][:], func=AF.Exp),
                                 r=[("tmp", tb)], w=[("PT", pti)])
            return (c0, ptis)

        def emit_PV(it, info):
            (g, hd, j, qs) = it
            (c0, ptis) = info
            cs = slice(c0 * 128, 512)
            for mp in range(2):
                pti = ptis[mp]
                S.op("pe", lambda: P.matmul(ps[2 + mp][:, cs], lhsT=VV[hd][:, j, :], rhs=PT[pti][:, cs],
                                            start=(j == 0), stop=(j == 4 * g + 3), skip_group_check=True),
                     r=[("PT", pti), ("V", hd)], w=[("ps", 2 + mp)])
            for mp in range(2):
                pti = ptis[mp]
                a = 1 if (2 * j + mp) % 3 == 0 else 0
                eng, E = ("dve", V) if a == 0 else ("pool", G)
                key = (g, hd, mp, a)
                if key not in pinit:
                    pinit[key] = True
                    if c0 > 0:
                        S.op(eng, lambda: E.memset(Pacc[mp][a][:, 0:c0 * 128], 0.0), w=[("Pacc", mp, a)])
                    S.op(eng, lambda: E.tensor_copy(out=Pacc[mp][a][:, cs], in_=PT[pti][:, cs]), r=[("PT", pti)], w=[("Pacc", mp, a)])
                else:
                    S.op(eng, lambda: E.tensor_tensor(out=Pacc[mp][a][:, cs], in0=Pacc[mp][a][:, cs], in1=PT[pti][:, cs], op=ALU.add),
                         r=[("PT", pti), ("Pacc", mp, a)], w=[("Pacc", mp, a)])
            if j == 4 * g + 3:
                for mp in range(2):
                    accs = [a2 for a2 in range(2) if (g, hd, mp, a2) in pinit]
                    for n2, a2 in enumerate(accs):
                        S.op("pe", lambda: P.matmul(ps[RS][:], lhsT=ones[:], rhs=Pacc[mp][a2][:], start=(n2 == 0), stop=(n2 == len(accs) - 1)),
                             r=["ones", ("Pacc", mp, a2)], w=[("ps", RS)])
                    S.op("dve", lambda: V.reciprocal(out=rinv[mp][:], in_=ps[RS][:]), r=[("ps", RS)], w=[("rinv", mp)])

        def epilogue(g, hd, ys):
            S.op("dve", lambda: V.tensor_tensor(out=oT[:], in0=ps[2][:], in1=rinv[0][:], op=ALU.mult), r=[("ps", 2), ("rinv", 0)], w=["oT"])
            S.op("dve", lambda: V.tensor_tensor(out=o2[:], in0=ps[3][:], in1=rinv[1][:], op=ALU.mult), r=[("ps", 3), ("rinv", 1)], w=["o2"])
            S.op("dve", lambda: V.scalar_tensor_tensor(out=oT[:], in0=o2[:], scalar=lam[:, 3:4], in1=oT[:], op0=ALU.mult, op1=ALU.add),
                 r=["o2", "oT", "lam"], w=["oT"])
            S.op("act", lambda: A.activation(out=o2[:], in_=oT[:], func=AF.Square), r=["oT"], w=["o2"])
            S.op("pe", lambda: P.matmul(ps[RS][:], lhsT=ones[:], rhs=o2[:], start=True, stop=True), r=["ones", "o2"], w=[("ps", RS)])
            S.op("dve", lambda: V.tensor_scalar(out=o2[:], in0=ps[RS][:], scalar1=1.0 / 128, scalar2=LN_EPS, op0=ALU.mult, op1=ALU.add),
                 r=[("ps", RS)], w=["o2"])
            S.op("act", lambda: A.activation(out=o2[:], in_=o2[:], func=AF.Ln), r=["o2"], w=["o2"])
            S.op("act", lambda: A.activation(out=o2[:], in_=o2[:], func=AF.Exp, scale=-0.5), r=["o2"], w=["o2"])
            S.op("dve", lambda: V.scalar_tensor_tensor(out=yT[hd][:], in0=oT[:], scalar=gcol[:, 0:1], in1=o2[:], op0=ALU.mult, op1=ALU.mult),
                 r=["oT", "o2", "gcol"], w=[("yT", hd)])
            for c in range(4):
                S.op("pe", lambda: P.transpose(out=ps[6][:, c * 128:(c + 1) * 128], in_=yT[hd][:, c * 128:(c + 1) * 128], identity=ident[:]),
                     r=[("yT", hd), "ident"], w=[("ps", 6)])
            S.op("act", lambda: A.copy(out=yo[hd][:], in_=ps[6][:].rearrange("p (c f) -> p c f", c=4)), r=[("ps", 6)], w=[("yo", hd)])
            S.dma("sp", y_d[g * 512:(g + 1) * 512, hd * 128:(hd + 1) * 128].rearrange("(c p) f -> p c f", p=128), yo[hd][:],
                  r=[("yo", hd)], sem=("yo", hd))

        def prep(g):
            qs = g % 2
            load_hT(h_d, g)
            for hd in range(2):
                for kc in range(8):
                    S.op("pe", lambda: P.matmul(ps[6][:], lhsT=wq[:, kc, hd * 128:(hd + 1) * 128], rhs=hT[:, kc, :],
                                                start=(kc == 0), stop=(kc == 7)), r=["wq"] + HTK, w=[("ps", 6)])
                S.op("dve", lambda: V.tensor_copy(out=QT[hd][qs][:], in_=ps[6][:]), r=[("ps", 6)], w=[("QT", hd, qs)])

        prep(0)
        for g in range(NG):
            qs = g % 2
            ys = g % 2
            if g >= 2 and g % 2 == 0 and chunk_done:
                chunk_done(g // 2 - 1)
            for hd in range(2):
                items = [(g, hd, j, qs) for j in range(4 * g + 4)]
                info = emit_S(items[0])
                for n in range(len(items)):
                    nxt = emit_S(items[n + 1]) if n + 1 < len(items) else None
                    emit_PV(items[n], info)
                    info = nxt
                    if hd == 1 and n == len(items) // 2 and g + 1 < NG:
                        prep(g + 1)
                epilogue(g, hd, ys)
        if chunk_done:
            chunk_done(NG // 2 - 1)


def build_fused():
    nc = bass.Bass("TRN2", target_bir_lowering=False)
    SL, D = 16384, 1024
    ext = lambda name, shape: nc.dram_tensor(name, shape, F32, kind="ExternalInput").ap()
    loc = lambda name, shape: nc.dram_tensor(name, shape, F32).ap()
    xb = ext("xb", [SL, D])
    xs = ext("xs", [4096, D])
    idx_d = nc.dram_tensor("idx", [128, 128], mybir.dt.uint32, kind="ExternalInput").ap()
    g_wcat, g_wg2, g_bg, g_gn = ext("g_wcat", [2, D, 784]), ext("g_wg2", [2, 16, 128]), ext("g_bg", [2, 1, 128]), ext("g_gn", [2, 1, 256])
    a_wq, a_wk, a_wv = ext("a_wq", [2, D, 256]), ext("a_wk", [D, 256]), ext("a_wv", [D, 256])
    a_bt, a_cfar, a_lamv = ext("a_biasT", [2, 2, 128, 128]), ext("a_cfar", [128, 2]), ext("a_lamv", [2, 4, 64])
    a_gsub, a_cst = ext("a_gsub", [2, 1, 128]), ext("a_cst", [2, 128, 2])
    p_wout, p_lnp, p_wr, p_rb = ext("p_wout", [4, D, D]), ext("p_lnp", [4, 4, D]), ext("p_wr", [4, D, 20]), ext("p_rb", [4, 1, 20])
    p_wg, p_wu, p_wd = ext("p_wg", [4, 16, D, 512]), ext("p_wu", [4, 16, D, 512]), ext("p_wd", [4, 16, 512, D])
    ident, trirev, cind = ext("ident", [128, 128]), ext("trirev", [128, 128]), ext("cind", [128, 2])
    ones1, ones128 = ext("ones1", [1, 128]), ext("ones128", [128, 128])
    out = nc.dram_tensor("out", [4096, D], F32, kind="ExternalOutput").ap()
    yloc, yg = loc("yloc", [SL, 256]), loc("yg", [4 * SL, 256])
    hloc = [loc("hloc%d" % i, [4096, D]) for i in range(3)]
    hg0, hkvg, hg2 = loc("hg0", [SL, D]), loc("hkvg", [SL, D]), loc("hg2", [SL, D])
    kvs = [nc.dram_tensor("kvs%d" % i, [128, SL], BF16).ap() for i in range(4)]
    GROUPS = [[0, 1, 2, 3], [4, 5, 6, 7]]

    def hperm(n):
        r_, w_ = n // 4096, n % 4096
        return (w_ // 256) * 1024 + r_ * 256 + (w_ % 256)

    def gather_y(S):
        S.coll_multi("AllGather", GROUPS, [(yloc[i * 1024:(i + 1) * 1024, :], yg[i * 4096:(i + 1) * 4096, :]) for i in range(16)])

    def gather_h(S, src, dst):
        S.coll_multi("AllGather", GROUPS, [(src[i * 256:(i + 1) * 256, :], dst[i * 1024:(i + 1) * 1024, :]) for i in range(16)])
    with ExitStack() as es:
        S = Sched(nc, es)
        ps = [es.enter_context(nc.psum_tensor("ps%d" % i, [128, 512], F32)) for i in range(8)]

        def ydone(i):
            S.coll_async("AllGather", GROUPS, yloc[i * 1024:(i + 1) * 1024, :], yg[i * 4096:(i + 1) * 4096, :], [("yo", 0), ("yo", 1)])

        def post(layer, ysrc, hp, dst, gdst=None):
            hdone = None
            if gdst is not None:
                hdone = lambda i: S.coll_async("AllGather", GROUPS, dst[i * 256:(i + 1) * 256, :], gdst[i * 1024:(i + 1) * 1024, :],
                                               [("ot", 0), ("ot", 1)])
            emit_post(nc, S, ps, {"y": ysrc[:, :], "idx": idx_d[:, :], "hp": hp, "chunk_done": hdone, "wout": p_wout[layer], "lnp": p_lnp[layer], "wr": p_wr[layer], "rb": p_rb[layer],
                                  "wg": p_wg[layer], "wu": p_wu[layer], "wd": p_wd[layer], "ident": ident, "out": dst},
                      "p%d" % layer)

        def gla(layer, h):
            emit_gla(nc, S, ps, {"h": h, "chunk_done": ydone, "hmap": (hperm if layer > 0 else (lambda n: n)), "wcat": g_wcat[layer], "wg2": g_wg2[layer], "bg": g_bg[layer], "gn": g_gn[layer],
                                 "ident": ident, "trirev": trirev, "cind": cind, "ones1": ones1, "yout": yloc}, "g%d" % layer)

        def att(j, h):
            emit_att(nc, S, ps, {"h": h, "hkv": hkvg, "chunk_done": ydone, "hmap": hperm, ("kv_store" if j == 0 else "kv_load"): kvs, "wq": a_wq[j], "wk": a_wk, "wv": a_wv, "biasT": a_bt, "cfar": a_cfar,
                                 "lamv": a_lamv[j], "gsub": a_gsub[j], "cst": a_cst[j], "ident": ident, "ones128": ones128,
                                 "yout": yloc}, "a%d" % j)

        import os
        LEVEL = int(os.environ.get("FUSED_LEVEL", "99"))
        steps = [
            lambda: gla(0, xb), lambda: S.barrier(), lambda: post(0, yg, xs, hloc[0], hg0), lambda: S.barrier(),
            lambda: gla(1, hg0), lambda: S.barrier(), lambda: post(1, yg, hloc[0], hloc[1], hkvg), lambda: S.barrier(),
            lambda: att(0, hkvg), lambda: S.barrier(), lambda: post(2, yg, hloc[1], hloc[2], hg2), lambda: S.barrier(),
            lambda: att(1, hg2), lambda: S.barrier(), lambda: post(3, yg, hloc[2], out),
        ]
        for i_, st_ in enumerate(steps):
            if i_ >= LEVEL:
                break
            st_()
        S.barrier()
    return nc


_NC = {}


def kernel(x, a_w_in, a_w_gate2, a_b_gate, a_g_norm, a_w_out, kv_w, b_w_q, b_lam_q1, b_lam_k1,
           b_lam_q2, b_lam_k2, b_g_sub, b_w_out, rel_table, moe_w_group, moe_b_group, moe_w_router,
           moe_b_router, moe_w_gate, moe_w_up, moe_w_down, ln_g, ln_b):
    f32 = np.float32
    A = lambda a: np.ascontiguousarray(np.asarray(a, dtype=f32))
    x = A(x)
    B, S_, D = x.shape
    if "nc" not in _NC:
        _NC["nc"] = build_fused()
    nc = _NC["nc"]
    w_in = A(a_w_in)
    kvw = A(kv_w)
    wqf = A(b_w_q)
    rt = A(rel_table)
    gconst = gla_consts()
    p_wout = A(np.stack([a_w_out[0], a_w_out[1], b_w_out[0], b_w_out[1]]))
    p_lnp = A(np.stack([np.stack([ln_g[l, 0], ln_b[l, 0], ln_g[l, 1], ln_b[l, 1]]) for l in range(4)]))
    p_wr = A(np.concatenate([moe_w_group, moe_w_router], axis=2))
    p_rb = A(np.concatenate([np.asarray(moe_b_group).reshape(4, -1), np.asarray(moe_b_router).reshape(4, -1)], axis=1)[:, None, :])
    p_wg, p_wu, p_wd = A(moe_w_gate), A(moe_w_up), A(moe_w_down)
    linits = [0.8 - 0.6 * math.exp(-0.3 * layer) for layer in (2, 3)]
    a_cst = A(np.stack([np.broadcast_to(np.array([[li, 1.0 - li]], f32), (128, 2)) for li in linits]))
    a_lamv = A(np.stack([np.stack([b_lam_q1[j], b_lam_k1[j], b_lam_q2[j], b_lam_k2[j]]) for j in range(2)]))
    a_gsub = A(np.asarray(b_g_sub)[:, None, :])
    in_maps = []
    for c in range(8):
        b, r = c // 4, c % 4
        hd = r
        g_wcat = np.stack([np.concatenate([w_in[l][:, hd * 128:(hd + 1) * 128], w_in[l][:, 512 + hd * 128:512 + (hd + 1) * 128],
                                           w_in[l][:, 1024 + hd * 256:1024 + (hd + 1) * 256], w_in[l][:, 2048 + hd * 256:2048 + (hd + 1) * 256],
                                           w_in[l][:, 3072:3088]], axis=1) for l in range(2)])
        heads = [2 * r, 2 * r + 1]
        a_wq = np.stack([np.concatenate([wqf[j][:, hh * 64:(hh + 1) * 64] if m_ == 0 else wqf[j][:, 512 + hh * 64:512 + (hh + 1) * 64]
                                         for hh in heads for m_ in range(2)], axis=1) for j in range(2)])
        a_wk = np.concatenate([kvw[:, hh * 64:(hh + 1) * 64] if m_ == 0 else kvw[:, 512 + hh * 64:512 + (hh + 1) * 64]
                               for hh in heads for m_ in range(2)], axis=1)
        a_wv = np.concatenate([kvw[:, 1024 + hh * 128:1024 + (hh + 1) * 128] for hh in heads], axis=1)
        idxv = np.zeros((128, 128), np.uint32)
        for r_ in range(4):
            for t_ in range(32):
                n_ = r * 4096 + t_ * 128
                idxv[:, r_ * 32 + t_] = (n_ // 1024) * 4096 + r_ * 1024 + (n_ % 1024) + np.arange(128)
        m = {"xb": x[b], "xs": np.ascontiguousarray(x[b, r * 4096:(r + 1) * 4096]), "idx": idxv, "g_wcat": A(g_wcat), "g_wg2": A(np.asarray(a_w_gate2)[:, :, hd * 128:(hd + 1) * 128]),
             "g_bg": A(np.asarray(a_b_gate)[:, None, hd * 128:(hd + 1) * 128]), "g_gn": A(np.asarray(a_g_norm)[:, None, hd * 256:(hd + 1) * 256]),
             "a_wq": A(a_wq), "a_wk": A(a_wk), "a_wv": A(a_wv), "a_biasT": att_bias_tiles(rt, heads),
             "a_cfar": A(np.broadcast_to(rt[15, heads][None, :], (128, 2))), "a_lamv": a_lamv, "a_gsub": a_gsub, "a_cst": a_cst,
             "p_wout": p_wout, "p_lnp": p_lnp, "p_wr": p_wr, "p_rb": p_rb, "p_wg": p_wg, "p_wu": p_wu, "p_wd": p_wd,
             "ident": gconst["ident"], "trirev": gconst["trirev"], "cind": gconst["cind"], "ones1": gconst["ones1"],
             "ones128": np.ones((128, 128), f32)}
        in_maps.append(m)
    res = run_bass_kernel_spmd(nc, in_maps, core_ids=list(range(8)))
    return np.concatenate([res.results[c]["out"] for c in range(8)], axis=0).reshape(B, S_, D)
```
